# Optimizing a Trainium2 kernel written in Bass

```python
import jax, jax.numpy as jnp
from jax import lax
import numpy as np

D_MODEL = 1024
BATCH = 4
SEQ = 4096
DEPTH = 4

CHUNK = 64
MEM_LEN = 256

POOL_GROUPS = 4
POOL_WIDTH = 512
POOL_GROUP_DIM = POOL_WIDTH // POOL_GROUPS
POOL_WINDOWS = (2, 4, 8, 16)
CONV_WIDTH = 512
DW_WIDTH = 31
ATTN_HEADS = 4
ATTN_HEAD_DIM = 128
ATTN_WIDTH = ATTN_HEADS * ATTN_HEAD_DIM
N_BRANCH = 3
COL_POOL = POOL_WIDTH
COL_CONV = COL_POOL + 2 * CONV_WIDTH
COL_Q = COL_CONV + ATTN_WIDTH
IN_COLS = COL_Q + N_BRANCH * D_MODEL
N_EXPERTS = 32
TOP_K = 4
D_FF = D_MODEL
SWIGLU_LIMIT = 7.0
SWIGLU_ALPHA = 1.702
MOE_BLOCK = 256
LN_EPS = 1e-5
DEEPNORM_ALPHA = (2.0 * DEPTH) ** 0.25
DEEPNORM_BETA = (8.0 * DEPTH) ** -0.25

kernel_name = "hybrid_pool_conv_memattn_moe_deepnorm"


def layer_norm(x, g, b):
    xf = x.astype(jnp.float32)
    mu = jnp.mean(xf, axis=-1, keepdims=True)
    var = jnp.mean(jnp.square(xf - mu), axis=-1, keepdims=True)
    y = (xf - mu) * lax.rsqrt(var + LN_EPS) * g.astype(jnp.float32) + b.astype(jnp.float32)
    return y.astype(x.dtype)


def multiscale_pool(u, w_grp, scale):
    b_, s_, _ = u.shape
    uf = u.astype(jnp.float32)
    cs = jnp.concatenate([jnp.zeros((b_, 1, POOL_WIDTH), jnp.float32),
                          jnp.cumsum(uf, axis=1)], axis=1)
    t = jnp.arange(s_)
    outs = []
    for g, w in enumerate(POOL_WINDOWS):
        sl = slice(g * POOL_GROUP_DIM, (g + 1) * POOL_GROUP_DIM)
        lo = jnp.maximum(t + 1 - w, 0)
        win_sum = cs[:, 1:, sl] - cs[:, lo, sl]
        cnt = (t + 1 - lo).astype(jnp.float32)[None, :, None]
        outs.append(win_sum / cnt - uf[:, :, sl])
    pooled = jnp.stack(outs, axis=2).astype(u.dtype)
    mixed = jnp.einsum('bsgc,gcd->bsgd', pooled, w_grp).reshape(b_, s_, POOL_WIDTH)
    return mixed * scale


def conformer_conv(u, w_dw, b_dw, g_ln, b_ln, w_pw, b_pw):
    a, gate = jnp.split(u, 2, axis=-1)
    h = a * jax.nn.sigmoid(gate)
    h = lax.conv_general_dilated(
        h, w_dw[:, None, :].astype(h.dtype), window_strides=(1,),
        padding=[(DW_WIDTH - 1, 0)], dimension_numbers=('NWC', 'WIO', 'NWC'),
        feature_group_count=CONV_WIDTH) + b_dw
    h = jax.nn.silu(layer_norm(h, g_ln, b_ln))
    return h @ w_pw + b_pw


def memory_attention(q, k, v):
    s = jnp.einsum('bshd,bmhd->bhsm', q, k).astype(jnp.float32) * (ATTN_HEAD_DIM ** -0.5)
    p = jax.nn.softmax(s, axis=-1).astype(v.dtype)
    return jnp.einsum('bhsm,bmhd->bshd', p, v)


def moe_ffn(x, w_router, b_router, w_up, b_up, w_down, b_down):
    b_, s_, d_ = x.shape
    n_tok = b_ * s_
    n_asg = n_tok * TOP_K
    xt = x.reshape(n_tok, d_)
    logits = (xt @ w_router).astype(jnp.float32) + b_router.astype(jnp.float32)
    top_logit, top_e = lax.top_k(logits, TOP_K)
    gate = jax.nn.softmax(top_logit, axis=-1)
    flat_e = top_e.reshape(-1)
    flat_tok = jnp.arange(n_asg, dtype=jnp.int32) // TOP_K
    flat_gate = gate.reshape(-1)
    order = jnp.argsort(flat_e)
    sorted_e = flat_e[order]
    counts = jnp.bincount(flat_e, length=N_EXPERTS)
    padded = (counts + MOE_BLOCK - 1) // MOE_BLOCK * MOE_BLOCK
    start = jnp.cumsum(counts) - counts
    pend = jnp.cumsum(padded)
    pstart = pend - padded
    dest = pstart[sorted_e] + (jnp.arange(n_asg, dtype=jnp.int32) - start[sorted_e])
    n_blocks = -(-n_asg // MOE_BLOCK) + N_EXPERTS
    n_rows = n_blocks * MOE_BLOCK
    row_tok = jnp.zeros((n_rows,), jnp.int32).at[dest].set(flat_tok[order])
    row_gate = jnp.zeros((n_rows,), jnp.float32).at[dest].set(flat_gate[order])
    block_e = jnp.minimum(
        jnp.searchsorted(pend, jnp.arange(n_blocks) * MOE_BLOCK, side='right'),
        N_EXPERTS - 1)
    xb = xt[row_tok].reshape(n_blocks, MOE_BLOCK, d_)

    def expert_block(args):
        xblk, e = args
        h = xblk @ w_up[e] + b_up[e]
        glu, lin = jnp.split(h, 2, axis=-1)
        glu = jnp.minimum(glu, SWIGLU_LIMIT)
        lin = jnp.clip(lin, -SWIGLU_LIMIT, SWIGLU_LIMIT)
        act = glu * jax.nn.sigmoid(SWIGLU_ALPHA * glu) * (lin + 1.0)
        return act @ w_down[e] + b_down[e]

    yb = lax.map(expert_block, (xb, block_e)).reshape(n_rows, d_)
    y = jnp.zeros((n_tok, d_), yb.dtype).at[row_tok].add(yb * row_gate[:, None].astype(yb.dtype))
    return y.reshape(b_, s_, d_).astype(x.dtype)


def setup_inputs(seed: int = 0) -> dict:
    key = jax.random.key(seed)
    ks = jax.random.split(key, 32)
    L, D = DEPTH, D_MODEL
    f32 = jnp.float32

    def nrm(k, shape, scale):
        return jax.random.normal(k, shape, f32) * scale

    w_kv_k = nrm(ks[9], (L, D, ATTN_WIDTH), D ** -0.5)
    w_kv_v = nrm(ks[10], (L, D, ATTN_WIDTH), D ** -0.5 * DEEPNORM_BETA)
    return {
        "x": nrm(ks[0], (BATCH, SEQ, D), 1.0),
        "mem": nrm(ks[1], (BATCH, MEM_LEN, D), 1.0),
        "mem_ln_g": 1.0 + nrm(ks[2], (D,), 0.02),
        "mem_ln_b": nrm(ks[3], (D,), 0.02),
        "w_in": nrm(ks[4], (L, D, IN_COLS), D ** -0.5),
        "b_in": nrm(ks[5], (L, IN_COLS), 0.01),
        "pool_w": nrm(ks[6], (L, POOL_GROUPS, POOL_GROUP_DIM, POOL_GROUP_DIM), POOL_GROUP_DIM ** -0.5),
        "pool_scale": 1.0 + nrm(ks[7], (L, POOL_WIDTH), 0.02),
        "pool_proj": nrm(ks[8], (L, POOL_WIDTH, D), POOL_WIDTH ** -0.5),
        "conv_dw": nrm(ks[11], (L, DW_WIDTH, CONV_WIDTH), DW_WIDTH ** -0.5),
        "conv_dw_b": nrm(ks[12], (L, CONV_WIDTH), 0.01),
        "conv_ln_g": 1.0 + nrm(ks[13], (L, CONV_WIDTH), 0.02),
        "conv_ln_b": nrm(ks[14], (L, CONV_WIDTH), 0.02),
        "conv_pw": nrm(ks[15], (L, CONV_WIDTH, D), CONV_WIDTH ** -0.5),
        "conv_pw_b": nrm(ks[16], (L, D), 0.01),
        "w_kv": jnp.concatenate([w_kv_k, w_kv_v], axis=-1),
        "attn_o": nrm(ks[17], (L, ATTN_WIDTH, D), ATTN_WIDTH ** -0.5),
        "w_out": nrm(ks[18], (L, D, D), D ** -0.5 * DEEPNORM_BETA),
        "b_out": nrm(ks[19], (L, D), 0.01),
        "ln1_g": 1.0 + nrm(ks[20], (L, D), 0.02),
        "ln1_b": nrm(ks[21], (L, D), 0.02),
        "router_w": nrm(ks[22], (L, D, N_EXPERTS), D ** -0.5),
        "router_b": nrm(ks[23], (L, N_EXPERTS), 0.01),
        "exp_up": nrm(ks[24], (L, N_EXPERTS, D, 2 * D_FF), D ** -0.5),
        "exp_up_b": nrm(ks[25], (L, N_EXPERTS, 2 * D_FF), 0.01),
        "exp_down": nrm(ks[26], (L, N_EXPERTS, D_FF, D), D_FF ** -0.5 * DEEPNORM_BETA),
        "exp_down_b": nrm(ks[27], (L, N_EXPERTS, D), 0.01),
        "ln2_g": 1.0 + nrm(ks[28], (L, D), 0.02),
        "ln2_b": nrm(ks[29], (L, D), 0.02),
    }


def reference(x, mem, mem_ln_g, mem_ln_b, w_in, b_in, pool_w, pool_scale, pool_proj,
              conv_dw, conv_dw_b, conv_ln_g, conv_ln_b, conv_pw, conv_pw_b,
              w_kv, attn_o, w_out, b_out, ln1_g, ln1_b,
              router_w, router_b, exp_up, exp_up_b, exp_down, exp_down_b,
              ln2_g, ln2_b):
    b_, s_, d_ = x.shape
    m_ = mem.shape[1]
    mem_n = layer_norm(mem, mem_ln_g, mem_ln_b)
    for l in range(DEPTH):
        h = x @ w_in[l] + b_in[l]
        u_pool = h[..., :COL_POOL]
        u_conv = h[..., COL_POOL:COL_CONV]
        q = h[..., COL_CONV:COL_Q].reshape(b_, s_, ATTN_HEADS, ATTN_HEAD_DIM)
        gates = jax.nn.sigmoid(h[..., COL_Q:].reshape(b_, s_, N_BRANCH, d_))

        y_pool = multiscale_pool(u_pool, pool_w[l], pool_scale[l]) @ pool_proj[l]
        y_conv = conformer_conv(u_conv, conv_dw[l], conv_dw_b[l], conv_ln_g[l], conv_ln_b[l],
                                conv_pw[l], conv_pw_b[l])
        kv = mem_n @ w_kv[l]
        k = kv[..., :ATTN_WIDTH].reshape(b_, m_, ATTN_HEADS, ATTN_HEAD_DIM)
        v = kv[..., ATTN_WIDTH:].reshape(b_, m_, ATTN_HEADS, ATTN_HEAD_DIM)
        y_attn = memory_attention(q, k, v).reshape(b_, s_, ATTN_WIDTH) @ attn_o[l]

        merged = (gates[:, :, 0, :] * y_pool + gates[:, :, 1, :] * y_conv
                  + gates[:, :, 2, :] * y_attn)
        x = layer_norm(DEEPNORM_ALPHA * x + (merged @ w_out[l] + b_out[l]), ln1_g[l], ln1_b[l])

        y_moe = moe_ffn(x, router_w[l], router_b[l], exp_up[l], exp_up_b[l],
                        exp_down[l], exp_down_b[l])
        x = layer_norm(DEEPNORM_ALPHA * x + y_moe, ln2_g[l], ln2_b[l])
    return x
```

```python
import contextlib
import numpy as np
import ml_dtypes
import concourse.bass as bass
import concourse.mybir as mybir
from concourse.bass_utils import run_bass_kernel_spmd

F32 = mybir.dt.float32
BF16 = mybir.dt.bfloat16
I32 = mybir.dt.int32
AF = mybir.ActivationFunctionType
ALU = mybir.AluOpType

D = 1024
NT = 17
T = NT * 128
SEQ = 4096
DEPTH = 4
NE = 32
CAP = 384
NSLOT = 1 + NE * CAP
IN_COLS = 5120
MEM = 256
ALPHA = (2.0 * DEPTH) ** 0.25
LN_EPS = 1e-5
CBS = [(0, 512), (512, 512), (1024, 512), (1536, 512), (2048, 128)]
NW = 4
ATT_SCALE = 128 ** -0.5

C_BIN = 0
C_PSC = 40
C_CDW = 44
C_CDWB = 168
C_CLG = 172
C_CLB = 176
C_CPWB = 180
C_UPB = 188
NCOLP = 700
K_ID = 0
K_TRI = 128
K_ONE = 256
K_VM = 384
K_VT = 512
K_PF = 513
K_B1 = 577
NCST = 609


ENGS = ("pe", "act", "dve", "pool", "sp")


class Reg:
    __slots__ = ("w", "r", "rd")

    def __init__(self):
        self.w = None
        self.r = {}
        self.rd = []


class Op:
    __slots__ = ("eng", "fn", "deps", "is_dma", "sem", "val", "needed", "epoch")


class Sched:
    def __init__(self, nc, n_dma_sems=20):
        self.nc = nc
        self.q = {e: [] for e in ENGS}
        self.n_dma_sems = n_dma_sems
        self.epoch = 0
        self.pending = {}
        self.last_compute = {}
        self.dma_since_barrier = []
        self.prologue = {}

    def _mk(self, eng, fn, reads, writes, deps, is_dma, bar=True):
        o = Op()
        o.eng = eng
        o.fn = fn
        o.is_dma = is_dma
        o.sem = None
        o.val = None
        o.needed = is_dma
        o.epoch = self.epoch
        d = [x for x in deps if x is not None]
        for r in reads:
            if r.w is not None:
                d.append(r.w)
        for w in writes:
            if w.w is not None:
                d.append(w.w)
            d.extend(w.r.values())
            d.extend(w.rd)
        pb = self.pending.pop(eng, None)
        if pb:
            d.extend(pb)
        o.deps = d
        for r in reads:
            if is_dma:
                r.rd.append(o)
            else:
                r.r[eng] = o
        for w in writes:
            w.w = o
            w.r = {}
            w.rd = []
        self.q[eng].append(o)
        if is_dma:
            if bar:
                self.dma_since_barrier.append(o)
        else:
            self.last_compute[eng] = o
        return o

    def op(self, eng, fn, reads=(), writes=(), deps=()):
        return self._mk(eng, fn, reads, writes, deps, False)

    def dma(self, eng, fn, reads=(), writes=(), deps=(), bar=True):
        return self._mk(eng, fn, reads, writes, deps, True, bar)

    def barrier(self):
        lst = list(self.last_compute.values()) + list(self.dma_since_barrier)
        self.dma_since_barrier = []
        for e in ENGS:
            self.pending[e] = list(lst) + self.pending.get(e, [])

    def finalize(self, final_ops):
        nc = self.nc
        for e in ENGS:
            for o in self.q[e]:
                for d in o.deps:
                    d.needed = True
        for o in final_ops:
            o.needed = True
        n_epochs = self.epoch + 1
        with contextlib.ExitStack() as stack:
            sems = {}
            for e in ENGS:
                for ep in range(n_epochs):
                    sems[(e, ep)] = stack.enter_context(nc.semaphore(f"s_{e}{ep}"))
            dsems = {}
            for e in ("sp", "act", "pool"):
                for i in range(self.n_dma_sems):
                    dsems[(e, i)] = stack.enter_context(nc.semaphore(f"d_{e}{i}"))
            cnt = {}
            slot_next = {e: 0 for e in ENGS}
            slot_cnt = {}
            slot_last = {}
            for e in ENGS:
                for o in self.q[e]:
                    if o.is_dma:
                        s = slot_next[e]
                        slot_next[e] = (s + 1) % self.n_dma_sems
                        key = (e, s)
                        prev = slot_last.get(key)
                        if prev is not None:
                            o.deps.append(prev)
                        slot_cnt[key] = slot_cnt.get(key, 0) + 16
                        slot_last[key] = o
                        o.sem = dsems[key]
                        o.val = slot_cnt[key]
                    elif o.needed:
                        k = (e, o.epoch)
                        cnt[k] = cnt.get(k, 0) + 1
                        o.sem = sems[k]
                        o.val = cnt[k]
            self.max_counts = cnt
            block = stack.enter_context(nc.Block())
            engmap = {"pe": block.tensor, "act": block.scalar, "dve": block.vector,
                      "pool": block.gpsimd, "sp": block.sync}

            def make(e):
                def body(eng):
                    seen = {}
                    for pf in self.prologue.get(e, ()):
                        pf(eng)
                    for o in self.q[e]:
                        for d in o.deps:
                            if (not d.is_dma) and d.eng == e and e == "pe":
                                continue
                            k = id(d.sem)
                            if seen.get(k, 0) >= d.val:
                                continue
                            seen[k] = d.val
                            eng.wait_ge(d.sem, d.val)
                        inst = o.fn(eng)
                        if o.sem is not None:
                            inst.then_inc(o.sem, 16 if o.is_dma else 1)
                    if e == "sp":
                        for o in final_ops:
                            if seen.get(id(o.sem), 0) >= o.val:
                                continue
                            seen[id(o.sem)] = o.val
                            eng.wait_ge(o.sem, o.val)
                return body

            for e in ENGS:
                engmap[e](make(e))


def build_program(NL, first_layer_is_input=True):
    nc = bass.Bass("TRN2", target_bir_lowering=False)
    dt_in = lambda n, s, d=F32: nc.dram_tensor(n, s, d, kind="ExternalInput")
    x_in = dt_in("x_in", [T, D])
    mem_in = dt_in("mem", [MEM, D])
    memln = dt_in("memln", [1, 2 * D])
    cst_in = dt_in("cst", [128, NCST])
    w_in = dt_in("w_in", [NL, D, IN_COLS])
    pool_w = dt_in("pool_w", [NL, 4, 128, 128])
    pool_proj = dt_in("pool_proj", [NL, 512, D])
    conv_pw = dt_in("conv_pw", [NL, 512, D])
    attn_o = dt_in("attn_o", [NL, 512, D])
    w_kv = dt_in("w_kv", [NL, D, D])
    w_out = dt_in("w_out", [NL, D, D])
    exp_up = dt_in("exp_up", [NL, NE, D, 2 * D])
    exp_down = dt_in("exp_down", [NL, NE, D, D])
    router_w = dt_in("router_w", [NL, D, NE])
    exp_down_b = dt_in("exp_down_b", [NL, NE, D])
    colp_in = dt_in("colp", [NL, 128, NCOLP])
    rowbc_in = dt_in("rowbc", [NL, 1, 4 * D])
    rowsm_in = dt_in("rowsm", [NL, 1, D + NE])
    out_d = nc.dram_tensor("out", [T - 128, D], F32, kind="ExternalOutput")
    xres = nc.dram_tensor("xres", [T, D], F32, kind="Internal")
    xd = nc.dram_tensor("xd", [NSLOT, D], BF16, kind="Internal")
    ys = nc.dram_tensor("ys", [NSLOT, D], BF16, kind="Internal")

    with contextlib.ExitStack() as st:
        sb = lambda n, s, d: st.enter_context(nc.sbuf_tensor("sb_" + n, s, d))
        S = Sched(nc)

        xT = sb("xT", [128, 8, T], BF16)
        mergedT = sb("mergedT", [128, 8, T], BF16)
        brT = sb("brT", [128, 4, T], BF16)
        ring = [sb(f"ring{i}", [128, 4096], BF16) for i in range(NW)]
        cst = sb("cst", [128, NCST], F32)
        cbf = sb("cbf", [128, 384], BF16)
        colp = sb("colp", [128, NCOLP], F32)
        upb1 = sb("upb1", [128, NE * 8], F32)
        lnbc = sb("lnbc", [128, 4 * D], F32)
        rowsm = sb("rowsm", [1, D + NE], F32)
        wr = sb("wr", [128, 8, NE], F32)
        bdown = sb("bdown", [NE, D], F32)
        memT = sb("memT", [128, 8, MEM], BF16)
        kT = sb("kT", [128, 4, MEM], BF16)
        vv = sb("vv", [128, 2, 512], BF16)
        poolw = sb("poolw", [128, 4, 128], BF16)
        idx_all = sb("idx_all", [128, NT * 4], I32)
        gate_all = sb("gate_all", [128, NT * 4], F32)
        G_all = sb("G_all", [128, NT, NE], F32)
        ARENA_BF = 24064
        arena = sb("arena", [128, ARENA_BF], BF16)
        ps = [st.enter_context(nc.psum_tensor(f"ps{i}", [128, 1024], F32)) for i in range(4)]

        epsc = sb("epsc", [128, 1], F32)
        r_eps = Reg()
        ident_bf = cbf[:, 0:128]
        tri_bf = cbf[:, 128:256]
        ones_bf = cbf[:, 256:384]
        ident_f = cst[:, K_ID:K_ID + 128]
        onesrow_f = cst[0:1, K_ONE:K_ONE + 128]
        vmask = cst[:, K_VM:K_VM + 128]
        vtok = cst[:, K_VT:K_VT + 1]
        base1 = cst[:, K_B1:K_B1 + 32]

        R = lambda: Reg()
        r_xT = [R() for _ in range(NT)]
        r_mg = [R() for _ in range(NT)]
        r_br = [[R() for _ in CBS] for _ in range(4)]
        r_ring = [R() for _ in range(NW)]
        r_cst, r_cbf, r_colp, r_upb1, r_lnbc, r_rowsm, r_wr, r_bdown = (R() for _ in range(8))
        r_memT, r_kT, r_vv, r_poolw = R(), R(), R(), R()
        r_idx = [R() for _ in range(NT)]
        r_gate = [R() for _ in range(NT)]
        r_G = [R() for _ in range(NT)]
        r_bank = [R() for _ in range(8)]

        def bank(b):
            return ps[b // 2][:, (b % 2) * 512:(b % 2) * 512 + 512]

        def bank_bf(b):
            return ps[b // 2][:, (b % 2) * 512:(b % 2) * 512 + 512].bitcast(BF16)

        bank_rr = [0]

        def next_bank():
            b = bank_rr[0]
            bank_rr[0] = (b + 1) % 8
            return b

        def next_bank2():
            b = bank_rr[0]
            if b % 2:
                b = (b + 1) % 8
            bank_rr[0] = (b + 2) % 8
            return b

        class Arena:
            def __init__(self):
                self.off = 0

            def reset(self):
                S.barrier()
                self.off = 0

            def alloc(self, shape, dtype):
                n = 1
                for s_ in shape[1:]:
                    n *= s_
                nb = n * (2 if dtype == BF16 else 4)
                self.off = (self.off + 63) // 64 * 64
                o_bf = self.off // 2
                self.off += nb
                assert self.off <= ARENA_BF * 2, f"arena overflow {self.off}"
                v = arena[0:shape[0], o_bf:o_bf + nb // 2]
                if dtype != BF16:
                    v = v.bitcast(dtype)
                if len(shape) == 3:
                    v = v.rearrange("p (a b) -> p a b", a=shape[1])
                return v

        AR = Arena()
        regs = {}
        S.prologue["pool"] = [lambda e: regs.__setitem__("bc", e.to_reg(NSLOT - 1))]

        def mm(out, lhsT, rhs, start, stop, reads, writes):
            return S.op("pe", lambda e: e.matmul(out, lhsT=lhsT, rhs=rhs, start=start, stop=stop),
                        reads, writes)

        def tr(out, in_, ident, reads, writes):
            return S.op("pe", lambda e: e.transpose(out=out, in_=in_, identity=ident), reads, writes)

        def act(out, in_, func, reads, writes, bias=None, scale=None, eng="act"):
            kw = {}
            if bias is not None:
                kw["bias"] = bias
            if scale is not None:
                kw["scale"] = scale
            return S.op(eng, lambda e: e.activation(out=out, in_=in_, func=func, **kw), reads, writes)

        def ts(eng, out, in0, s1, s2, op0, op1, reads, writes, accum_out=None):
            kw = {}
            if op1 is not None:
                kw["op1"] = op1
            if accum_out is not None:
                kw["accum_out"] = accum_out
            return S.op(eng, lambda e: e.tensor_scalar(out=out, in0=in0, scalar1=s1, scalar2=s2,
                                                       op0=op0, **kw), reads, writes)

        def tt(eng, out, in0, in1, op, reads, writes):
            return S.op(eng, lambda e: e.tensor_tensor(out=out, in0=in0, in1=in1, op=op), reads, writes)

        def stt(eng, out, in0, scalar, in1, op0, op1, reads, writes, accum_out=None):
            kw = {}
            if accum_out is not None:
                kw["accum_out"] = accum_out
            return S.op(eng, lambda e: e.scalar_tensor_tensor(out=out, in0=in0, scalar=scalar, in1=in1,
                                                              op0=op0, op1=op1, **kw), reads, writes)

        def cp(eng, out, in_, reads, writes):
            if eng == "act":
                return S.op(eng, lambda e: e.activation(out=out, in_=in_, func=AF.Copy), reads, writes)
            return S.op(eng, lambda e: e.tensor_copy(out=out, in_=in_), reads, writes)

        def memset(eng, ap, val, writes):
            return S.op(eng, lambda e: e.memset(ap, val), (), writes)

        def dma(eng, out, in_, reads, writes, deps=(), bar=True):
            return S.dma(eng, lambda e: e.dma_start(out=out, in_=in_), reads, writes, deps, bar)

        memset("dve", epsc[:, :], LN_EPS, [r_eps])

        plan = []
        kinds = []

        def kcview(ap2d):
            return ap2d.rearrange("(kc p) f -> p kc f", p=128)

        for l in range(NL):
            plan.append(kcview(w_kv[l][:, 0:512]))
            plan.append(kcview(w_kv[l][:, 512:1024]))
            plan.append(kcview(w_in[l][:, 0:512]))
            plan.append(kcview(pool_proj[l]))
            plan.append(kcview(w_in[l][:, 2048:2560]))
            plan.append(kcview(w_in[l][:, 2560:3072]))
            plan.append(kcview(w_in[l][:, 512:1024]))
            plan.append(kcview(w_in[l][:, 1024:1536]))
            plan.append(kcview(conv_pw[l]))
            plan.append(kcview(w_in[l][:, 3072:3584]))
            plan.append(kcview(w_in[l][:, 3584:4096]))
            plan.append(kcview(w_in[l][:, 1536:2048]))
            plan.append(kcview(attn_o[l]))
            plan.append(kcview(w_in[l][:, 4096:4608]))
            plan.append(kcview(w_in[l][:, 4608:5120]))
            plan.append(kcview(w_out[l][:, 0:512]))
            plan.append(kcview(w_out[l][:, 512:1024]))
            kinds.extend(["m"] * 17)
            def _up(e, hf):
                kinds.extend(["e"] * 2)
                plan.append(kcview(exp_up[l, e][:, hf * 512:(hf + 1) * 512]))
                plan.append(kcview(exp_up[l, e][:, 1024 + hf * 512:1024 + (hf + 1) * 512]))

            def _down(e):
                kinds.extend(["e"] * 2)
                plan.append(kcview(exp_down[l, e][:, 0:512]))
                plan.append(kcview(exp_down[l, e][:, 512:1024]))

            _up(0, 0)
            _up(0, 1)
            for e in range(NE):
                if e + 1 < NE:
                    _up(e + 1, 0)
                _down(e)
                if e + 1 < NE:
                    _up(e + 1, 1)

        NWE = NW + 8
        xTflat = xT[:, :, :].rearrange("p a b -> p (a b)")
        mgflat = mergedT[:, :, :].rearrange("p a b -> p (a b)")
        ringv = [ring[i][:, :] for i in range(NW)]
        ringv += [xTflat[:, q_ * 4096:(q_ + 1) * 4096] for q_ in range(4)]
        ringv += [mgflat[:, q_ * 4096:(q_ + 1) * 4096] for q_ in range(4)]
        r_ring.extend(Reg() for _ in range(8))

        class Ring:
            def __init__(self):
                self.next_load = 0
                self.next_acq = 0
                self.released = [False] * len(plan)
                self.allow_alias = False
                self.slot = []
                self.prev = []
                last = {}
                bc = ec = 0
                for n, k_ in enumerate(kinds):
                    if k_ == "m":
                        sl = bc % NW
                        bc += 1
                    else:
                        sl = ec % NWE
                        ec += 1
                    self.slot.append(sl)
                    self.prev.append(last.get(sl))
                    last[sl] = n

            def _try_load(self):
                while self.next_load < len(plan):
                    n = self.next_load
                    pv = self.prev[n]
                    if pv is not None and not self.released[pv]:
                        break
                    sl = self.slot[n]
                    if sl >= NW and not self.allow_alias:
                        break
                    src = plan[n]
                    a = src.shape[1]
                    dst = ringv[sl].rearrange("p (a b) -> p a b", a=a)
                    dma("pool", dst, src, (), [r_ring[sl]], bar=False)
                    self.next_load += 1

            def acquire(self, a):
                self._try_load()
                n = self.next_acq
                assert n < self.next_load, "ring: item not loaded (release order problem)"
                assert plan[n].shape[1] == a
                self.next_acq += 1
                sl = self.slot[n]
                v = ringv[sl].rearrange("p (a b) -> p a b", a=a)
                return n, v, r_ring[sl]

            def release(self, n):
                self.released[n] = True
                self._try_load()

            def set_alias(self, flag):
                self.allow_alias = flag
                if flag:
                    self._try_load()

        RG = Ring()

        dma("sp", cst[:, :], cst_in[:, :], (), [r_cst])
        cp("dve", cbf[:, :], cst[:, 0:384], [r_cst], [r_cbf])

        def ln_tmp():
            return (AR.alloc([128, 12], F32), AR.alloc([128, 2], F32), AR.alloc([128, 1], F32),
                    AR.alloc([128, 1], F32), R(), R(), R(), R())

        def ln_stats(zt, r_z, tmp_):
            stt_, mv, rs, nmr, r_s, r_mv, r_rs, r_nmr = tmp_
            S.op("dve", lambda e: e.bn_stats(out=stt_[:, 0:6], in_=zt[:, 0:512]), [r_z], [r_s])
            S.op("dve", lambda e: e.bn_stats(out=stt_[:, 6:12], in_=zt[:, 512:1024]), [r_z], [r_s])
            S.op("dve", lambda e: e.bn_aggr(out=mv, in_=stt_), [r_s], [r_mv])
            act(rs, mv[:, 1:2], AF.Sqrt, [r_mv, r_eps], [r_rs], bias=epsc[:, 0:1])

        def ln_norm(zt, r_z, tmp_):
            stt_, mv, rs, nmr, r_s, r_mv, r_rs, r_nmr = tmp_
            S.op("dve", lambda e: e.reciprocal(out=rs, in_=rs), [r_rs], [r_rs])
            ts("dve", nmr, mv[:, 0:1], rs[:, 0:1], -1.0, ALU.mult, ALU.mult, [r_mv, r_rs], [r_nmr])
            act(zt, zt, AF.Identity, [r_z, r_rs, r_nmr], [r_z], bias=nmr[:, 0:1], scale=rs[:, 0:1])

        def ln_affine(zt, r_z, gv, bv, r_gb):
            tt("dve", zt, zt, gv, ALU.mult, [r_z, r_gb], [r_z])
            tt("dve", zt, zt, bv, ALU.add, [r_z, r_gb], [r_z])

        def ln_apply(zt, r_z, gv, bv, r_gb, tmp_):
            ln_norm(zt, r_z, tmp_)
            ln_affine(zt, r_z, gv, bv, r_gb)

        def ln_rows(zt, r_z, out32, r_out, gv, bv, r_gb, tmp_):
            ln_stats(zt, r_z, tmp_)
            ln_apply(zt, r_z, gv, bv, r_gb, tmp_)

        def to_xT(src_bf, r_src, i):
            b = next_bank()
            pb = bank_bf(b)
            for kc in range(8):
                tr(pb[:, kc * 128:(kc + 1) * 128], src_bf[:, kc * 128:(kc + 1) * 128], ident_bf,
                   [r_src, r_cbf], [r_bank[b]])
            cp("act" if i % 2 else "dve", xT[:, :, i * 128:(i + 1) * 128],
               pb.rearrange("p (a b) -> p a b", a=8), [r_bank[b]], [r_xT[i]])

        AR.reset()
        mg_bc = AR.alloc([128, 2 * D], F32)
        r_mgbc = R()
        lnt = [ln_tmp(), ln_tmp()]
        zer = AR.alloc([128, 8 * D], BF16)
        r_zer = R()
        memset("pool", zer, 0.0, [r_zer])
        for j_ in range(NE * CAP // 1024):
            dma("sp", xd[1 + j_ * 1024:1 + (j_ + 1) * 1024, :].rearrange("(p r) d -> p r d", p=128),
                zer.rearrange("p (r d) -> p r d", r=8), [r_zer], ())
        dma("sp", xd[0:1, :], zer[0:1, 0:D], [r_zer], ())
        dma("sp", ys[0:1, :], zer[0:1, 0:D], [r_zer], ())
        dma("sp", mg_bc, memln[0:1, :].to_broadcast([128, 2 * D]), (), [r_mgbc])
        for mt in range(2):
            mtile = AR.alloc([128, D], F32)
            mbf = AR.alloc([128, D], BF16)
            r_mt, r_mb = R(), R()
            dma("sp", mtile, mem_in[mt * 128:(mt + 1) * 128, :], (), [r_mt])
            ln_rows(mtile, r_mt, mtile, r_mt, mg_bc[:, 0:D], mg_bc[:, D:2 * D], r_mgbc, lnt[mt])
            cp("act", mbf, mtile, [r_mt], [r_mb])
            b = next_bank()
            pb = bank_bf(b)
            for kc in range(8):
                tr(pb[:, kc * 128:(kc + 1) * 128], mbf[:, kc * 128:(kc + 1) * 128], ident_bf,
                   [r_mb, r_cbf], [r_bank[b]])
            cp("dve", memT[:, :, mt * 128:(mt + 1) * 128], pb.rearrange("p (a b) -> p a b", a=8),
               [r_bank[b]], [r_memT])

        AR.reset()
        xl = [AR.alloc([128, D], F32) for _ in range(2)]
        xb = [AR.alloc([128, D], BF16) for _ in range(2)]
        r_xl = [R(), R()]
        r_xb = [R(), R()]
        for i in range(NT):
            k = i % 2
            dma("sp", xl[k], x_in[i * 128:(i + 1) * 128, :], (), [r_xl[k]])
            cp("act" if i % 2 == 0 else "dve", xb[k], xl[k], [r_xl[k]], [r_xb[k]])
            to_xT(xb[k], r_xb[k], i)

        res_src = {i: (x_in, None) for i in range(NT)}
        final_ops = []

        for l in range(NL):
            last = (l == NL - 1)
            S.epoch += 1
            AR.reset()
            dma("sp", colp[:, :], colp_in[l], (), [r_colp])
            dma("sp", lnbc[:, :], rowbc_in[l].to_broadcast([128, 4 * D]), (), [r_lnbc])
            dma("sp", rowsm[:, :], rowsm_in[l], (), [r_rowsm])
            dma("sp", wr[:, :, :], router_w[l].rearrange("(kc p) e -> p kc e", p=128), (), [r_wr])
            dma("sp", bdown[:, :], exp_down_b[l], (), [r_bdown])
            dma("pool", poolw[:, :, :], pool_w[l].rearrange("g c d -> c g d"), (), [r_poolw])
            ts("dve", upb1[:, :].rearrange("p (e j) -> p e j", e=NE),
               colp[:, C_UPB:C_UPB + 512].rearrange("p (e j) -> p e j", e=NE)[:, :, 8:16],
               1.0, None, ALU.add, None, [r_colp], [r_upb1])

            def bcol(c):
                return colp[:, c:c + 1]

            n_k, Wk, r_Wk = RG.acquire(8)
            for hd in range(4):
                b = next_bank()
                for kc in range(8):
                    mm(bank(b)[:, 0:MEM], Wk[:, kc, hd * 128:(hd + 1) * 128], memT[:, kc, :],
                       kc == 0, kc == 7, [r_Wk, r_memT], [r_bank[b]])
                cp("act", kT[:, hd, :], bank(b)[:, 0:MEM], [r_bank[b]], [r_kT])
            RG.release(n_k)
            n_v, Wv, r_Wv = RG.acquire(8)
            for mc in range(2):
                b = next_bank()
                for kc in range(8):
                    mm(bank(b), memT[:, kc, mc * 128:(mc + 1) * 128], Wv[:, kc, :],
                       kc == 0, kc == 7, [r_Wv, r_memT], [r_bank[b]])
                cp("act", vv[:, mc, :], bank(b), [r_bank[b]], [r_vv])
            RG.release(n_v)

            def xT_regs(cbi):
                c0, n = CBS[cbi]
                return [r_xT[i] for i in range(c0 // 128, (c0 + n) // 128)]

            def mg_regs(cbi):
                c0, n = CBS[cbi]
                return [r_mg[i] for i in range(c0 // 128, (c0 + n) // 128)]

            def gate_pass(br, P, r_P, bias_cols=None):
                sg = [AR.alloc([128, 512], F32) for _ in range(2)]
                tmp = [AR.alloc([128, 512], F32) for _ in range(2)]
                r_sg = [R(), R()]
                r_tmp = [R(), R()]
                it = 0
                for half in range(2):
                    n_g, G, r_Gs = RG.acquire(8)
                    for jj in range(4):
                        j = half * 4 + jj
                        for cbi, (c0, n) in enumerate(CBS):
                            bg = next_bank()
                            for kc in range(8):
                                mm(bank(bg)[:, 0:n], G[:, kc, jj * 128:(jj + 1) * 128], xT[:, kc, c0:c0 + n],
                                   kc == 0, kc == 7, [r_Gs] + xT_regs(cbi), [r_bank[bg]])
                            by = next_bank()
                            for kc in range(4):
                                mm(bank(by)[:, 0:n], P[:, kc, j * 128:(j + 1) * 128], brT[:, kc, c0:c0 + n],
                                   kc == 0, kc == 3, [r_P] + [r_br[kc][cbi]], [r_bank[by]])
                            k = it % 2
                            it += 1
                            act(sg[k][:, 0:n], bank(bg)[:, 0:n], AF.Sigmoid, [r_bank[bg], r_colp], [r_sg[k]],
                                bias=bcol(C_BIN + 16 + br * 8 + j))
                            mdst = mergedT[:, j, c0:c0 + n]
                            if br == 0:
                                tt("dve", mdst, sg[k][:, 0:n], bank(by)[:, 0:n], ALU.mult,
                                   [r_sg[k], r_bank[by]], mg_regs(cbi))
                            else:
                                if bias_cols is not None:
                                    stt("dve", tmp[k][:, 0:n], bank(by)[:, 0:n], bcol(bias_cols + j), sg[k][:, 0:n],
                                        ALU.add, ALU.mult, [r_bank[by], r_sg[k], r_colp], [r_tmp[k]])
                                else:
                                    tt("dve", tmp[k][:, 0:n], sg[k][:, 0:n], bank(by)[:, 0:n], ALU.mult,
                                       [r_sg[k], r_bank[by]], [r_tmp[k]])
                                tt("dve", mdst, mdst, tmp[k][:, 0:n], ALU.add, [r_tmp[k]] + mg_regs(cbi),
                                   mg_regs(cbi))
                    RG.release(n_g)

            AR.reset()
            n_wp, Wp, r_Wp = RG.acquire(8)
            u = AR.alloc([128, 16 + T], F32)
            sA = AR.alloc([128, 16 + T], F32)
            sB = AR.alloc([128, 16 + T], F32)
            pooled = AR.alloc([128, T], BF16)
            r_u, r_sA, r_sB, r_pl = R(), R(), R(), R()
            memset("pool", u[:, 0:16], 0.0, [r_u])
            memset("pool", sA[:, 0:16], 0.0, [r_sA])
            memset("pool", sB[:, 0:16], 0.0, [r_sB])
            for g in range(4):
                for cbi, (c0, n) in enumerate(CBS):
                    b = next_bank()
                    for kc in range(8):
                        mm(bank(b)[:, 0:n], Wp[:, kc, g * 128:(g + 1) * 128], xT[:, kc, c0:c0 + n],
                           kc == 0, kc == 7, [r_Wp] + xT_regs(cbi), [r_bank[b]])
                    act(u[:, 16 + c0:16 + c0 + n], bank(b)[:, 0:n], AF.Identity, [r_bank[b], r_colp], [r_u],
                        bias=bcol(C_BIN + g))
                tt("dve", u[:, 16:144], u[:, 16:144], vmask, ALU.mult, [r_u, r_cst], [r_u])
                src, r_src = u, r_u
                bufs = [(sA, r_sA), (sB, r_sB)]
                sh = 1
                for step in range(g + 1):
                    dst, r_dst = bufs[step % 2]
                    tt("dve", dst[:, 16:16 + T], src[:, 16:16 + T], src[:, 16 - sh:16 + T - sh], ALU.add,
                       [r_src], [r_dst])
                    src, r_src = dst, r_dst
                    sh *= 2
                w_ = 2 ** (g + 1)
                tt("dve", src[:, 16 + 128:16 + 144], src[:, 16 + 128:16 + 144],
                   cst[:, K_PF + g * 16:K_PF + (g + 1) * 16], ALU.mult, [r_src, r_cst], [r_src])
                stt("dve", pooled[:, :], src[:, 16:16 + T], 1.0 / w_, u[:, 16:16 + T], ALU.mult, ALU.subtract,
                    [r_src, r_u], [r_pl])
                for cbi, (c0, n) in enumerate(CBS):
                    b = next_bank()
                    mm(bank(b)[:, 0:n], poolw[:, g, :], pooled[:, c0:c0 + n], True, True,
                       [r_poolw, r_pl], [r_bank[b]])
                    act(brT[:, g, c0:c0 + n], bank(b)[:, 0:n], AF.Identity, [r_bank[b], r_colp], [r_br[g][cbi]],
                        scale=bcol(C_PSC + g))
            RG.release(n_wp)
            n_pp, Pp, r_Pp = RG.acquire(4)
            gate_pass(0, Pp, r_Pp)
            RG.release(n_pp)

            AR.reset()
            n_wa, Wa, r_Wa = RG.acquire(8)
            n_wg, Wg, r_Wg = RG.acquire(8)
            diag = AR.alloc([128, 31, 128], BF16)
            hb = [AR.alloc([128, 32 + T], BF16) for _ in range(2)]
            sgc = [AR.alloc([128, 512], F32) for _ in range(2)]
            r_diag = R()
            r_hb = [R(), R()]
            r_sgc = [R(), R()]
            memset("pool", hb[0][:, 0:32], 0.0, [r_hb[0]])
            memset("pool", hb[1][:, 0:32], 0.0, [r_hb[1]])
            it = 0
            for c in range(4):
                h_, r_h = hb[c % 2], r_hb[c % 2]
                for k in range(31):
                    ts("dve", diag[:, k, :], ident_bf, bcol(C_CDW + c * 31 + k), None,
                       ALU.mult, None, [r_cbf, r_colp], [r_diag])
                for cbi, (c0, n) in enumerate(CBS):
                    ba = next_bank()
                    for kc in range(8):
                        mm(bank(ba)[:, 0:n], Wa[:, kc, c * 128:(c + 1) * 128], xT[:, kc, c0:c0 + n],
                           kc == 0, kc == 7, [r_Wa] + xT_regs(cbi), [r_bank[ba]])
                    bg = next_bank()
                    for kc in range(8):
                        mm(bank(bg)[:, 0:n], Wg[:, kc, c * 128:(c + 1) * 128], xT[:, kc, c0:c0 + n],
                           kc == 0, kc == 7, [r_Wg] + xT_regs(cbi), [r_bank[bg]])
                    k = it % 2
                    it += 1
                    act(sgc[k][:, 0:n], bank(bg)[:, 0:n], AF.Sigmoid, [r_bank[bg], r_colp], [r_sgc[k]],
                        bias=bcol(C_BIN + 8 + c))
                    stt("dve", h_[:, 32 + c0:32 + c0 + n], bank(ba)[:, 0:n], bcol(C_BIN + 4 + c), sgc[k][:, 0:n],
                        ALU.add, ALU.mult, [r_bank[ba], r_sgc[k], r_colp], [r_h])
                tt("dve", h_[:, 32:160], h_[:, 32:160], vmask, ALU.mult, [r_h, r_cst], [r_h])
                for cbi, (c0, n) in enumerate(CBS):
                    b = next_bank()
                    for k in range(31):
                        mm(bank(b)[:, 0:n], diag[:, k, :], h_[:, 32 + c0 - 30 + k:32 + c0 - 30 + k + n],
                           k == 0, k == 30, [r_diag, r_h], [r_bank[b]])
                    act(brT[:, c, c0:c0 + n], bank(b)[:, 0:n], AF.Identity, [r_bank[b], r_colp], [r_br[c][cbi]],
                        bias=bcol(C_CDWB + c))
            RG.release(n_wa)
            RG.release(n_wg)
            sq = AR.alloc([128, 4, 512], BF16)
            mean = AR.alloc([128, 512], F32)
            msq = AR.alloc([128, 512], F32)
            rstd = AR.alloc([128, 512], F32)
            zc = [AR.alloc([128, 512], F32) for _ in range(2)]
            r_sq, r_mean, r_msq, r_rstd = R(), R(), R(), R()
            r_zc = [R(), R()]
            for cbi, (c0, n) in enumerate(CBS):
                for c in range(4):
                    act(sq[:, c, 0:n], brT[:, c, c0:c0 + n], AF.Square, [r_br[c][cbi]], [r_sq])
                b1 = next_bank()
                for c in range(4):
                    mm(bank(b1)[:, 0:n], ones_bf, brT[:, c, c0:c0 + n], c == 0, c == 3,
                       [r_cbf, r_br[c][cbi]], [r_bank[b1]])
                b2 = next_bank()
                for c in range(4):
                    mm(bank(b2)[:, 0:n], ones_bf, sq[:, c, 0:n], c == 0, c == 3, [r_cbf, r_sq], [r_bank[b2]])
                ts("dve", mean[:, 0:n], bank(b1)[:, 0:n], 1.0 / 512, None, ALU.mult, None, [r_bank[b1]], [r_mean])
                tt("dve", msq[:, 0:n], mean[:, 0:n], mean[:, 0:n], ALU.mult, [r_mean], [r_msq])
                stt("dve", rstd[:, 0:n], bank(b2)[:, 0:n], 1.0 / 512, msq[:, 0:n], ALU.mult, ALU.subtract,
                    [r_bank[b2], r_msq], [r_rstd])
                ts("dve", rstd[:, 0:n], rstd[:, 0:n], 0.0, None, ALU.max, None, [r_rstd], [r_rstd])
                act(rstd[:, 0:n], rstd[:, 0:n], AF.Ln, [r_rstd, r_eps], [r_rstd], bias=epsc[:, 0:1])
                act(rstd[:, 0:n], rstd[:, 0:n], AF.Exp, [r_rstd], [r_rstd], scale=-0.5)
                for c in range(4):
                    k = c % 2
                    tt("dve", zc[k][:, 0:n], brT[:, c, c0:c0 + n], mean[:, 0:n], ALU.subtract,
                       [r_br[c][cbi], r_mean], [r_zc[k]])
                    tt("dve", zc[k][:, 0:n], zc[k][:, 0:n], rstd[:, 0:n], ALU.mult, [r_zc[k], r_rstd], [r_zc[k]])
                    act(brT[:, c, c0:c0 + n], zc[k][:, 0:n], AF.Silu, [r_zc[k], r_colp], [r_br[c][cbi]],
                        bias=bcol(C_CLB + c), scale=bcol(C_CLG + c))
            n_cp, Pc, r_Pc = RG.acquire(4)
            gate_pass(1, Pc, r_Pc, bias_cols=C_CPWB)
            RG.release(n_cp)

            AR.reset()
            n_wq, Wq, r_Wq = RG.acquire(8)
            qb = [AR.alloc([128, 512], BF16) for _ in range(2)]
            eb = [AR.alloc([128, 2, 512], BF16) for _ in range(2)]
            rden = [AR.alloc([128, 512], F32) for _ in range(2)]
            r_qb = [R(), R()]
            r_eb = [R(), R()]
            r_rden = [R(), R()]
            it = 0
            for hd in range(4):
                for cbi, (c0, n) in enumerate(CBS):
                    k = it % 2
                    it += 1
                    b = next_bank()
                    for kc in range(8):
                        mm(bank(b)[:, 0:n], Wq[:, kc, hd * 128:(hd + 1) * 128], xT[:, kc, c0:c0 + n],
                           kc == 0, kc == 7, [r_Wq] + xT_regs(cbi), [r_bank[b]])
                    act(qb[k][:, 0:n], bank(b)[:, 0:n], AF.Identity, [r_bank[b], r_colp], [r_qb[k]],
                        bias=bcol(C_BIN + 12 + hd))
                    for mc in range(2):
                        bs = next_bank()
                        mm(bank(bs)[:, 0:n], kT[:, hd, mc * 128:(mc + 1) * 128], qb[k][:, 0:n], True, True,
                           [r_kT, r_qb[k]], [r_bank[bs]])
                        act(eb[k][:, mc, 0:n], bank(bs)[:, 0:n], AF.Exp, [r_bank[bs]], [r_eb[k]], scale=ATT_SCALE)
                    bo = next_bank()
                    for mc in range(2):
                        mm(bank(bo)[:, 0:n], vv[:, mc, hd * 128:(hd + 1) * 128], eb[k][:, mc, 0:n],
                           mc == 0, mc == 1, [r_vv, r_eb[k]], [r_bank[bo]])
                    bd = next_bank()
                    for mc in range(2):
                        mm(bank(bd)[:, 0:n], ones_bf, eb[k][:, mc, 0:n], mc == 0, mc == 1,
                           [r_cbf, r_eb[k]], [r_bank[bd]])
                    act(rden[k][:, 0:n], bank(bd)[:, 0:n], AF.Ln, [r_bank[bd]], [r_rden[k]])
                    act(rden[k][:, 0:n], rden[k][:, 0:n], AF.Exp, [r_rden[k]], [r_rden[k]], scale=-1.0)
                    tt("dve", brT[:, hd, c0:c0 + n], bank(bo)[:, 0:n], rden[k][:, 0:n], ALU.mult,
                       [r_bank[bo], r_rden[k]], [r_br[hd][cbi]])
            RG.release(n_wq)
            n_ao, Pa, r_Pa = RG.acquire(4)
            gate_pass(2, Pa, r_Pa)
            RG.release(n_ao)

            AR.reset()
            n_o0, Wo0, r_Wo0 = RG.acquire(8)
            n_o1, Wo1, r_Wo1 = RG.acquire(8)
            Wo = [(Wo0, r_Wo0), (Wo1, r_Wo1)]
            xr = [AR.alloc([128, D], F32) for _ in range(2)]
            zt = [AR.alloc([128, D], F32) for _ in range(2)]
            x1b = [AR.alloc([128, D], BF16) for _ in range(2)]
            x1T = AR.alloc([128, 8, 128], F32)
            lnt = [ln_tmp(), ln_tmp()]
            lg = AR.alloc([128, NE], F32)
            m8 = AR.alloc([128, 8], F32)
            nm = AR.alloc([128, 1], F32)
            Af = AR.alloc([128, NE], F32)
            ex = AR.alloc([128, NE], F32)
            ssum = AR.alloc([128, 1], F32)
            Abf = AR.alloc([128, NE], BF16)
            Asum = [AR.alloc([128, NE], BF16) for _ in range(2)]
            key = AR.alloc([128, NE], F32)
            k8 = AR.alloc([128, 8], F32)
            junk = AR.alloc([128, NE], F32)
            r_xr = [R(), R()]
            r_zt = [R(), R()]
            r_x1b = [R(), R()]
            r_x1T, r_lg, r_m8, r_nm, r_Af, r_ex, r_ss, r_Abf, r_key, r_k8, r_junk = (R() for _ in range(11))
            r_As = [R(), R()]
            scat_ops = []
            new_res = {}
            lg_all = AR.alloc([128, NT, NE], F32)
            r_lga = [R() for _ in range(NT)]

            x1b3 = [x1b[0], x1b[1], AR.alloc([128, D], BF16), AR.alloc([128, D], BF16)]
            r_x1b3 = [r_x1b[0], r_x1b[1], R(), R()]
            zt3 = [zt[0], zt[1], AR.alloc([128, D], F32)]
            r_zt3 = [r_zt[0], r_zt[1], R()]
            lnt3 = [lnt[0], lnt[1], ln_tmp()]

            def l1_A(i):
                k = i % 2
                z3 = i % 3
                src_t, src_op = res_src[i]
                dma("sp", xr[k], src_t[i * 128:(i + 1) * 128, :], (), [r_xr[k]], deps=[src_op])
                b = 2 * k
                for half in range(2):
                    W_, r_W = Wo[half]
                    for kc in range(8):
                        mm(bank(b + half), mergedT[:, kc, i * 128:(i + 1) * 128], W_[:, kc, :],
                           kc == 0, False, [r_mg[i], r_W], [r_bank[b + half]])
                    mm(bank(b + half), onesrow_f, rowsm[0:1, half * 512:(half + 1) * 512], False, True,
                       [r_cst, r_rowsm], [r_bank[b + half]])
                for half in range(2):
                    stt("dve", zt3[z3][:, half * 512:(half + 1) * 512], xr[k][:, half * 512:(half + 1) * 512], ALPHA,
                        bank(b + half), ALU.mult, ALU.add, [r_xr[k], r_bank[b + half]], [r_zt3[z3]])
                ln_stats(zt3[z3], r_zt3[z3], lnt3[z3])

            def l1_B1(i):
                z3 = i % 3
                ln_norm(zt3[z3], r_zt3[z3], lnt3[z3])

            def l1_Bg(i):
                z3 = i % 3
                k4 = i % 4
                ln_affine(zt3[z3], r_zt3[z3], lnbc[:, 0:D], lnbc[:, D:2 * D], r_lnbc)
                st_op = dma("sp", xres[i * 128:(i + 1) * 128, :], zt3[z3], [r_zt3[z3]], ())
                new_res[i] = (xres, st_op)
                cp("act", x1b3[k4], zt3[z3], [r_zt3[z3]], [r_x1b3[k4]])

            def l1_Bt(i):
                z3 = i % 3
                b = 4
                for kc in range(8):
                    bb = b + kc // 4
                    tr(bank(bb)[:, (kc % 4) * 128:(kc % 4 + 1) * 128], zt3[z3][:, kc * 128:(kc + 1) * 128], ident_f,
                       [r_zt3[z3], r_cst], [r_bank[bb]])
                cp("act", x1T[:, 0:4, :], bank(b).rearrange("p (a b) -> p a b", a=4), [r_bank[b]], [r_x1T])
                cp("act", x1T[:, 4:8, :], bank(b + 1).rearrange("p (a b) -> p a b", a=4), [r_bank[b + 1]], [r_x1T])
                c0_ = (i % 4) * NE
                for kc in range(8):
                    mm(bank(6)[:, c0_:c0_ + NE], x1T[:, kc, :], wr[:, kc, :], kc == 0, False,
                       [r_x1T, r_wr], [r_bank[6]])
                mm(bank(6)[:, c0_:c0_ + NE], onesrow_f, rowsm[0:1, D:D + NE], False, True,
                   [r_cst, r_rowsm], [r_bank[6]])

            def l1_C(i):
                k = i % 2
                c0_ = (i % 4) * NE
                lg = lg_all[:, i, :]
                r_lg = r_lga[i]
                cp("dve", lg, bank(6)[:, c0_:c0_ + NE], [r_bank[6]], [r_lg])
                S.op("dve", lambda e: e.max(out=m8, in_=lg), [r_lg], [r_m8])
                ts("dve", Af, lg, m8[:, 3:4], None, ALU.is_ge, None, [r_lg, r_m8], [r_Af])
                if i == 0:
                    ts("dve", Af, Af, vtok, None, ALU.mult, None, [r_Af, r_cst], [r_Af])
                ts("dve", nm, m8[:, 0:1], -1.0, None, ALU.mult, None, [r_m8], [r_nm])
                act(ex, lg, AF.Exp, [r_lg, r_nm], [r_ex], bias=nm[:, 0:1])
                stt("dve", ex, Af, 1.0, ex, ALU.mult, ALU.mult, [r_Af, r_ex], [r_ex, r_ss], accum_out=ssum)
                ts("dve", ssum, ssum, 1e-30, None, ALU.max, None, [r_ss], [r_ss])
                S.op("dve", lambda e: e.reciprocal(out=ssum, in_=ssum), [r_ss], [r_ss])
                ts("dve", G_all[:, i, :], ex, ssum[:, 0:1], None, ALU.mult, None, [r_ex, r_ss], [r_G[i]])
                cp("dve", Abf, Af, [r_Af], [r_Abf])
                if i > 0:
                    tt("dve", Asum[k], Asum[(i - 1) % 2], Abf, ALU.add, [r_As[(i - 1) % 2], r_Abf], [r_As[k]])
                else:
                    cp("dve", Asum[k], Abf, [r_Abf], [r_As[k]])

            def l1_D(i):
                k3 = i % 4
                c0_ = (i % 4) * NE
                pp = bank(7)[:, c0_:c0_ + NE]
                mm(pp, tri_bf, Abf, True, i == 0, [r_cbf, r_Abf], [r_bank[7]])
                if i > 0:
                    mm(pp, ones_bf, Asum[(i - 1) % 2], False, True,
                       [r_cbf, r_As[(i - 1) % 2]], [r_bank[7]])
                stt("dve", key, pp, float(CAP - 1), base1, ALU.min, ALU.add,
                    [r_bank[7], r_cst], [r_key])
                tt("dve", key, key, Af, ALU.mult, [r_key, r_Af], [r_key])
                S.op("dve", lambda e: e.max(out=k8, in_=key), [r_key], [r_k8])
                cp("dve", idx_all[:, i * 4:(i + 1) * 4], k8[:, 0:4], [r_k8], [r_idx[i]])
                for kk in range(4):
                    stt("dve", junk, key, k8[:, kk:kk + 1], G_all[:, i, :], ALU.is_equal, ALU.mult,
                        [r_key, r_k8, r_G[i]], [r_junk, r_gate[i]],
                        accum_out=gate_all[:, i * 4 + kk:i * 4 + kk + 1])
                for kk in range(4):
                    o = S.dma("pool", lambda e, s_=x1b3[k3], ix=idx_all[:, i * 4 + kk:i * 4 + kk + 1]:
                              e.indirect_dma_start(out=xd[:, :],
                                                   out_offset=bass.IndirectOffsetOnAxis(ap=ix, axis=0),
                                                   in_=s_, in_offset=None,
                                                   bounds_check=regs["bc"], oob_is_err=False),
                              [r_x1b3[k3], r_idx[i]], ())
                    scat_ops.append(o)

            for step in range(NT + 4):
                if 0 <= step - 1 < NT:
                    l1_B1(step - 1)
                if step < NT:
                    l1_A(step)
                if 0 <= step - 1 < NT:
                    l1_Bg(step - 1)
                if 0 <= step - 2 < NT:
                    l1_Bt(step - 2)
                if 0 <= step - 4 < NT:
                    l1_D(step - 4)
                if 0 <= step - 3 < NT:
                    l1_C(step - 3)
            RG.release(n_o0)
            RG.release(n_o1)
            res_src = new_res

            AR.reset()
            RG.set_alias(True)
            brflat = brT[:, :, :].rearrange("p a b -> p (a b)")
            xg = [brflat[:, q_ * 3 * D:(q_ + 1) * 3 * D].rearrange("p (a b) -> p a b", a=3) for q_ in range(2)]
            xgT = [AR.alloc([128, 8, CAP], BF16) for _ in range(2)]
            actT = [AR.alloc([128, 8, CAP], BF16) for _ in range(2)]
            gc = [AR.alloc([128, CAP], F32) for _ in range(2)]
            sgm = [AR.alloc([128, CAP], F32) for _ in range(2)]
            t1 = [AR.alloc([128, CAP], F32) for _ in range(2)]
            ybf = [AR.alloc([128, D], BF16) for _ in range(2)]
            r_xg = [R(), R()]
            r_xgT = [R(), R()]
            r_actT = [R(), R()]
            r_gc = [R(), R()]
            r_sgm = [R(), R()]
            r_t1 = [R(), R()]
            r_ybf = [R(), R()]
            ys_ops = []
            cnt_ = {"it": 0, "ity": 0}

            def load_xg(e_):
                p = e_ % 2
                r0 = 1 + e_ * CAP
                dma("sp", xg[p], xd[r0:r0 + CAP, :].rearrange("(t p) d -> p t d", p=128), (), [r_xg[p]],
                    deps=scat_ops if e_ < 2 else ())

            def transposes(e_):
                p = e_ % 2
                for kp in range(4):
                    b = next_bank()
                    pb = bank_bf(b)
                    for kc2 in range(2):
                        kc = kp * 2 + kc2
                        for ti in range(3):
                            tr(pb[:, kc2 * CAP + ti * 128:kc2 * CAP + (ti + 1) * 128],
                               xg[p][:, ti, kc * 128:(kc + 1) * 128], ident_bf, [r_xg[p], r_cbf], [r_bank[b]])
                    cp("act" if kp % 2 else "dve", xgT[p][:, kp * 2:kp * 2 + 2, :],
                       pb[:, 0:2 * CAP].rearrange("p (a b) -> p a b", a=2), [r_bank[b]], [r_xgT[p]])

            def up(e_, hf):
                p = e_ % 2
                n_ug, Ug, r_Ug = RG.acquire(8)
                n_ul, Ul, r_Ul = RG.acquire(8)
                for jj in range(4):
                    j = hf * 4 + jj
                    bg = next_bank()
                    for kc in range(8):
                        mm(bank(bg)[:, 0:CAP], Ug[:, kc, jj * 128:(jj + 1) * 128], xgT[p][:, kc, :],
                           kc == 0, kc == 7, [r_Ug, r_xgT[p]], [r_bank[bg]])
                    bl = next_bank()
                    for kc in range(8):
                        mm(bank(bl)[:, 0:CAP], Ul[:, kc, jj * 128:(jj + 1) * 128], xgT[p][:, kc, :],
                           kc == 0, kc == 7, [r_Ul, r_xgT[p]], [r_bank[bl]])
                    k = cnt_["it"] % 2
                    cnt_["it"] += 1
                    ts("dve", gc[k], bank(bg)[:, 0:CAP], bcol(C_UPB + e_ * 16 + j), 7.0, ALU.add, ALU.min,
                       [r_bank[bg], r_colp], [r_gc[k]])
                    act(sgm[k], gc[k], AF.Sigmoid, [r_gc[k]], [r_sgm[k]], scale=1.702)
                    act(t1[k], bank(bl)[:, 0:CAP], AF.Identity, [r_bank[bl], r_upb1], [r_t1[k]],
                        bias=upb1[:, e_ * 8 + j:e_ * 8 + j + 1])
                    ts("dve", t1[k], t1[k], -6.0, 8.0, ALU.max, ALU.min, [r_t1[k]], [r_t1[k]])
                    tt("dve", gc[k], gc[k], sgm[k], ALU.mult, [r_gc[k], r_sgm[k]], [r_gc[k]])
                    tt("dve", actT[p][:, j, :], t1[k], gc[k], ALU.mult, [r_t1[k], r_gc[k]], [r_actT[p]])
                RG.release(n_ug)
                RG.release(n_ul)

            def down(e_):
                p = e_ % 2
                r0 = 1 + e_ * CAP
                n_d0, D0, r_D0 = RG.acquire(8)
                n_d1, D1, r_D1 = RG.acquire(8)
                Dn = [(D0, r_D0), (D1, r_D1)]
                for ti in range(3):
                    b = next_bank2()
                    for half in range(2):
                        W_, r_W = Dn[half]
                        for kc in range(8):
                            mm(bank(b + half), actT[p][:, kc, ti * 128:(ti + 1) * 128], W_[:, kc, :],
                               kc == 0, kc == 7, [r_actT[p], r_W], [r_bank[b + half]])
                    k = cnt_["ity"] % 2
                    cnt_["ity"] += 1
                    cp("act", ybf[k][:, 0:512], bank(b), [r_bank[b]], [r_ybf[k]])
                    cp("dve", ybf[k][:, 512:1024], bank(b + 1), [r_bank[b + 1]], [r_ybf[k]])
                    o = dma("sp", ys[r0 + ti * 128:r0 + (ti + 1) * 128, :], ybf[k], [r_ybf[k]], ())
                    ys_ops.append(o)
                RG.release(n_d0)
                RG.release(n_d1)

            load_xg(0)
            load_xg(1)
            transposes(0)
            up(0, 0)
            up(0, 1)
            for e_ in range(NE):
                if e_ + 1 < NE:
                    if e_ + 2 < NE:
                        load_xg(e_ + 2)
                    transposes(e_ + 1)
                    up(e_ + 1, 0)
                down(e_)
                if e_ + 1 < NE:
                    up(e_ + 1, 1)

            RG.set_alias(False)
            AR.reset()
            yk = [AR.alloc([128, 4, D], BF16) for _ in range(2)]
            xr = [AR.alloc([128, D], F32) for _ in range(2)]
            zt = [AR.alloc([128, D], F32) for _ in range(2)]
            x2b = [AR.alloc([128, D], BF16) for _ in range(2)]
            GT = AR.alloc([NE, 128], F32)
            lnt = [ln_tmp(), ln_tmp()]
            r_yk = [R(), R()]
            r_xr = [R(), R()]
            r_zt = [R(), R()]
            r_x2b = [R(), R()]
            r_GT = R()
            new_res = {}

            def l2_G(i):
                k = i % 2
                for kk in range(4):
                    S.dma("pool", lambda e, d_=yk[k][:, kk, :], ix=idx_all[:, i * 4 + kk:i * 4 + kk + 1]:
                          e.indirect_dma_start(out=d_, out_offset=None, in_=ys[:, :],
                                               in_offset=bass.IndirectOffsetOnAxis(ap=ix, axis=0),
                                               bounds_check=regs["bc"], oob_is_err=False),
                          [r_idx[i]], [r_yk[k]], deps=ys_ops if (i == 0 and kk == 0) else ())
                src_t, src_op = res_src[i]
                dma("sp", xr[k], src_t[i * 128:(i + 1) * 128, :], (), [r_xr[k]], deps=[src_op])

            def l2_A0(i):
                k = i % 2
                tr(bank(4)[0:NE, 0:128], G_all[:, i, :], ident_f, [r_G[i], r_cst], [r_bank[4]])
                cp("act", GT, bank(4)[0:NE, 0:128], [r_bank[4]], [r_GT])
                b = 2 * k
                for half in range(2):
                    mm(bank(b + half), GT, bdown[:, half * 512:(half + 1) * 512], True, True,
                       [r_GT, r_bdown], [r_bank[b + half]])

            def l2_A(i):
                k = i % 2
                b = 2 * k
                for half in range(2):
                    stt("dve", zt[k][:, half * 512:(half + 1) * 512], xr[k][:, half * 512:(half + 1) * 512], ALPHA,
                        bank(b + half), ALU.mult, ALU.add, [r_xr[k], r_bank[b + half]], [r_zt[k]])
                for kk in range(4):
                    stt("dve", zt[k], yk[k][:, kk, :], gate_all[:, i * 4 + kk:i * 4 + kk + 1], zt[k],
                        ALU.mult, ALU.add, [r_yk[k], r_gate[i], r_zt[k]], [r_zt[k]])
                ln_stats(zt[k], r_zt[k], lnt[k])

            def l2_B1(i):
                k = i % 2
                ln_norm(zt[k], r_zt[k], lnt[k])

            def l2_B2(i):
                k = i % 2
                ln_affine(zt[k], r_zt[k], lnbc[:, 2 * D:3 * D], lnbc[:, 3 * D:4 * D], r_lnbc)
                if last:
                    if i > 0:
                        o = dma("sp", out_d[(i - 1) * 128:i * 128, :], zt[k], [r_zt[k]], ())
                        final_ops.append(o)
                else:
                    st_op = dma("sp", xres[i * 128:(i + 1) * 128, :], zt[k], [r_zt[k]], ())
                    new_res[i] = (xres, st_op)
                    cp("act", x2b[k], zt[k], [r_zt[k]], [r_x2b[k]])
                    b = 5 + k
                    pb = bank_bf(b)
                    for kc in range(8):
                        tr(pb[:, kc * 128:(kc + 1) * 128], x2b[k][:, kc * 128:(kc + 1) * 128], ident_bf,
                           [r_x2b[k], r_cbf], [r_bank[b]])
                    cp("act", xT[:, :, i * 128:(i + 1) * 128], pb.rearrange("p (a b) -> p a b", a=8),
                       [r_bank[b]], [r_xT[i]])

            l2_G(0)
            for step in range(NT + 1):
                if step + 1 < NT:
                    l2_G(step + 1)
                if step < NT:
                    l2_A0(step)
                if 0 <= step - 1 < NT:
                    l2_B1(step - 1)
                if step < NT:
                    l2_A(step)
                if 0 <= step - 1 < NT:
                    l2_B2(step - 1)
            res_src = new_res

        assert RG.next_acq == len(plan), (RG.next_acq, len(plan))
        S.finalize(final_ops)
    return nc


def _consts(half):
    c = np.zeros((128, NCST), np.float32)
    c[:, K_ID:K_ID + 128] = np.eye(128, dtype=np.float32)
    tp = np.arange(128)
    c[:, K_TRI:K_TRI + 128] = (tp[:, None] < tp[None, :]).astype(np.float32)
    c[:, K_ONE:K_ONE + 128] = 1.0
    valid = 0.0 if half == 0 else 1.0
    c[:, K_VM:K_VM + 128] = valid
    c[:, K_VT] = valid
    for g, w in enumerate((2, 4, 8, 16)):
        for t in range(16):
            cnt = min(t + 1, w) if half == 0 else w
            c[:, K_PF + g * 16 + t] = w / cnt
    c[:, K_B1:K_B1 + 32] = (1 + np.arange(NE) * CAP)[None, :].astype(np.float32)
    return c


def _layer_params(inp, ls):
    f = lambda a: np.ascontiguousarray(np.asarray(a, dtype=np.float32))
    colp = np.zeros((len(ls), 128, NCOLP), np.float32)
    for n, l in enumerate(ls):
        colp[n, :, C_BIN:C_BIN + 40] = inp["b_in"][l].reshape(40, 128).T
        colp[n, :, C_PSC:C_PSC + 4] = inp["pool_scale"][l].reshape(4, 128).T
        colp[n, :, C_CDW:C_CDW + 124] = inp["conv_dw"][l].T.reshape(4, 128, 31).transpose(1, 0, 2).reshape(128, 124)
        colp[n, :, C_CDWB:C_CDWB + 4] = inp["conv_dw_b"][l].reshape(4, 128).T
        colp[n, :, C_CLG:C_CLG + 4] = inp["conv_ln_g"][l].reshape(4, 128).T
        colp[n, :, C_CLB:C_CLB + 4] = inp["conv_ln_b"][l].reshape(4, 128).T
        colp[n, :, C_CPWB:C_CPWB + 8] = inp["conv_pw_b"][l].reshape(8, 128).T
        colp[n, :, C_UPB:C_UPB + 512] = inp["exp_up_b"][l].reshape(NE, 16, 128).transpose(2, 0, 1).reshape(128, 512)
    rowbc = np.stack([np.concatenate([inp["ln1_g"][l], inp["ln1_b"][l], inp["ln2_g"][l], inp["ln2_b"][l]])[None, :]
                      for l in ls]).astype(np.float32)
    rowsm = np.stack([np.concatenate([inp["b_out"][l], inp["router_b"][l]])[None, :] for l in ls]).astype(np.float32)
    sl = slice(ls[0], ls[-1] + 1)
    d = {
        "w_in": f(inp["w_in"][sl]), "pool_w": f(inp["pool_w"][sl]), "pool_proj": f(inp["pool_proj"][sl]),
        "conv_pw": f(inp["conv_pw"][sl]), "attn_o": f(inp["attn_o"][sl]), "w_kv": f(inp["w_kv"][sl]),
        "w_out": f(inp["w_out"][sl]), "exp_up": f(inp["exp_up"][sl]), "exp_down": f(inp["exp_down"][sl]),
        "router_w": f(inp["router_w"][sl]), "exp_down_b": f(inp["exp_down_b"][sl]),
        "colp": colp, "rowbc": rowbc, "rowsm": rowsm,
    }
    return d


def _x_shards(xfull):
    outs = []
    for c in range(8):
        b, half = c // 2, c % 2
        xs = np.zeros((T, D), np.float32)
        s0 = half * 2048
        xs[128:] = xfull[b, s0:s0 + 2048]
        if half == 1:
            xs[:128] = xfull[b, s0 - 128:s0]
        outs.append(xs)
    return outs


_PROG_CACHE = {}


def _get_prog(NL):
    if NL not in _PROG_CACHE:
        _PROG_CACHE[NL] = build_program(NL)
    return _PROG_CACHE[NL]


LAYERS_PER_LAUNCH = 4


def kernel(**inp):
    inp = {k: np.asarray(v) for k, v in inp.items()}
    x = np.asarray(inp["x"], np.float32)
    memln = np.concatenate([inp["mem_ln_g"], inp["mem_ln_b"]])[None, :].astype(np.float32)
    NL = LAYERS_PER_LAUNCH
    nc = _get_prog(NL)
    csts = [_consts(c % 2) for c in range(8)]
    for l0 in range(0, DEPTH, NL):
        lp = _layer_params(inp, list(range(l0, l0 + NL)))
        xs = _x_shards(x)
        in_maps = []
        for c in range(8):
            m = dict(lp)
            m["x_in"] = xs[c]
            m["mem"] = np.ascontiguousarray(inp["mem"][c // 2], dtype=np.float32)
            m["memln"] = memln
            m["cst"] = csts[c]
            in_maps.append(m)
        res = run_bass_kernel_spmd(nc, in_maps, core_ids=list(range(8)))
        xn = np.empty_like(x)
        for c in range(8):
            b, half = c // 2, c % 2
            xn[b, half * 2048:(half + 1) * 2048] = np.asarray(res.results[c]["out"], np.float32)
        x = xn
    return x
```

```python
import contextlib
import numpy as np
import ml_dtypes
import concourse.bass as bass
import concourse.mybir as mybir
from concourse.bass_utils import run_bass_kernel_spmd

F32 = mybir.dt.float32
BF16 = mybir.dt.bfloat16
I32 = mybir.dt.int32
AF = mybir.ActivationFunctionType
ALU = mybir.AluOpType

D = 1024
NT = 17
T = NT * 128
SEQ = 4096
DEPTH = 4
NE = 32
CAP = 384
NSLOT = 1 + NE * CAP
IN_COLS = 5120
MEM = 256
ALPHA = (2.0 * DEPTH) ** 0.25
LN_EPS = 1e-5
CBS = [(0, 512), (512, 512), (1024, 512), (1536, 512), (2048, 128)]
NW = 4
ATT_SCALE = 128 ** -0.5

C_BIN = 0
C_PSC = 40
C_CDW = 44
C_CDWB = 168
C_CLG = 172
C_CLB = 176
C_CPWB = 180
C_UPB = 188
NCOLP = 700
K_ID = 0
K_TRI = 128
K_ONE = 256
K_VM = 384
K_VT = 512
K_PF = 513
K_B1 = 577
NCST = 609


ENGS = ("pe", "act", "dve", "pool", "sp")


class Reg:
    __slots__ = ("w", "r", "rd")

    def __init__(self):
        self.w = None
        self.r = {}
        self.rd = []


class Op:
    __slots__ = ("eng", "fn", "deps", "is_dma", "sem", "val", "needed", "epoch")


class Sched:
    def __init__(self, nc, n_dma_sems=20):
        self.nc = nc
        self.q = {e: [] for e in ENGS}
        self.n_dma_sems = n_dma_sems
        self.epoch = 0
        self.pending = {}
        self.last_compute = {}
        self.dma_since_barrier = []
        self.prologue = {}

    def _mk(self, eng, fn, reads, writes, deps, is_dma, bar=True):
        o = Op()
        o.eng = eng
        o.fn = fn
        o.is_dma = is_dma
        o.sem = None
        o.val = None
        o.needed = is_dma
        o.epoch = self.epoch
        d = [x for x in deps if x is not None]
        for r in reads:
            if r.w is not None:
                d.append(r.w)
        for w in writes:
            if w.w is not None:
                d.append(w.w)
            d.extend(w.r.values())
            d.extend(w.rd)
        pb = self.pending.pop(eng, None)
        if pb:
            d.extend(pb)
        o.deps = d
        for r in reads:
            if is_dma:
                r.rd.append(o)
            else:
                r.r[eng] = o
        for w in writes:
            w.w = o
            w.r = {}
            w.rd = []
        self.q[eng].append(o)
        if is_dma:
            if bar:
                self.dma_since_barrier.append(o)
        else:
            self.last_compute[eng] = o
        return o

    def op(self, eng, fn, reads=(), writes=(), deps=()):
        return self._mk(eng, fn, reads, writes, deps, False)

    def dma(self, eng, fn, reads=(), writes=(), deps=(), bar=True):
        return self._mk(eng, fn, reads, writes, deps, True, bar)

    def barrier(self):
        lst = list(self.last_compute.values()) + list(self.dma_since_barrier)
        self.dma_since_barrier = []
        for e in ENGS:
            self.pending[e] = list(lst) + self.pending.get(e, [])

    def finalize(self, final_ops):
        nc = self.nc
        for e in ENGS:
            for o in self.q[e]:
                for d in o.deps:
                    d.needed = True
        for o in final_ops:
            o.needed = True
        n_epochs = self.epoch + 1
        with contextlib.ExitStack() as stack:
            sems = {}
            for e in ENGS:
                for ep in range(n_epochs):
                    sems[(e, ep)] = stack.enter_context(nc.semaphore(f"s_{e}{ep}"))
            dsems = {}
            for e in ("sp", "act", "pool"):
                for i in range(self.n_dma_sems):
                    dsems[(e, i)] = stack.enter_context(nc.semaphore(f"d_{e}{i}"))
            cnt = {}
            slot_next = {e: 0 for e in ENGS}
            slot_cnt = {}
            slot_last = {}
            for e in ENGS:
                for o in self.q[e]:
                    if o.is_dma:
                        s = slot_next[e]
                        slot_next[e] = (s + 1) % self.n_dma_sems
                        key = (e, s)
                        prev = slot_last.get(key)
                        if prev is not None:
                            o.deps.append(prev)
                        slot_cnt[key] = slot_cnt.get(key, 0) + 16
                        slot_last[key] = o
                        o.sem = dsems[key]
                        o.val = slot_cnt[key]
                    elif o.needed:
                        k = (e, o.epoch)
                        cnt[k] = cnt.get(k, 0) + 1
                        o.sem = sems[k]
                        o.val = cnt[k]
            self.max_counts = cnt
            block = stack.enter_context(nc.Block())
            engmap = {"pe": block.tensor, "act": block.scalar, "dve": block.vector,
                      "pool": block.gpsimd, "sp": block.sync}

            def make(e):
                def body(eng):
                    seen = {}
                    for pf in self.prologue.get(e, ()):
                        pf(eng)
                    for o in self.q[e]:
                        for d in o.deps:
                            if (not d.is_dma) and d.eng == e and e == "pe":
                                continue
                            k = id(d.sem)
                            if seen.get(k, 0) >= d.val:
                                continue
                            seen[k] = d.val
                            eng.wait_ge(d.sem, d.val)
                        inst = o.fn(eng)
                        if o.sem is not None:
                            inst.then_inc(o.sem, 16 if o.is_dma else 1)
                    if e == "sp":
                        for o in final_ops:
                            if seen.get(id(o.sem), 0) >= o.val:
                                continue
                            seen[id(o.sem)] = o.val
                            eng.wait_ge(o.sem, o.val)
                return body

            for e in ENGS:
                engmap[e](make(e))


def build_program(NL, first_layer_is_input=True):
    nc = bass.Bass("TRN2", target_bir_lowering=False)
    dt_in = lambda n, s, d=F32: nc.dram_tensor(n, s, d, kind="ExternalInput")
    x_in = dt_in("x_in", [T, D])
    mem_in = dt_in("mem", [MEM, D])
    memln = dt_in("memln", [1, 2 * D])
    cst_in = dt_in("cst", [128, NCST])
    w_in = dt_in("w_in", [NL, D, IN_COLS])
    pool_w = dt_in("pool_w", [NL, 4, 128, 128])
    pool_proj = dt_in("pool_proj", [NL, 512, D])
    conv_pw = dt_in("conv_pw", [NL, 512, D])
    attn_o = dt_in("attn_o", [NL, 512, D])
    w_kv = dt_in("w_kv", [NL, D, D])
    w_out = dt_in("w_out", [NL, D, D])
    exp_up = dt_in("exp_up", [NL, NE, D, 2 * D])
    exp_down = dt_in("exp_down", [NL, NE, D, D])
    router_w = dt_in("router_w", [NL, D, NE])
    exp_down_b = dt_in("exp_down_b", [NL, NE, D])
    colp_in = dt_in("colp", [NL, 128, NCOLP])
    rowbc_in = dt_in("rowbc", [NL, 1, 4 * D])
    rowsm_in = dt_in("rowsm", [NL, 1, D + NE])
    out_d = nc.dram_tensor("out", [T - 128, D], F32, kind="ExternalOutput")
    xres = nc.dram_tensor("xres", [T, D], F32, kind="Internal")
    xd = nc.dram_tensor("xd", [NSLOT, D], BF16, kind="Internal")
    ys = nc.dram_tensor("ys", [NSLOT, D], BF16, kind="Internal")

    with contextlib.ExitStack() as st:
        sb = lambda n, s, d: st.enter_context(nc.sbuf_tensor("sb_" + n, s, d))
        S = Sched(nc)

        xT = sb("xT", [128, 8, T], BF16)
        mergedT = sb("mergedT", [128, 8, T], BF16)
        brT = sb("brT", [128, 4, T], BF16)
        ring = [sb(f"ring{i}", [128, 4096], BF16) for i in range(NW)]
        cst = sb("cst", [128, NCST], F32)
        cbf = sb("cbf", [128, 384], BF16)
        colp = sb("colp", [128, NCOLP], F32)
        upb1 = sb("upb1", [128, NE * 8], F32)
        lnbc = sb("lnbc", [128, 4 * D], F32)
        rowsm = sb("rowsm", [1, D + NE], F32)
        wr = sb("wr", [128, 8, NE], F32)
        bdown = sb("bdown", [NE, D], F32)
        memT = sb("memT", [128, 8, MEM], BF16)
        kT = sb("kT", [128, 4, MEM], BF16)
        vv = sb("vv", [128, 2, 512], BF16)
        poolw = sb("poolw", [128, 4, 128], BF16)
        idx_all = sb("idx_all", [128, NT * 4], I32)
        gate_all = sb("gate_all", [128, NT * 4], F32)
        G_all = sb("G_all", [128, NT, NE], F32)
        ARENA_BF = 24064
        arena = sb("arena", [128, ARENA_BF], BF16)
        ps = [st.enter_context(nc.psum_tensor(f"ps{i}", [128, 1024], F32)) for i in range(4)]

        epsc = sb("epsc", [128, 1], F32)
        r_eps = Reg()
        ident_bf = cbf[:, 0:128]
        tri_bf = cbf[:, 128:256]
        ones_bf = cbf[:, 256:384]
        ident_f = cst[:, K_ID:K_ID + 128]
        onesrow_f = cst[0:1, K_ONE:K_ONE + 128]
        vmask = cst[:, K_VM:K_VM + 128]
        vtok = cst[:, K_VT:K_VT + 1]
        base1 = cst[:, K_B1:K_B1 + 32]

        R = lambda: Reg()
        r_xT = [R() for _ in range(NT)]
        r_mg = [R() for _ in range(NT)]
        r_br = [[R() for _ in CBS] for _ in range(4)]
        r_ring = [R() for _ in range(NW)]
        r_cst, r_cbf, r_colp, r_upb1, r_lnbc, r_rowsm, r_wr, r_bdown = (R() for _ in range(8))
        r_memT, r_kT, r_vv, r_poolw = R(), R(), R(), R()
        r_idx = [R() for _ in range(NT)]
        r_gate = [R() for _ in range(NT)]
        r_G = [R() for _ in range(NT)]
        r_bank = [R() for _ in range(8)]

        def bank(b):
            return ps[b // 2][:, (b % 2) * 512:(b % 2) * 512 + 512]

        def bank_bf(b):
            return ps[b // 2][:, (b % 2) * 512:(b % 2) * 512 + 512].bitcast(BF16)

        bank_rr = [0]

        def next_bank():
            b = bank_rr[0]
            bank_rr[0] = (b + 1) % 8
            return b

        def next_bank2():
            b = bank_rr[0]
            if b % 2:
                b = (b + 1) % 8
            bank_rr[0] = (b + 2) % 8
            return b

        class Arena:
            def __init__(self):
                self.off = 0

            def reset(self):
                S.barrier()
                self.off = 0

            def alloc(self, shape, dtype):
                n = 1
                for s_ in shape[1:]:
                    n *= s_
                nb = n * (2 if dtype == BF16 else 4)
                self.off = (self.off + 63) // 64 * 64
                o_bf = self.off // 2
                self.off += nb
                assert self.off <= ARENA_BF * 2, f"arena overflow {self.off}"
                v = arena[0:shape[0], o_bf:o_bf + nb // 2]
                if dtype != BF16:
                    v = v.bitcast(dtype)
                if len(shape) == 3:
                    v = v.rearrange("p (a b) -> p a b", a=shape[1])
                return v

        AR = Arena()
        regs = {}
        S.prologue["pool"] = [lambda e: regs.__setitem__("bc", e.to_reg(NSLOT - 1))]

        def mm(out, lhsT, rhs, start, stop, reads, writes):
            return S.op("pe", lambda e: e.matmul(out, lhsT=lhsT, rhs=rhs, start=start, stop=stop),
                        reads, writes)

        def tr(out, in_, ident, reads, writes):
            return S.op("pe", lambda e: e.transpose(out=out, in_=in_, identity=ident), reads, writes)

        def act(out, in_, func, reads, writes, bias=None, scale=None, eng="act"):
            kw = {}
            if bias is not None:
                kw["bias"] = bias
            if scale is not None:
                kw["scale"] = scale
            return S.op(eng, lambda e: e.activation(out=out, in_=in_, func=func, **kw), reads, writes)

        def ts(eng, out, in0, s1, s2, op0, op1, reads, writes, accum_out=None):
            kw = {}
            if op1 is not None:
                kw["op1"] = op1
            if accum_out is not None:
                kw["accum_out"] = accum_out
            return S.op(eng, lambda e: e.tensor_scalar(out=out, in0=in0, scalar1=s1, scalar2=s2,
                                                       op0=op0, **kw), reads, writes)

        def tt(eng, out, in0, in1, op, reads, writes):
            return S.op(eng, lambda e: e.tensor_tensor(out=out, in0=in0, in1=in1, op=op), reads, writes)

        def stt(eng, out, in0, scalar, in1, op0, op1, reads, writes, accum_out=None):
            kw = {}
            if accum_out is not None:
                kw["accum_out"] = accum_out
            return S.op(eng, lambda e: e.scalar_tensor_tensor(out=out, in0=in0, scalar=scalar, in1=in1,
                                                              op0=op0, op1=op1, **kw), reads, writes)

        def cp(eng, out, in_, reads, writes):
            if eng == "act":
                return S.op(eng, lambda e: e.activation(out=out, in_=in_, func=AF.Copy), reads, writes)
            return S.op(eng, lambda e: e.tensor_copy(out=out, in_=in_), reads, writes)

        def memset(eng, ap, val, writes):
            return S.op(eng, lambda e: e.memset(ap, val), (), writes)

        def dma(eng, out, in_, reads, writes, deps=(), bar=True):
            return S.dma(eng, lambda e: e.dma_start(out=out, in_=in_), reads, writes, deps, bar)

        memset("dve", epsc[:, :], LN_EPS, [r_eps])

        plan = []
        kinds = []

        def kcview(ap2d):
            return ap2d.rearrange("(kc p) f -> p kc f", p=128)

        for l in range(NL):
            plan.append(kcview(w_kv[l][:, 0:512]))
            plan.append(kcview(w_kv[l][:, 512:1024]))
            plan.append(kcview(w_in[l][:, 0:512]))
            plan.append(kcview(pool_proj[l]))
            plan.append(kcview(w_in[l][:, 2048:2560]))
            plan.append(kcview(w_in[l][:, 2560:3072]))
            plan.append(kcview(w_in[l][:, 512:1024]))
            plan.append(kcview(w_in[l][:, 1024:1536]))
            plan.append(kcview(conv_pw[l]))
            plan.append(kcview(w_in[l][:, 3072:3584]))
            plan.append(kcview(w_in[l][:, 3584:4096]))
            plan.append(kcview(w_in[l][:, 1536:2048]))
            plan.append(kcview(attn_o[l]))
            plan.append(kcview(w_in[l][:, 4096:4608]))
            plan.append(kcview(w_in[l][:, 4608:5120]))
            plan.append(kcview(w_out[l][:, 0:512]))
            plan.append(kcview(w_out[l][:, 512:1024]))
            kinds.extend(["m"] * 17)
            def _up(e, hf):
                kinds.extend(["e"] * 2)
                plan.append(kcview(exp_up[l, e][:, hf * 512:(hf + 1) * 512]))
                plan.append(kcview(exp_up[l, e][:, 1024 + hf * 512:1024 + (hf + 1) * 512]))

            def _down(e):
                kinds.extend(["e"] * 2)
                plan.append(kcview(exp_down[l, e][:, 0:512]))
                plan.append(kcview(exp_down[l, e][:, 512:1024]))

            _up(0, 0)
            _up(0, 1)
            for e in range(NE):
                if e + 1 < NE:
                    _up(e + 1, 0)
                _down(e)
                if e + 1 < NE:
                    _up(e + 1, 1)

        NWE = NW + 8
        xTflat = xT[:, :, :].rearrange("p a b -> p (a b)")
        mgflat = mergedT[:, :, :].rearrange("p a b -> p (a b)")
        ringv = [ring[i][:, :] for i in range(NW)]
        ringv += [xTflat[:, q_ * 4096:(q_ + 1) * 4096] for q_ in range(4)]
        ringv += [mgflat[:, q_ * 4096:(q_ + 1) * 4096] for q_ in range(4)]
        r_ring.extend(Reg() for _ in range(8))

        class Ring:
            def __init__(self):
                self.next_load = 0
                self.next_acq = 0
                self.released = [False] * len(plan)
                self.allow_alias = 0
                self.slot = []
                self.prev = []
                last = {}
                bc = ec = 0
                for n, k_ in enumerate(kinds):
                    if k_ == "m":
                        sl = bc % NW
                        bc += 1
                    else:
                        sl = ec % NWE
                        ec += 1
                    self.slot.append(sl)
                    self.prev.append(last.get(sl))
                    last[sl] = n

            def _try_load(self):
                while self.next_load < len(plan):
                    n = self.next_load
                    pv = self.prev[n]
                    if pv is not None and not self.released[pv]:
                        break
                    sl = self.slot[n]
                    if sl >= NW + 4 and self.allow_alias < 2:
                        break
                    if sl >= NW and self.allow_alias < 1:
                        break
                    src = plan[n]
                    a = src.shape[1]
                    dst = ringv[sl].rearrange("p (a b) -> p a b", a=a)
                    dma("pool", dst, src, (), [r_ring[sl]], bar=False)
                    self.next_load += 1

            def acquire(self, a):
                self._try_load()
                n = self.next_acq
                assert n < self.next_load, "ring: item not loaded (release order problem)"
                assert plan[n].shape[1] == a
                self.next_acq += 1
                sl = self.slot[n]
                v = ringv[sl].rearrange("p (a b) -> p a b", a=a)
                return n, v, r_ring[sl]

            def release(self, n):
                self.released[n] = True
                self._try_load()

            def set_alias(self, level):
                self.allow_alias = level
                if level:
                    self._try_load()

        RG = Ring()

        dma("sp", cst[:, :], cst_in[:, :], (), [r_cst])
        cp("dve", cbf[:, :], cst[:, 0:384], [r_cst], [r_cbf])

        def ln_tmp():
            return (AR.alloc([128, 12], F32), AR.alloc([128, 2], F32), AR.alloc([128, 1], F32),
                    AR.alloc([128, 1], F32), R(), R(), R(), R())

        def ln_stats(zt, r_z, tmp_):
            stt_, mv, rs, nmr, r_s, r_mv, r_rs, r_nmr = tmp_
            S.op("dve", lambda e: e.bn_stats(out=stt_[:, 0:6], in_=zt[:, 0:512]), [r_z], [r_s])
            S.op("dve", lambda e: e.bn_stats(out=stt_[:, 6:12], in_=zt[:, 512:1024]), [r_z], [r_s])
            S.op("dve", lambda e: e.bn_aggr(out=mv, in_=stt_), [r_s], [r_mv])
            act(rs, mv[:, 1:2], AF.Sqrt, [r_mv, r_eps], [r_rs], bias=epsc[:, 0:1])

        def ln_norm(zt, r_z, tmp_):
            stt_, mv, rs, nmr, r_s, r_mv, r_rs, r_nmr = tmp_
            S.op("dve", lambda e: e.reciprocal(out=rs, in_=rs), [r_rs], [r_rs])
            ts("dve", nmr, mv[:, 0:1], rs[:, 0:1], -1.0, ALU.mult, ALU.mult, [r_mv, r_rs], [r_nmr])
            act(zt, zt, AF.Identity, [r_z, r_rs, r_nmr], [r_z], bias=nmr[:, 0:1], scale=rs[:, 0:1])

        def ln_affine(zt, r_z, gv, bv, r_gb):
            tt("dve", zt, zt, gv, ALU.mult, [r_z, r_gb], [r_z])
            tt("dve", zt, zt, bv, ALU.add, [r_z, r_gb], [r_z])

        def ln_apply(zt, r_z, gv, bv, r_gb, tmp_):
            ln_norm(zt, r_z, tmp_)
            ln_affine(zt, r_z, gv, bv, r_gb)

        def ln_rows(zt, r_z, out32, r_out, gv, bv, r_gb, tmp_):
            ln_stats(zt, r_z, tmp_)
            ln_apply(zt, r_z, gv, bv, r_gb, tmp_)

        def to_xT(src_bf, r_src, i):
            b = next_bank()
            pb = bank_bf(b)
            for kc in range(8):
                tr(pb[:, kc * 128:(kc + 1) * 128], src_bf[:, kc * 128:(kc + 1) * 128], ident_bf,
                   [r_src, r_cbf], [r_bank[b]])
            cp("act" if i % 2 else "dve", xT[:, :, i * 128:(i + 1) * 128],
               pb.rearrange("p (a b) -> p a b", a=8), [r_bank[b]], [r_xT[i]])

        AR.reset()
        mg_bc = AR.alloc([128, 2 * D], F32)
        r_mgbc = R()
        lnt = [ln_tmp(), ln_tmp()]
        zer = AR.alloc([128, 8 * D], BF16)
        r_zer = R()
        memset("pool", zer, 0.0, [r_zer])
        for j_ in range(NE * CAP // 1024):
            dma("sp", xd[1 + j_ * 1024:1 + (j_ + 1) * 1024, :].rearrange("(p r) d -> p r d", p=128),
                zer.rearrange("p (r d) -> p r d", r=8), [r_zer], ())
        dma("sp", xd[0:1, :], zer[0:1, 0:D], [r_zer], ())
        dma("sp", ys[0:1, :], zer[0:1, 0:D], [r_zer], ())
        dma("sp", mg_bc, memln[0:1, :].to_broadcast([128, 2 * D]), (), [r_mgbc])
        for mt in range(2):
            mtile = AR.alloc([128, D], F32)
            mbf = AR.alloc([128, D], BF16)
            r_mt, r_mb = R(), R()
            dma("sp", mtile, mem_in[mt * 128:(mt + 1) * 128, :], (), [r_mt])
            ln_rows(mtile, r_mt, mtile, r_mt, mg_bc[:, 0:D], mg_bc[:, D:2 * D], r_mgbc, lnt[mt])
            cp("act", mbf, mtile, [r_mt], [r_mb])
            b = next_bank()
            pb = bank_bf(b)
            for kc in range(8):
                tr(pb[:, kc * 128:(kc + 1) * 128], mbf[:, kc * 128:(kc + 1) * 128], ident_bf,
                   [r_mb, r_cbf], [r_bank[b]])
            cp("dve", memT[:, :, mt * 128:(mt + 1) * 128], pb.rearrange("p (a b) -> p a b", a=8),
               [r_bank[b]], [r_memT])

        AR.reset()
        xl = [AR.alloc([128, D], F32) for _ in range(2)]
        xb = [AR.alloc([128, D], BF16) for _ in range(2)]
        r_xl = [R(), R()]
        r_xb = [R(), R()]
        for i in range(NT):
            k = i % 2
            dma("sp", xl[k], x_in[i * 128:(i + 1) * 128, :], (), [r_xl[k]])
            cp("act" if i % 2 == 0 else "dve", xb[k], xl[k], [r_xl[k]], [r_xb[k]])
            to_xT(xb[k], r_xb[k], i)

        res_src = {i: (x_in, None) for i in range(NT)}
        final_ops = []

        for l in range(NL):
            last = (l == NL - 1)
            S.epoch += 1
            AR.reset()
            dma("sp", colp[:, :], colp_in[l], (), [r_colp])
            dma("sp", lnbc[:, :], rowbc_in[l].to_broadcast([128, 4 * D]), (), [r_lnbc])
            dma("sp", rowsm[:, :], rowsm_in[l], (), [r_rowsm])
            dma("sp", wr[:, :, :], router_w[l].rearrange("(kc p) e -> p kc e", p=128), (), [r_wr])
            dma("sp", bdown[:, :], exp_down_b[l], (), [r_bdown])
            dma("pool", poolw[:, :, :], pool_w[l].rearrange("g c d -> c g d"), (), [r_poolw])
            ts("dve", upb1[:, :].rearrange("p (e j) -> p e j", e=NE),
               colp[:, C_UPB:C_UPB + 512].rearrange("p (e j) -> p e j", e=NE)[:, :, 8:16],
               1.0, None, ALU.add, None, [r_colp], [r_upb1])

            def bcol(c):
                return colp[:, c:c + 1]

            n_k, Wk, r_Wk = RG.acquire(8)
            for hd in range(4):
                b = next_bank()
                for kc in range(8):
                    mm(bank(b)[:, 0:MEM], Wk[:, kc, hd * 128:(hd + 1) * 128], memT[:, kc, :],
                       kc == 0, kc == 7, [r_Wk, r_memT], [r_bank[b]])
                cp("act", kT[:, hd, :], bank(b)[:, 0:MEM], [r_bank[b]], [r_kT])
            RG.release(n_k)
            n_v, Wv, r_Wv = RG.acquire(8)
            for mc in range(2):
                b = next_bank()
                for kc in range(8):
                    mm(bank(b), memT[:, kc, mc * 128:(mc + 1) * 128], Wv[:, kc, :],
                       kc == 0, kc == 7, [r_Wv, r_memT], [r_bank[b]])
                cp("act", vv[:, mc, :], bank(b), [r_bank[b]], [r_vv])
            RG.release(n_v)

            def xT_regs(cbi):
                c0, n = CBS[cbi]
                return [r_xT[i] for i in range(c0 // 128, (c0 + n) // 128)]

            def mg_regs(cbi):
                c0, n = CBS[cbi]
                return [r_mg[i] for i in range(c0 // 128, (c0 + n) // 128)]

            def gate_pass(br, P, r_P, bias_cols=None):
                sg = [AR.alloc([128, 512], F32) for _ in range(2)]
                tmp = [AR.alloc([128, 512], F32) for _ in range(2)]
                r_sg = [R(), R()]
                r_tmp = [R(), R()]
                it = 0
                for half in range(2):
                    n_g, G, r_Gs = RG.acquire(8)
                    for jj in range(4):
                        j = half * 4 + jj
                        for cbi, (c0, n) in enumerate(CBS):
                            bg = next_bank()
                            for kc in range(8):
                                mm(bank(bg)[:, 0:n], G[:, kc, jj * 128:(jj + 1) * 128], xT[:, kc, c0:c0 + n],
                                   kc == 0, kc == 7, [r_Gs] + xT_regs(cbi), [r_bank[bg]])
                            by = next_bank()
                            for kc in range(4):
                                mm(bank(by)[:, 0:n], P[:, kc, j * 128:(j + 1) * 128], brT[:, kc, c0:c0 + n],
                                   kc == 0, kc == 3, [r_P] + [r_br[kc][cbi]], [r_bank[by]])
                            k = it % 2
                            it += 1
                            act(sg[k][:, 0:n], bank(bg)[:, 0:n], AF.Sigmoid, [r_bank[bg], r_colp], [r_sg[k]],
                                bias=bcol(C_BIN + 16 + br * 8 + j))
                            mdst = mergedT[:, j, c0:c0 + n]
                            if br == 0:
                                tt("dve", mdst, sg[k][:, 0:n], bank(by)[:, 0:n], ALU.mult,
                                   [r_sg[k], r_bank[by]], mg_regs(cbi))
                            else:
                                if bias_cols is not None:
                                    stt("dve", tmp[k][:, 0:n], bank(by)[:, 0:n], bcol(bias_cols + j), sg[k][:, 0:n],
                                        ALU.add, ALU.mult, [r_bank[by], r_sg[k], r_colp], [r_tmp[k]])
                                else:
                                    tt("dve", tmp[k][:, 0:n], sg[k][:, 0:n], bank(by)[:, 0:n], ALU.mult,
                                       [r_sg[k], r_bank[by]], [r_tmp[k]])
                                tt("dve", mdst, mdst, tmp[k][:, 0:n], ALU.add, [r_tmp[k]] + mg_regs(cbi),
                                   mg_regs(cbi))
                    RG.release(n_g)

            AR.reset()
            n_wp, Wp, r_Wp = RG.acquire(8)
            u = AR.alloc([128, 16 + T], F32)
            sA = AR.alloc([128, 16 + T], F32)
            sB = AR.alloc([128, 16 + T], F32)
            pooled = AR.alloc([128, T], BF16)
            r_u, r_sA, r_sB, r_pl = R(), R(), R(), R()
            memset("pool", u[:, 0:16], 0.0, [r_u])
            memset("pool", sA[:, 0:16], 0.0, [r_sA])
            memset("pool", sB[:, 0:16], 0.0, [r_sB])
            for g in range(4):
                for cbi, (c0, n) in enumerate(CBS):
                    b = next_bank()
                    for kc in range(8):
                        mm(bank(b)[:, 0:n], Wp[:, kc, g * 128:(g + 1) * 128], xT[:, kc, c0:c0 + n],
                           kc == 0, kc == 7, [r_Wp] + xT_regs(cbi), [r_bank[b]])
                    act(u[:, 16 + c0:16 + c0 + n], bank(b)[:, 0:n], AF.Identity, [r_bank[b], r_colp], [r_u],
                        bias=bcol(C_BIN + g))
                tt("dve", u[:, 16:144], u[:, 16:144], vmask, ALU.mult, [r_u, r_cst], [r_u])
                src, r_src = u, r_u
                bufs = [(sA, r_sA), (sB, r_sB)]
                sh = 1
                for step in range(g + 1):
                    dst, r_dst = bufs[step % 2]
                    tt("dve", dst[:, 16:16 + T], src[:, 16:16 + T], src[:, 16 - sh:16 + T - sh], ALU.add,
                       [r_src], [r_dst])
                    src, r_src = dst, r_dst
                    sh *= 2
                w_ = 2 ** (g + 1)
                tt("dve", src[:, 16 + 128:16 + 144], src[:, 16 + 128:16 + 144],
                   cst[:, K_PF + g * 16:K_PF + (g + 1) * 16], ALU.mult, [r_src, r_cst], [r_src])
                stt("dve", pooled[:, :], src[:, 16:16 + T], 1.0 / w_, u[:, 16:16 + T], ALU.mult, ALU.subtract,
                    [r_src, r_u], [r_pl])
                for cbi, (c0, n) in enumerate(CBS):
                    b = next_bank()
                    mm(bank(b)[:, 0:n], poolw[:, g, :], pooled[:, c0:c0 + n], True, True,
                       [r_poolw, r_pl], [r_bank[b]])
                    act(brT[:, g, c0:c0 + n], bank(b)[:, 0:n], AF.Identity, [r_bank[b], r_colp], [r_br[g][cbi]],
                        scale=bcol(C_PSC + g))
            RG.release(n_wp)
            n_pp, Pp, r_Pp = RG.acquire(4)
            gate_pass(0, Pp, r_Pp)
            RG.release(n_pp)

            AR.reset()
            n_wa, Wa, r_Wa = RG.acquire(8)
            n_wg, Wg, r_Wg = RG.acquire(8)
            diag = AR.alloc([128, 31, 128], BF16)
            hb = [AR.alloc([128, 32 + T], BF16) for _ in range(2)]
            sgc = [AR.alloc([128, 512], F32) for _ in range(2)]
            r_diag = R()
            r_hb = [R(), R()]
            r_sgc = [R(), R()]
            memset("pool", hb[0][:, 0:32], 0.0, [r_hb[0]])
            memset("pool", hb[1][:, 0:32], 0.0, [r_hb[1]])
            it = 0
            for c in range(4):
                h_, r_h = hb[c % 2], r_hb[c % 2]
                for k in range(31):
                    ts("dve", diag[:, k, :], ident_bf, bcol(C_CDW + c * 31 + k), None,
                       ALU.mult, None, [r_cbf, r_colp], [r_diag])
                for cbi, (c0, n) in enumerate(CBS):
                    ba = next_bank()
                    for kc in range(8):
                        mm(bank(ba)[:, 0:n], Wa[:, kc, c * 128:(c + 1) * 128], xT[:, kc, c0:c0 + n],
                           kc == 0, kc == 7, [r_Wa] + xT_regs(cbi), [r_bank[ba]])
                    bg = next_bank()
                    for kc in range(8):
                        mm(bank(bg)[:, 0:n], Wg[:, kc, c * 128:(c + 1) * 128], xT[:, kc, c0:c0 + n],
                           kc == 0, kc == 7, [r_Wg] + xT_regs(cbi), [r_bank[bg]])
                    k = it % 2
                    it += 1
                    act(sgc[k][:, 0:n], bank(bg)[:, 0:n], AF.Sigmoid, [r_bank[bg], r_colp], [r_sgc[k]],
                        bias=bcol(C_BIN + 8 + c))
                    stt("dve", h_[:, 32 + c0:32 + c0 + n], bank(ba)[:, 0:n], bcol(C_BIN + 4 + c), sgc[k][:, 0:n],
                        ALU.add, ALU.mult, [r_bank[ba], r_sgc[k], r_colp], [r_h])
                tt("dve", h_[:, 32:160], h_[:, 32:160], vmask, ALU.mult, [r_h, r_cst], [r_h])
                for cbi, (c0, n) in enumerate(CBS):
                    b = next_bank()
                    for k in range(31):
                        mm(bank(b)[:, 0:n], diag[:, k, :], h_[:, 32 + c0 - 30 + k:32 + c0 - 30 + k + n],
                           k == 0, k == 30, [r_diag, r_h], [r_bank[b]])
                    act(brT[:, c, c0:c0 + n], bank(b)[:, 0:n], AF.Identity, [r_bank[b], r_colp], [r_br[c][cbi]],
                        bias=bcol(C_CDWB + c))
            RG.release(n_wa)
            RG.release(n_wg)
            sq = AR.alloc([128, 4, 512], BF16)
            mean = AR.alloc([128, 512], F32)
            msq = AR.alloc([128, 512], F32)
            rstd = AR.alloc([128, 512], F32)
            zc = [AR.alloc([128, 512], F32) for _ in range(2)]
            r_sq, r_mean, r_msq, r_rstd = R(), R(), R(), R()
            r_zc = [R(), R()]
            for cbi, (c0, n) in enumerate(CBS):
                for c in range(4):
                    act(sq[:, c, 0:n], brT[:, c, c0:c0 + n], AF.Square, [r_br[c][cbi]], [r_sq])
                b1 = next_bank()
                for c in range(4):
                    mm(bank(b1)[:, 0:n], ones_bf, brT[:, c, c0:c0 + n], c == 0, c == 3,
                       [r_cbf, r_br[c][cbi]], [r_bank[b1]])
                b2 = next_bank()
                for c in range(4):
                    mm(bank(b2)[:, 0:n], ones_bf, sq[:, c, 0:n], c == 0, c == 3, [r_cbf, r_sq], [r_bank[b2]])
                ts("dve", mean[:, 0:n], bank(b1)[:, 0:n], 1.0 / 512, None, ALU.mult, None, [r_bank[b1]], [r_mean])
                tt("dve", msq[:, 0:n], mean[:, 0:n], mean[:, 0:n], ALU.mult, [r_mean], [r_msq])
                stt("dve", rstd[:, 0:n], bank(b2)[:, 0:n], 1.0 / 512, msq[:, 0:n], ALU.mult, ALU.subtract,
                    [r_bank[b2], r_msq], [r_rstd])
                ts("dve", rstd[:, 0:n], rstd[:, 0:n], 0.0, None, ALU.max, None, [r_rstd], [r_rstd])
                act(rstd[:, 0:n], rstd[:, 0:n], AF.Ln, [r_rstd, r_eps], [r_rstd], bias=epsc[:, 0:1])
                act(rstd[:, 0:n], rstd[:, 0:n], AF.Exp, [r_rstd], [r_rstd], scale=-0.5)
                for c in range(4):
                    k = c % 2
                    tt("dve", zc[k][:, 0:n], brT[:, c, c0:c0 + n], mean[:, 0:n], ALU.subtract,
                       [r_br[c][cbi], r_mean], [r_zc[k]])
                    tt("dve", zc[k][:, 0:n], zc[k][:, 0:n], rstd[:, 0:n], ALU.mult, [r_zc[k], r_rstd], [r_zc[k]])
                    act(brT[:, c, c0:c0 + n], zc[k][:, 0:n], AF.Silu, [r_zc[k], r_colp], [r_br[c][cbi]],
                        bias=bcol(C_CLB + c), scale=bcol(C_CLG + c))
            n_cp, Pc, r_Pc = RG.acquire(4)
            gate_pass(1, Pc, r_Pc, bias_cols=C_CPWB)
            RG.release(n_cp)

            AR.reset()
            n_wq, Wq, r_Wq = RG.acquire(8)
            qb = [AR.alloc([128, 512], BF16) for _ in range(2)]
            eb = [AR.alloc([128, 2, 512], BF16) for _ in range(2)]
            rden = [AR.alloc([128, 512], F32) for _ in range(2)]
            r_qb = [R(), R()]
            r_eb = [R(), R()]
            r_rden = [R(), R()]
            items = [(hd, cbi) for hd in range(4) for cbi in range(len(CBS))]

            def at_S1(t):
                hd, cbi = items[t]
                c0, n = CBS[cbi]
                k = t % 2
                b = next_bank()
                for kc in range(8):
                    mm(bank(b)[:, 0:n], Wq[:, kc, hd * 128:(hd + 1) * 128], xT[:, kc, c0:c0 + n],
                       kc == 0, kc == 7, [r_Wq] + xT_regs(cbi), [r_bank[b]])
                act(qb[k][:, 0:n], bank(b)[:, 0:n], AF.Identity, [r_bank[b], r_colp], [r_qb[k]],
                    bias=bcol(C_BIN + 12 + hd))

            def at_S2(t):
                hd, cbi = items[t]
                c0, n = CBS[cbi]
                k = t % 2
                for mc in range(2):
                    bs = next_bank()
                    mm(bank(bs)[:, 0:n], kT[:, hd, mc * 128:(mc + 1) * 128], qb[k][:, 0:n], True, True,
                       [r_kT, r_qb[k]], [r_bank[bs]])
                    act(eb[k][:, mc, 0:n], bank(bs)[:, 0:n], AF.Exp, [r_bank[bs]], [r_eb[k]], scale=ATT_SCALE)

            def at_S3(t):
                hd, cbi = items[t]
                c0, n = CBS[cbi]
                k = t % 2
                bo = next_bank()
                for mc in range(2):
                    mm(bank(bo)[:, 0:n], vv[:, mc, hd * 128:(hd + 1) * 128], eb[k][:, mc, 0:n],
                       mc == 0, mc == 1, [r_vv, r_eb[k]], [r_bank[bo]])
                bd = next_bank()
                for mc in range(2):
                    mm(bank(bd)[:, 0:n], ones_bf, eb[k][:, mc, 0:n], mc == 0, mc == 1,
                       [r_cbf, r_eb[k]], [r_bank[bd]])
                act(rden[k][:, 0:n], bank(bd)[:, 0:n], AF.Ln, [r_bank[bd]], [r_rden[k]])
                act(rden[k][:, 0:n], rden[k][:, 0:n], AF.Exp, [r_rden[k]], [r_rden[k]], scale=-1.0)
                tt("dve", brT[:, hd, c0:c0 + n], bank(bo)[:, 0:n], rden[k][:, 0:n], ALU.mult,
                   [r_bank[bo], r_rden[k]], [r_br[hd][cbi]])

            NI = len(items)
            for step in range(NI + 2):
                if step < NI:
                    at_S1(step)
                if 0 <= step - 1 < NI:
                    at_S2(step - 1)
                if 0 <= step - 2 < NI:
                    at_S3(step - 2)
            RG.release(n_wq)
            n_ao, Pa, r_Pa = RG.acquire(4)
            gate_pass(2, Pa, r_Pa)
            RG.release(n_ao)

            AR.reset()
            RG.set_alias(1)
            n_o0, Wo0, r_Wo0 = RG.acquire(8)
            n_o1, Wo1, r_Wo1 = RG.acquire(8)
            Wo = [(Wo0, r_Wo0), (Wo1, r_Wo1)]
            xr = [AR.alloc([128, D], F32) for _ in range(2)]
            zt = [AR.alloc([128, D], F32) for _ in range(2)]
            x1b = [AR.alloc([128, D], BF16) for _ in range(2)]
            x1T = AR.alloc([128, 8, 128], F32)
            lnt = [ln_tmp(), ln_tmp()]
            lg = AR.alloc([128, NE], F32)
            m8 = AR.alloc([128, 8], F32)
            nm = AR.alloc([128, 1], F32)
            Af = AR.alloc([128, NE], F32)
            ex = AR.alloc([128, NE], F32)
            ssum = AR.alloc([128, 1], F32)
            Abf = AR.alloc([128, NE], BF16)
            Asum = [AR.alloc([128, NE], BF16) for _ in range(2)]
            key = AR.alloc([128, NE], F32)
            k8 = AR.alloc([128, 8], F32)
            junk = AR.alloc([128, NE], F32)
            r_xr = [R(), R()]
            r_zt = [R(), R()]
            r_x1b = [R(), R()]
            r_x1T, r_lg, r_m8, r_nm, r_Af, r_ex, r_ss, r_Abf, r_key, r_k8, r_junk = (R() for _ in range(11))
            r_As = [R(), R()]
            scat_ops = []
            new_res = {}
            lg_all = AR.alloc([128, NT, NE], F32)
            r_lga = [R() for _ in range(NT)]

            x1b3 = [x1b[0], x1b[1], AR.alloc([128, D], BF16), AR.alloc([128, D], BF16)]
            r_x1b3 = [r_x1b[0], r_x1b[1], R(), R()]
            zt3 = [zt[0], zt[1], AR.alloc([128, D], F32)]
            r_zt3 = [r_zt[0], r_zt[1], R()]
            lnt3 = [lnt[0], lnt[1], ln_tmp()]

            def l1_A(i):
                k = i % 2
                z3 = i % 3
                src_t, src_op = res_src[i]
                dma("sp", xr[k], src_t[i * 128:(i + 1) * 128, :], (), [r_xr[k]], deps=[src_op])
                b = 2 * k
                for half in range(2):
                    W_, r_W = Wo[half]
                    for kc in range(8):
                        mm(bank(b + half), mergedT[:, kc, i * 128:(i + 1) * 128], W_[:, kc, :],
                           kc == 0, False, [r_mg[i], r_W], [r_bank[b + half]])
                    mm(bank(b + half), onesrow_f, rowsm[0:1, half * 512:(half + 1) * 512], False, True,
                       [r_cst, r_rowsm], [r_bank[b + half]])
                for half in range(2):
                    stt("dve", zt3[z3][:, half * 512:(half + 1) * 512], xr[k][:, half * 512:(half + 1) * 512], ALPHA,
                        bank(b + half), ALU.mult, ALU.add, [r_xr[k], r_bank[b + half]], [r_zt3[z3]])
                ln_stats(zt3[z3], r_zt3[z3], lnt3[z3])

            def l1_B1(i):
                z3 = i % 3
                ln_norm(zt3[z3], r_zt3[z3], lnt3[z3])

            def l1_Bg(i):
                z3 = i % 3
                k4 = i % 4
                ln_affine(zt3[z3], r_zt3[z3], lnbc[:, 0:D], lnbc[:, D:2 * D], r_lnbc)
                st_op = dma("sp", xres[i * 128:(i + 1) * 128, :], zt3[z3], [r_zt3[z3]], ())
                new_res[i] = (xres, st_op)
                cp("act", x1b3[k4], zt3[z3], [r_zt3[z3]], [r_x1b3[k4]])

            def l1_Bt(i):
                z3 = i % 3
                b = 4
                for kc in range(8):
                    bb = b + kc // 4
                    tr(bank(bb)[:, (kc % 4) * 128:(kc % 4 + 1) * 128], zt3[z3][:, kc * 128:(kc + 1) * 128], ident_f,
                       [r_zt3[z3], r_cst], [r_bank[bb]])
                cp("act", x1T[:, 0:4, :], bank(b).rearrange("p (a b) -> p a b", a=4), [r_bank[b]], [r_x1T])
                cp("act", x1T[:, 4:8, :], bank(b + 1).rearrange("p (a b) -> p a b", a=4), [r_bank[b + 1]], [r_x1T])
                c0_ = (i % 4) * NE
                for kc in range(8):
                    mm(bank(6)[:, c0_:c0_ + NE], x1T[:, kc, :], wr[:, kc, :], kc == 0, False,
                       [r_x1T, r_wr], [r_bank[6]])
                mm(bank(6)[:, c0_:c0_ + NE], onesrow_f, rowsm[0:1, D:D + NE], False, True,
                   [r_cst, r_rowsm], [r_bank[6]])

            def l1_C(i):
                k = i % 2
                c0_ = (i % 4) * NE
                lg = lg_all[:, i, :]
                r_lg = r_lga[i]
                cp("dve", lg, bank(6)[:, c0_:c0_ + NE], [r_bank[6]], [r_lg])
                S.op("dve", lambda e: e.max(out=m8, in_=lg), [r_lg], [r_m8])
                ts("dve", Af, lg, m8[:, 3:4], None, ALU.is_ge, None, [r_lg, r_m8], [r_Af])
                if i == 0:
                    ts("dve", Af, Af, vtok, None, ALU.mult, None, [r_Af, r_cst], [r_Af])
                ts("dve", nm, m8[:, 0:1], -1.0, None, ALU.mult, None, [r_m8], [r_nm])
                act(ex, lg, AF.Exp, [r_lg, r_nm], [r_ex], bias=nm[:, 0:1])
                stt("dve", ex, Af, 1.0, ex, ALU.mult, ALU.mult, [r_Af, r_ex], [r_ex, r_ss], accum_out=ssum)
                ts("dve", ssum, ssum, 1e-30, None, ALU.max, None, [r_ss], [r_ss])
                S.op("dve", lambda e: e.reciprocal(out=ssum, in_=ssum), [r_ss], [r_ss])
                ts("dve", G_all[:, i, :], ex, ssum[:, 0:1], None, ALU.mult, None, [r_ex, r_ss], [r_G[i]])
                cp("dve", Abf, Af, [r_Af], [r_Abf])
                if i > 0:
                    tt("dve", Asum[k], Asum[(i - 1) % 2], Abf, ALU.add, [r_As[(i - 1) % 2], r_Abf], [r_As[k]])
                else:
                    cp("dve", Asum[k], Abf, [r_Abf], [r_As[k]])

            def l1_D(i):
                k3 = i % 4
                c0_ = (i % 4) * NE
                pp = bank(7)[:, c0_:c0_ + NE]
                mm(pp, tri_bf, Abf, True, i == 0, [r_cbf, r_Abf], [r_bank[7]])
                if i > 0:
                    mm(pp, ones_bf, Asum[(i - 1) % 2], False, True,
                       [r_cbf, r_As[(i - 1) % 2]], [r_bank[7]])
                stt("dve", key, pp, float(CAP - 1), base1, ALU.min, ALU.add,
                    [r_bank[7], r_cst], [r_key])
                tt("dve", key, key, Af, ALU.mult, [r_key, r_Af], [r_key])
                S.op("dve", lambda e: e.max(out=k8, in_=key), [r_key], [r_k8])
                cp("dve", idx_all[:, i * 4:(i + 1) * 4], k8[:, 0:4], [r_k8], [r_idx[i]])
                for kk in range(4):
                    stt("dve", junk, key, k8[:, kk:kk + 1], G_all[:, i, :], ALU.is_equal, ALU.mult,
                        [r_key, r_k8, r_G[i]], [r_junk, r_gate[i]],
                        accum_out=gate_all[:, i * 4 + kk:i * 4 + kk + 1])
                for kk in range(4):
                    o = S.dma("pool", lambda e, s_=x1b3[k3], ix=idx_all[:, i * 4 + kk:i * 4 + kk + 1]:
                              e.indirect_dma_start(out=xd[:, :],
                                                   out_offset=bass.IndirectOffsetOnAxis(ap=ix, axis=0),
                                                   in_=s_, in_offset=None,
                                                   bounds_check=regs["bc"], oob_is_err=False),
                              [r_x1b3[k3], r_idx[i]], ())
                    scat_ops.append(o)

            for step in range(NT + 4):
                if 0 <= step - 1 < NT:
                    l1_B1(step - 1)
                if step < NT:
                    l1_A(step)
                if 0 <= step - 1 < NT:
                    l1_Bg(step - 1)
                if 0 <= step - 2 < NT:
                    l1_Bt(step - 2)
                if 0 <= step - 4 < NT:
                    l1_D(step - 4)
                if 0 <= step - 3 < NT:
                    l1_C(step - 3)
            RG.release(n_o0)
            RG.release(n_o1)
            res_src = new_res

            AR.reset()
            RG.set_alias(2)
            brflat = brT[:, :, :].rearrange("p a b -> p (a b)")
            xg = [brflat[:, q_ * 3 * D:(q_ + 1) * 3 * D].rearrange("p (a b) -> p a b", a=3) for q_ in range(2)]
            xgT = [AR.alloc([128, 8, CAP], BF16) for _ in range(2)]
            actT = [AR.alloc([128, 8, CAP], BF16) for _ in range(2)]
            gc = [AR.alloc([128, CAP], F32) for _ in range(2)]
            sgm = [AR.alloc([128, CAP], F32) for _ in range(2)]
            t1 = [AR.alloc([128, CAP], F32) for _ in range(2)]
            ybf = [AR.alloc([128, D], BF16) for _ in range(2)]
            r_xg = [R(), R()]
            r_xgT = [R(), R()]
            r_actT = [R(), R()]
            r_gc = [R(), R()]
            r_sgm = [R(), R()]
            r_t1 = [R(), R()]
            r_ybf = [R(), R()]
            ys_ops = []
            cnt_ = {"it": 0, "ity": 0}

            def load_xg(e_):
                p = e_ % 2
                r0 = 1 + e_ * CAP
                dma("sp", xg[p], xd[r0:r0 + CAP, :].rearrange("(t p) d -> p t d", p=128), (), [r_xg[p]],
                    deps=scat_ops if e_ < 2 else ())

            def transposes(e_):
                p = e_ % 2
                for kp in range(4):
                    b = next_bank()
                    pb = bank_bf(b)
                    for kc2 in range(2):
                        kc = kp * 2 + kc2
                        for ti in range(3):
                            tr(pb[:, kc2 * CAP + ti * 128:kc2 * CAP + (ti + 1) * 128],
                               xg[p][:, ti, kc * 128:(kc + 1) * 128], ident_bf, [r_xg[p], r_cbf], [r_bank[b]])
                    cp("act" if kp % 2 else "dve", xgT[p][:, kp * 2:kp * 2 + 2, :],
                       pb[:, 0:2 * CAP].rearrange("p (a b) -> p a b", a=2), [r_bank[b]], [r_xgT[p]])

            def up(e_, hf):
                p = e_ % 2
                n_ug, Ug, r_Ug = RG.acquire(8)
                n_ul, Ul, r_Ul = RG.acquire(8)
                for jj in range(4):
                    j = hf * 4 + jj
                    bg = next_bank()
                    for kc in range(8):
                        mm(bank(bg)[:, 0:CAP], Ug[:, kc, jj * 128:(jj + 1) * 128], xgT[p][:, kc, :],
                           kc == 0, kc == 7, [r_Ug, r_xgT[p]], [r_bank[bg]])
                    bl = next_bank()
                    for kc in range(8):
                        mm(bank(bl)[:, 0:CAP], Ul[:, kc, jj * 128:(jj + 1) * 128], xgT[p][:, kc, :],
                           kc == 0, kc == 7, [r_Ul, r_xgT[p]], [r_bank[bl]])
                    k = cnt_["it"] % 2
                    cnt_["it"] += 1
                    ts("dve", gc[k], bank(bg)[:, 0:CAP], bcol(C_UPB + e_ * 16 + j), 7.0, ALU.add, ALU.min,
                       [r_bank[bg], r_colp], [r_gc[k]])
                    act(sgm[k], gc[k], AF.Sigmoid, [r_gc[k]], [r_sgm[k]], scale=1.702)
                    act(t1[k], bank(bl)[:, 0:CAP], AF.Identity, [r_bank[bl], r_upb1], [r_t1[k]],
                        bias=upb1[:, e_ * 8 + j:e_ * 8 + j + 1])
                    ts("dve", t1[k], t1[k], -6.0, 8.0, ALU.max, ALU.min, [r_t1[k]], [r_t1[k]])
                    tt("dve", gc[k], gc[k], sgm[k], ALU.mult, [r_gc[k], r_sgm[k]], [r_gc[k]])
                    tt("dve", actT[p][:, j, :], t1[k], gc[k], ALU.mult, [r_t1[k], r_gc[k]], [r_actT[p]])
                RG.release(n_ug)
                RG.release(n_ul)

            def down(e_):
                p = e_ % 2
                r0 = 1 + e_ * CAP
                n_d0, D0, r_D0 = RG.acquire(8)
                n_d1, D1, r_D1 = RG.acquire(8)
                Dn = [(D0, r_D0), (D1, r_D1)]
                for ti in range(3):
                    b = next_bank2()
                    for half in range(2):
                        W_, r_W = Dn[half]
                        for kc in range(8):
                            mm(bank(b + half), actT[p][:, kc, ti * 128:(ti + 1) * 128], W_[:, kc, :],
                               kc == 0, kc == 7, [r_actT[p], r_W], [r_bank[b + half]])
                    k = cnt_["ity"] % 2
                    cnt_["ity"] += 1
                    cp("act", ybf[k][:, 0:512], bank(b), [r_bank[b]], [r_ybf[k]])
                    cp("dve", ybf[k][:, 512:1024], bank(b + 1), [r_bank[b + 1]], [r_ybf[k]])
                    o = dma("sp", ys[r0 + ti * 128:r0 + (ti + 1) * 128, :], ybf[k], [r_ybf[k]], ())
                    ys_ops.append(o)
                RG.release(n_d0)
                RG.release(n_d1)

            load_xg(0)
            load_xg(1)
            transposes(0)
            up(0, 0)
            up(0, 1)
            for e_ in range(NE):
                if e_ + 1 < NE:
                    if e_ + 2 < NE:
                        load_xg(e_ + 2)
                    transposes(e_ + 1)
                    up(e_ + 1, 0)
                down(e_)
                if e_ + 1 < NE:
                    up(e_ + 1, 1)

            RG.set_alias(0)
            AR.reset()
            yk = [AR.alloc([128, 4, D], BF16) for _ in range(2)]
            xr = [AR.alloc([128, D], F32) for _ in range(2)]
            zt = [AR.alloc([128, D], F32) for _ in range(2)]
            x2b = [AR.alloc([128, D], BF16) for _ in range(2)]
            GT = AR.alloc([NE, 128], F32)
            lnt = [ln_tmp(), ln_tmp()]
            r_yk = [R(), R()]
            r_xr = [R(), R()]
            r_zt = [R(), R()]
            r_x2b = [R(), R()]
            r_GT = R()
            new_res = {}

            def l2_G(i):
                k = i % 2
                for kk in range(4):
                    S.dma("pool", lambda e, d_=yk[k][:, kk, :], ix=idx_all[:, i * 4 + kk:i * 4 + kk + 1]:
                          e.indirect_dma_start(out=d_, out_offset=None, in_=ys[:, :],
                                               in_offset=bass.IndirectOffsetOnAxis(ap=ix, axis=0),
                                               bounds_check=regs["bc"], oob_is_err=False),
                          [r_idx[i]], [r_yk[k]], deps=ys_ops if (i == 0 and kk == 0) else ())
                src_t, src_op = res_src[i]
                dma("sp", xr[k], src_t[i * 128:(i + 1) * 128, :], (), [r_xr[k]], deps=[src_op])

            def l2_A0(i):
                k = i % 2
                tr(bank(4)[0:NE, 0:128], G_all[:, i, :], ident_f, [r_G[i], r_cst], [r_bank[4]])
                cp("act", GT, bank(4)[0:NE, 0:128], [r_bank[4]], [r_GT])
                b = 2 * k
                for half in range(2):
                    mm(bank(b + half), GT, bdown[:, half * 512:(half + 1) * 512], True, True,
                       [r_GT, r_bdown], [r_bank[b + half]])

            def l2_A(i):
                k = i % 2
                b = 2 * k
                for half in range(2):
                    stt("dve", zt[k][:, half * 512:(half + 1) * 512], xr[k][:, half * 512:(half + 1) * 512], ALPHA,
                        bank(b + half), ALU.mult, ALU.add, [r_xr[k], r_bank[b + half]], [r_zt[k]])
                for kk in range(4):
                    stt("dve", zt[k], yk[k][:, kk, :], gate_all[:, i * 4 + kk:i * 4 + kk + 1], zt[k],
                        ALU.mult, ALU.add, [r_yk[k], r_gate[i], r_zt[k]], [r_zt[k]])
                ln_stats(zt[k], r_zt[k], lnt[k])

            def l2_B1(i):
                k = i % 2
                ln_norm(zt[k], r_zt[k], lnt[k])

            def l2_B2(i):
                k = i % 2
                ln_affine(zt[k], r_zt[k], lnbc[:, 2 * D:3 * D], lnbc[:, 3 * D:4 * D], r_lnbc)
                if last:
                    if i > 0:
                        o = dma("sp", out_d[(i - 1) * 128:i * 128, :], zt[k], [r_zt[k]], ())
                        final_ops.append(o)
                else:
                    st_op = dma("sp", xres[i * 128:(i + 1) * 128, :], zt[k], [r_zt[k]], ())
                    new_res[i] = (xres, st_op)
                    cp("act", x2b[k], zt[k], [r_zt[k]], [r_x2b[k]])
                    b = 5 + k
                    pb = bank_bf(b)
                    for kc in range(8):
                        tr(pb[:, kc * 128:(kc + 1) * 128], x2b[k][:, kc * 128:(kc + 1) * 128], ident_bf,
                           [r_x2b[k], r_cbf], [r_bank[b]])
                    cp("act", xT[:, :, i * 128:(i + 1) * 128], pb.rearrange("p (a b) -> p a b", a=8),
                       [r_bank[b]], [r_xT[i]])

            l2_G(0)
            for step in range(NT + 1):
                if step + 1 < NT:
                    l2_G(step + 1)
                if step < NT:
                    l2_A0(step)
                if 0 <= step - 1 < NT:
                    l2_B1(step - 1)
                if step < NT:
                    l2_A(step)
                if 0 <= step - 1 < NT:
                    l2_B2(step - 1)
            res_src = new_res

        assert RG.next_acq == len(plan), (RG.next_acq, len(plan))
        S.finalize(final_ops)
    return nc


def _consts(half):
    c = np.zeros((128, NCST), np.float32)
    c[:, K_ID:K_ID + 128] = np.eye(128, dtype=np.float32)
    tp = np.arange(128)
    c[:, K_TRI:K_TRI + 128] = (tp[:, None] < tp[None, :]).astype(np.float32)
    c[:, K_ONE:K_ONE + 128] = 1.0
    valid = 0.0 if half == 0 else 1.0
    c[:, K_VM:K_VM + 128] = valid
    c[:, K_VT] = valid
    for g, w in enumerate((2, 4, 8, 16)):
        for t in range(16):
            cnt = min(t + 1, w) if half == 0 else w
            c[:, K_PF + g * 16 + t] = w / cnt
    c[:, K_B1:K_B1 + 32] = (1 + np.arange(NE) * CAP)[None, :].astype(np.float32)
    return c


def _layer_params(inp, ls):
    f = lambda a: np.ascontiguousarray(np.asarray(a, dtype=np.float32))
    colp = np.zeros((len(ls), 128, NCOLP), np.float32)
    for n, l in enumerate(ls):
        colp[n, :, C_BIN:C_BIN + 40] = inp["b_in"][l].reshape(40, 128).T
        colp[n, :, C_PSC:C_PSC + 4] = inp["pool_scale"][l].reshape(4, 128).T
        colp[n, :, C_CDW:C_CDW + 124] = inp["conv_dw"][l].T.reshape(4, 128, 31).transpose(1, 0, 2).reshape(128, 124)
        colp[n, :, C_CDWB:C_CDWB + 4] = inp["conv_dw_b"][l].reshape(4, 128).T
        colp[n, :, C_CLG:C_CLG + 4] = inp["conv_ln_g"][l].reshape(4, 128).T
        colp[n, :, C_CLB:C_CLB + 4] = inp["conv_ln_b"][l].reshape(4, 128).T
        colp[n, :, C_CPWB:C_CPWB + 8] = inp["conv_pw_b"][l].reshape(8, 128).T
        colp[n, :, C_UPB:C_UPB + 512] = inp["exp_up_b"][l].reshape(NE, 16, 128).transpose(2, 0, 1).reshape(128, 512)
    rowbc = np.stack([np.concatenate([inp["ln1_g"][l], inp["ln1_b"][l], inp["ln2_g"][l], inp["ln2_b"][l]])[None, :]
                      for l in ls]).astype(np.float32)
    rowsm = np.stack([np.concatenate([inp["b_out"][l], inp["router_b"][l]])[None, :] for l in ls]).astype(np.float32)
    sl = slice(ls[0], ls[-1] + 1)
    d = {
        "w_in": f(inp["w_in"][sl]), "pool_w": f(inp["pool_w"][sl]), "pool_proj": f(inp["pool_proj"][sl]),
        "conv_pw": f(inp["conv_pw"][sl]), "attn_o": f(inp["attn_o"][sl]), "w_kv": f(inp["w_kv"][sl]),
        "w_out": f(inp["w_out"][sl]), "exp_up": f(inp["exp_up"][sl]), "exp_down": f(inp["exp_down"][sl]),
        "router_w": f(inp["router_w"][sl]), "exp_down_b": f(inp["exp_down_b"][sl]),
        "colp": colp, "rowbc": rowbc, "rowsm": rowsm,
    }
    return d


def _x_shards(xfull):
    outs = []
    for c in range(8):
        b, half = c // 2, c % 2
        xs = np.zeros((T, D), np.float32)
        s0 = half * 2048
        xs[128:] = xfull[b, s0:s0 + 2048]
        if half == 1:
            xs[:128] = xfull[b, s0 - 128:s0]
        outs.append(xs)
    return outs


_PROG_CACHE = {}


def _get_prog(NL):
    if NL not in _PROG_CACHE:
        _PROG_CACHE[NL] = build_program(NL)
    return _PROG_CACHE[NL]


LAYERS_PER_LAUNCH = 4


def kernel(**inp):
    inp = {k: np.asarray(v) for k, v in inp.items()}
    x = np.asarray(inp["x"], np.float32)
    memln = np.concatenate([inp["mem_ln_g"], inp["mem_ln_b"]])[None, :].astype(np.float32)
    NL = LAYERS_PER_LAUNCH
    nc = _get_prog(NL)
    csts = [_consts(c % 2) for c in range(8)]
    for l0 in range(0, DEPTH, NL):
        lp = _layer_params(inp, list(range(l0, l0 + NL)))
        xs = _x_shards(x)
        in_maps = []
        for c in range(8):
            m = dict(lp)
            m["x_in"] = xs[c]
            m["mem"] = np.ascontiguousarray(inp["mem"][c // 2], dtype=np.float32)
            m["memln"] = memln
            m["cst"] = csts[c]
            in_maps.append(m)
        res = run_bass_kernel_spmd(nc, in_maps, core_ids=list(range(8)))
        xn = np.empty_like(x)
        for c in range(8):
            b, half = c // 2, c % 2
            xn[b, half * 2048:(half + 1) * 2048] = np.asarray(res.results[c]["out"], np.float32)
        x = xn
    return x
```

```python
import contextlib
import numpy as np
import ml_dtypes
import concourse.bass as bass
import concourse.mybir as mybir
from concourse.bass_utils import run_bass_kernel_spmd

F32 = mybir.dt.float32
BF16 = mybir.dt.bfloat16
I32 = mybir.dt.int32
AF = mybir.ActivationFunctionType
ALU = mybir.AluOpType

D = 1024
NT = 17
T = NT * 128
SEQ = 4096
DEPTH = 4
NE = 32
CAP = 384
NSLOT = 1 + NE * CAP
IN_COLS = 5120
MEM = 256
ALPHA = (2.0 * DEPTH) ** 0.25
LN_EPS = 1e-5
CBS = [(0, 512), (512, 512), (1024, 512), (1536, 512), (2048, 128)]
NW = 4
ATT_SCALE = 128 ** -0.5

C_BIN = 0
C_PSC = 40
C_CDW = 44
C_CDWB = 168
C_CLG = 172
C_CLB = 176
C_CPWB = 180
C_UPB = 188
NCOLP = 700
K_ID = 0
K_TRI = 128
K_ONE = 256
K_VM = 384
K_VT = 512
K_PF = 513
K_B1 = 577
NCST = 609


ENGS = ("pe", "act", "dve", "pool", "sp")


class Reg:
    __slots__ = ("w", "r", "rd")

    def __init__(self):
        self.w = None
        self.r = {}
        self.rd = []


class Op:
    __slots__ = ("eng", "fn", "deps", "is_dma", "sem", "val", "needed", "epoch")


class Sched:
    def __init__(self, nc, n_dma_sems=20):
        self.nc = nc
        self.q = {e: [] for e in ENGS}
        self.n_dma_sems = n_dma_sems
        self.epoch = 0
        self.pending = {}
        self.last_compute = {}
        self.dma_since_barrier = []
        self.prologue = {}

    def _mk(self, eng, fn, reads, writes, deps, is_dma, bar=True):
        o = Op()
        o.eng = eng
        o.fn = fn
        o.is_dma = is_dma
        o.sem = None
        o.val = None
        o.needed = is_dma
        o.epoch = self.epoch
        d = [x for x in deps if x is not None]
        for r in reads:
            if r.w is not None:
                d.append(r.w)
        for w in writes:
            if w.w is not None:
                d.append(w.w)
            d.extend(w.r.values())
            d.extend(w.rd)
        pb = self.pending.pop(eng, None)
        if pb:
            d.extend(pb)
        o.deps = d
        for r in reads:
            if is_dma:
                r.rd.append(o)
            else:
                r.r[eng] = o
        for w in writes:
            w.w = o
            w.r = {}
            w.rd = []
        self.q[eng].append(o)
        if is_dma:
            if bar:
                self.dma_since_barrier.append(o)
        else:
            self.last_compute[eng] = o
        return o

    def op(self, eng, fn, reads=(), writes=(), deps=()):
        return self._mk(eng, fn, reads, writes, deps, False)

    def dma(self, eng, fn, reads=(), writes=(), deps=(), bar=True):
        return self._mk(eng, fn, reads, writes, deps, True, bar)

    def barrier(self):
        lst = list(self.last_compute.values()) + list(self.dma_since_barrier)
        self.dma_since_barrier = []
        for e in ENGS:
            self.pending[e] = list(lst) + self.pending.get(e, [])

    def finalize(self, final_ops):
        nc = self.nc
        for e in ENGS:
            for o in self.q[e]:
                for d in o.deps:
                    d.needed = True
        for o in final_ops:
            o.needed = True
        n_epochs = self.epoch + 1
        with contextlib.ExitStack() as stack:
            sems = {}
            for e in ENGS:
                for ep in range(n_epochs):
                    sems[(e, ep)] = stack.enter_context(nc.semaphore(f"s_{e}{ep}"))
            dsems = {}
            for e in ("sp", "act", "pool"):
                for i in range(self.n_dma_sems):
                    dsems[(e, i)] = stack.enter_context(nc.semaphore(f"d_{e}{i}"))
            cnt = {}
            slot_next = {e: 0 for e in ENGS}
            slot_cnt = {}
            slot_last = {}
            for e in ENGS:
                for o in self.q[e]:
                    if o.is_dma:
                        s = slot_next[e]
                        slot_next[e] = (s + 1) % self.n_dma_sems
                        key = (e, s)
                        prev = slot_last.get(key)
                        if prev is not None:
                            o.deps.append(prev)
                        slot_cnt[key] = slot_cnt.get(key, 0) + 16
                        slot_last[key] = o
                        o.sem = dsems[key]
                        o.val = slot_cnt[key]
                    elif o.needed:
                        k = (e, o.epoch)
                        cnt[k] = cnt.get(k, 0) + 1
                        o.sem = sems[k]
                        o.val = cnt[k]
            self.max_counts = cnt
            block = stack.enter_context(nc.Block())
            engmap = {"pe": block.tensor, "act": block.scalar, "dve": block.vector,
                      "pool": block.gpsimd, "sp": block.sync}

            def make(e):
                def body(eng):
                    seen = {}
                    for pf in self.prologue.get(e, ()):
                        pf(eng)
                    for o in self.q[e]:
                        for d in o.deps:
                            if (not d.is_dma) and d.eng == e and e == "pe":
                                continue
                            k = id(d.sem)
                            if seen.get(k, 0) >= d.val:
                                continue
                            seen[k] = d.val
                            eng.wait_ge(d.sem, d.val)
                        inst = o.fn(eng)
                        if o.sem is not None:
                            inst.then_inc(o.sem, 16 if o.is_dma else 1)
                    if e == "sp":
                        for o in final_ops:
                            if seen.get(id(o.sem), 0) >= o.val:
                                continue
                            seen[id(o.sem)] = o.val
                            eng.wait_ge(o.sem, o.val)
                return body

            for e in ENGS:
                engmap[e](make(e))


def build_program(NL, first_layer_is_input=True):
    nc = bass.Bass("TRN2", target_bir_lowering=False)
    dt_in = lambda n, s, d=F32: nc.dram_tensor(n, s, d, kind="ExternalInput")
    x_in = dt_in("x_in", [T, D])
    mem_in = dt_in("mem", [MEM, D])
    memln = dt_in("memln", [1, 2 * D])
    cst_in = dt_in("cst", [128, NCST])
    w_in = dt_in("w_in", [NL, D, IN_COLS])
    pool_w = dt_in("pool_w", [NL, 4, 128, 128])
    pool_proj = dt_in("pool_proj", [NL, 512, D])
    conv_pw = dt_in("conv_pw", [NL, 512, D])
    attn_o = dt_in("attn_o", [NL, 512, D])
    w_kv = dt_in("w_kv", [NL, D, D])
    w_out = dt_in("w_out", [NL, D, D])
    exp_up = dt_in("exp_up", [NL, NE, D, 2 * D])
    exp_down = dt_in("exp_down", [NL, NE, D, D])
    router_w = dt_in("router_w", [NL, D, NE])
    exp_down_b = dt_in("exp_down_b", [NL, NE, D])
    colp_in = dt_in("colp", [NL, 128, NCOLP])
    rowbc_in = dt_in("rowbc", [NL, 1, 4 * D])
    rowsm_in = dt_in("rowsm", [NL, 1, D + NE])
    out_d = nc.dram_tensor("out", [T - 128, D], F32, kind="ExternalOutput")
    xres = nc.dram_tensor("xres", [T, D], F32, kind="Internal")
    xd = nc.dram_tensor("xd", [NSLOT, D], BF16, kind="Internal")
    ys = nc.dram_tensor("ys", [NSLOT, D], BF16, kind="Internal")

    with contextlib.ExitStack() as st:
        sb = lambda n, s, d: st.enter_context(nc.sbuf_tensor("sb_" + n, s, d))
        S = Sched(nc)

        xT = sb("xT", [128, 8, T], BF16)
        mergedT = sb("mergedT", [128, 8, T], BF16)
        brT = sb("brT", [128, 4, T], BF16)
        ring = [sb(f"ring{i}", [128, 4096], BF16) for i in range(NW)]
        cst = sb("cst", [128, NCST], F32)
        cbf = sb("cbf", [128, 384], BF16)
        colp = sb("colp", [128, NCOLP], F32)
        upb1 = sb("upb1", [128, NE * 8], F32)
        lnbc = sb("lnbc", [128, 4 * D], F32)
        rowsm = sb("rowsm", [1, D + NE], F32)
        wr = sb("wr", [128, 8, NE], F32)
        bdown = sb("bdown", [NE, D], F32)
        memT = sb("memT", [128, 8, MEM], BF16)
        kT = sb("kT", [128, 4, MEM], BF16)
        vv = sb("vv", [128, 2, 512], BF16)
        poolw = sb("poolw", [128, 4, 128], BF16)
        idx_all = sb("idx_all", [128, NT * 4], I32)
        gate_all = sb("gate_all", [128, NT * 4], F32)
        G_all = sb("G_all", [128, NT, NE], F32)
        ARENA_BF = 24064
        arena = sb("arena", [128, ARENA_BF], BF16)
        ps = [st.enter_context(nc.psum_tensor(f"ps{i}", [128, 1024], F32)) for i in range(4)]

        epsc = sb("epsc", [128, 1], F32)
        r_eps = Reg()
        ident_bf = cbf[:, 0:128]
        tri_bf = cbf[:, 128:256]
        ones_bf = cbf[:, 256:384]
        ident_f = cst[:, K_ID:K_ID + 128]
        onesrow_f = cst[0:1, K_ONE:K_ONE + 128]
        vmask = cst[:, K_VM:K_VM + 128]
        vtok = cst[:, K_VT:K_VT + 1]
        base1 = cst[:, K_B1:K_B1 + 32]

        R = lambda: Reg()
        r_xT = [R() for _ in range(NT)]
        r_mg = [R() for _ in range(NT)]
        r_br = [[R() for _ in CBS] for _ in range(4)]
        r_ring = [R() for _ in range(NW)]
        r_cst, r_cbf, r_colp, r_upb1, r_lnbc, r_rowsm, r_wr, r_bdown = (R() for _ in range(8))
        r_memT, r_kT, r_vv, r_poolw = R(), R(), R(), R()
        r_idx = [R() for _ in range(NT)]
        r_gate = [R() for _ in range(NT)]
        r_G = [R() for _ in range(NT)]
        r_bank = [R() for _ in range(8)]

        def bank(b):
            return ps[b // 2][:, (b % 2) * 512:(b % 2) * 512 + 512]

        def bank_bf(b):
            return ps[b // 2][:, (b % 2) * 512:(b % 2) * 512 + 512].bitcast(BF16)

        bank_rr = [0]

        def next_bank():
            b = bank_rr[0]
            bank_rr[0] = (b + 1) % 8
            return b

        def next_bank2():
            b = bank_rr[0]
            if b % 2:
                b = (b + 1) % 8
            bank_rr[0] = (b + 2) % 8
            return b

        class Arena:
            def __init__(self):
                self.off = 0

            def reset(self):
                S.barrier()
                self.off = 0

            def alloc(self, shape, dtype):
                n = 1
                for s_ in shape[1:]:
                    n *= s_
                nb = n * (2 if dtype == BF16 else 4)
                self.off = (self.off + 63) // 64 * 64
                o_bf = self.off // 2
                self.off += nb
                assert self.off <= ARENA_BF * 2, f"arena overflow {self.off}"
                v = arena[0:shape[0], o_bf:o_bf + nb // 2]
                if dtype != BF16:
                    v = v.bitcast(dtype)
                if len(shape) == 3:
                    v = v.rearrange("p (a b) -> p a b", a=shape[1])
                return v

        AR = Arena()
        regs = {}
        S.prologue["pool"] = [lambda e: regs.__setitem__("bc", e.to_reg(NSLOT - 1))]

        def mm(out, lhsT, rhs, start, stop, reads, writes):
            return S.op("pe", lambda e: e.matmul(out, lhsT=lhsT, rhs=rhs, start=start, stop=stop),
                        reads, writes)

        def tr(out, in_, ident, reads, writes):
            return S.op("pe", lambda e: e.transpose(out=out, in_=in_, identity=ident), reads, writes)

        def act(out, in_, func, reads, writes, bias=None, scale=None, eng="act"):
            kw = {}
            if bias is not None:
                kw["bias"] = bias
            if scale is not None:
                kw["scale"] = scale
            return S.op(eng, lambda e: e.activation(out=out, in_=in_, func=func, **kw), reads, writes)

        def ts(eng, out, in0, s1, s2, op0, op1, reads, writes, accum_out=None):
            kw = {}
            if op1 is not None:
                kw["op1"] = op1
            if accum_out is not None:
                kw["accum_out"] = accum_out
            return S.op(eng, lambda e: e.tensor_scalar(out=out, in0=in0, scalar1=s1, scalar2=s2,
                                                       op0=op0, **kw), reads, writes)

        def tt(eng, out, in0, in1, op, reads, writes):
            return S.op(eng, lambda e: e.tensor_tensor(out=out, in0=in0, in1=in1, op=op), reads, writes)

        def stt(eng, out, in0, scalar, in1, op0, op1, reads, writes, accum_out=None):
            kw = {}
            if accum_out is not None:
                kw["accum_out"] = accum_out
            return S.op(eng, lambda e: e.scalar_tensor_tensor(out=out, in0=in0, scalar=scalar, in1=in1,
                                                              op0=op0, op1=op1, **kw), reads, writes)

        def cp(eng, out, in_, reads, writes):
            if eng == "act":
                return S.op(eng, lambda e: e.activation(out=out, in_=in_, func=AF.Copy), reads, writes)
            return S.op(eng, lambda e: e.tensor_copy(out=out, in_=in_), reads, writes)

        def memset(eng, ap, val, writes):
            return S.op(eng, lambda e: e.memset(ap, val), (), writes)

        def dma(eng, out, in_, reads, writes, deps=(), bar=True):
            return S.dma(eng, lambda e: e.dma_start(out=out, in_=in_), reads, writes, deps, bar)

        memset("dve", epsc[:, :], LN_EPS, [r_eps])

        plan = []
        kinds = []

        def kcview(ap2d):
            return ap2d.rearrange("(kc p) f -> p kc f", p=128)

        for l in range(NL):
            plan.append(kcview(w_kv[l][:, 0:512]))
            plan.append(kcview(w_kv[l][:, 512:1024]))
            plan.append(kcview(w_in[l][:, 0:512]))
            plan.append(kcview(pool_proj[l]))
            plan.append(kcview(w_in[l][:, 2048:2560]))
            plan.append(kcview(w_in[l][:, 2560:3072]))
            plan.append(kcview(w_in[l][:, 512:1024]))
            plan.append(kcview(w_in[l][:, 1024:1536]))
            plan.append(kcview(conv_pw[l]))
            plan.append(kcview(w_in[l][:, 3072:3584]))
            plan.append(kcview(w_in[l][:, 3584:4096]))
            plan.append(kcview(w_in[l][:, 1536:2048]))
            plan.append(kcview(attn_o[l]))
            plan.append(kcview(w_in[l][:, 4096:4608]))
            plan.append(kcview(w_in[l][:, 4608:5120]))
            plan.append(kcview(w_out[l][:, 0:512]))
            plan.append(kcview(w_out[l][:, 512:1024]))
            kinds.extend(["m"] * 17)
            def _up(e, hf):
                kinds.extend(["e"] * 2)
                plan.append(kcview(exp_up[l, e][:, hf * 512:(hf + 1) * 512]))
                plan.append(kcview(exp_up[l, e][:, 1024 + hf * 512:1024 + (hf + 1) * 512]))

            def _down(e):
                kinds.extend(["e"] * 2)
                plan.append(kcview(exp_down[l, e][:, 0:512]))
                plan.append(kcview(exp_down[l, e][:, 512:1024]))

            _up(0, 0)
            _up(0, 1)
            for e in range(NE):
                if e + 1 < NE:
                    _up(e + 1, 0)
                _down(e)
                if e + 1 < NE:
                    _up(e + 1, 1)

        NWE = NW + 8
        xTflat = xT[:, :, :].rearrange("p a b -> p (a b)")
        mgflat = mergedT[:, :, :].rearrange("p a b -> p (a b)")
        ringv = [ring[i][:, :] for i in range(NW)]
        ringv += [xTflat[:, q_ * 4096:(q_ + 1) * 4096] for q_ in range(4)]
        ringv += [mgflat[:, q_ * 4096:(q_ + 1) * 4096] for q_ in range(4)]
        r_ring.extend(Reg() for _ in range(8))

        class Ring:
            def __init__(self):
                self.next_load = 0
                self.next_acq = 0
                self.released = [False] * len(plan)
                self.allow_alias = 0
                self.slot = []
                self.prev = []
                last = {}
                bc = ec = 0
                for n, k_ in enumerate(kinds):
                    if k_ == "m":
                        sl = bc % NW
                        bc += 1
                    else:
                        sl = ec % NWE
                        ec += 1
                    self.slot.append(sl)
                    self.prev.append(last.get(sl))
                    last[sl] = n

            def _try_load(self):
                while self.next_load < len(plan):
                    n = self.next_load
                    pv = self.prev[n]
                    if pv is not None and not self.released[pv]:
                        break
                    sl = self.slot[n]
                    if sl >= NW + 4 and self.allow_alias < 2:
                        break
                    if sl >= NW and self.allow_alias < 1:
                        break
                    src = plan[n]
                    a = src.shape[1]
                    dst = ringv[sl].rearrange("p (a b) -> p a b", a=a)
                    dma("pool", dst, src, (), [r_ring[sl]], bar=False)
                    self.next_load += 1

            def acquire(self, a):
                self._try_load()
                n = self.next_acq
                assert n < self.next_load, "ring: item not loaded (release order problem)"
                assert plan[n].shape[1] == a
                self.next_acq += 1
                sl = self.slot[n]
                v = ringv[sl].rearrange("p (a b) -> p a b", a=a)
                return n, v, r_ring[sl]

            def release(self, n):
                self.released[n] = True
                self._try_load()

            def set_alias(self, level):
                self.allow_alias = level
                if level:
                    self._try_load()

        RG = Ring()

        dma("sp", cst[:, :], cst_in[:, :], (), [r_cst])
        cp("dve", cbf[:, :], cst[:, 0:384], [r_cst], [r_cbf])

        def ln_tmp():
            return (AR.alloc([128, 12], F32), AR.alloc([128, 2], F32), AR.alloc([128, 1], F32),
                    AR.alloc([128, 1], F32), R(), R(), R(), R())

        def ln_stats(zt, r_z, tmp_):
            stt_, mv, rs, nmr, r_s, r_mv, r_rs, r_nmr = tmp_
            S.op("dve", lambda e: e.bn_stats(out=stt_[:, 0:6], in_=zt[:, 0:512]), [r_z], [r_s])
            S.op("dve", lambda e: e.bn_stats(out=stt_[:, 6:12], in_=zt[:, 512:1024]), [r_z], [r_s])
            S.op("dve", lambda e: e.bn_aggr(out=mv, in_=stt_), [r_s], [r_mv])
            act(rs, mv[:, 1:2], AF.Sqrt, [r_mv, r_eps], [r_rs], bias=epsc[:, 0:1])

        def ln_norm(zt, r_z, tmp_):
            stt_, mv, rs, nmr, r_s, r_mv, r_rs, r_nmr = tmp_
            S.op("dve", lambda e: e.reciprocal(out=rs, in_=rs), [r_rs], [r_rs])
            ts("dve", nmr, mv[:, 0:1], rs[:, 0:1], -1.0, ALU.mult, ALU.mult, [r_mv, r_rs], [r_nmr])
            act(zt, zt, AF.Identity, [r_z, r_rs, r_nmr], [r_z], bias=nmr[:, 0:1], scale=rs[:, 0:1])

        def ln_affine(zt, r_z, gv, bv, r_gb):
            tt("dve", zt, zt, gv, ALU.mult, [r_z, r_gb], [r_z])
            tt("dve", zt, zt, bv, ALU.add, [r_z, r_gb], [r_z])

        def ln_apply(zt, r_z, gv, bv, r_gb, tmp_):
            ln_norm(zt, r_z, tmp_)
            ln_affine(zt, r_z, gv, bv, r_gb)

        def ln_rows(zt, r_z, out32, r_out, gv, bv, r_gb, tmp_):
            ln_stats(zt, r_z, tmp_)
            ln_apply(zt, r_z, gv, bv, r_gb, tmp_)

        def to_xT(src_bf, r_src, i):
            b = next_bank()
            pb = bank_bf(b)
            for kc in range(8):
                tr(pb[:, kc * 128:(kc + 1) * 128], src_bf[:, kc * 128:(kc + 1) * 128], ident_bf,
                   [r_src, r_cbf], [r_bank[b]])
            cp("act" if i % 2 else "dve", xT[:, :, i * 128:(i + 1) * 128],
               pb.rearrange("p (a b) -> p a b", a=8), [r_bank[b]], [r_xT[i]])

        AR.reset()
        mg_bc = AR.alloc([128, 2 * D], F32)
        r_mgbc = R()
        lnt = [ln_tmp(), ln_tmp()]
        zer = AR.alloc([128, 8 * D], BF16)
        r_zer = R()
        memset("pool", zer, 0.0, [r_zer])
        for j_ in range(NE * CAP // 1024):
            dma("sp", xd[1 + j_ * 1024:1 + (j_ + 1) * 1024, :].rearrange("(p r) d -> p r d", p=128),
                zer.rearrange("p (r d) -> p r d", r=8), [r_zer], ())
        dma("sp", xd[0:1, :], zer[0:1, 0:D], [r_zer], ())
        dma("sp", ys[0:1, :], zer[0:1, 0:D], [r_zer], ())
        dma("sp", mg_bc, memln[0:1, :].to_broadcast([128, 2 * D]), (), [r_mgbc])
        for mt in range(2):
            mtile = AR.alloc([128, D], F32)
            mbf = AR.alloc([128, D], BF16)
            r_mt, r_mb = R(), R()
            dma("sp", mtile, mem_in[mt * 128:(mt + 1) * 128, :], (), [r_mt])
            ln_rows(mtile, r_mt, mtile, r_mt, mg_bc[:, 0:D], mg_bc[:, D:2 * D], r_mgbc, lnt[mt])
            cp("act", mbf, mtile, [r_mt], [r_mb])
            b = next_bank()
            pb = bank_bf(b)
            for kc in range(8):
                tr(pb[:, kc * 128:(kc + 1) * 128], mbf[:, kc * 128:(kc + 1) * 128], ident_bf,
                   [r_mb, r_cbf], [r_bank[b]])
            cp("dve", memT[:, :, mt * 128:(mt + 1) * 128], pb.rearrange("p (a b) -> p a b", a=8),
               [r_bank[b]], [r_memT])

        AR.reset()
        xl = [AR.alloc([128, D], F32) for _ in range(2)]
        xb = [AR.alloc([128, D], BF16) for _ in range(2)]
        r_xl = [R(), R()]
        r_xb = [R(), R()]
        for i in range(NT):
            k = i % 2
            dma("sp", xl[k], x_in[i * 128:(i + 1) * 128, :], (), [r_xl[k]])
            cp("act" if i % 2 == 0 else "dve", xb[k], xl[k], [r_xl[k]], [r_xb[k]])
            to_xT(xb[k], r_xb[k], i)

        res_src = {i: (x_in, None) for i in range(NT)}
        final_ops = []

        for l in range(NL):
            last = (l == NL - 1)
            S.epoch += 1
            AR.reset()
            dma("sp", colp[:, :], colp_in[l], (), [r_colp])
            dma("sp", lnbc[:, :], rowbc_in[l].to_broadcast([128, 4 * D]), (), [r_lnbc])
            dma("sp", rowsm[:, :], rowsm_in[l], (), [r_rowsm])
            dma("sp", wr[:, :, :], router_w[l].rearrange("(kc p) e -> p kc e", p=128), (), [r_wr])
            dma("sp", bdown[:, :], exp_down_b[l], (), [r_bdown])
            dma("pool", poolw[:, :, :], pool_w[l].rearrange("g c d -> c g d"), (), [r_poolw])
            ts("dve", upb1[:, :].rearrange("p (e j) -> p e j", e=NE),
               colp[:, C_UPB:C_UPB + 512].rearrange("p (e j) -> p e j", e=NE)[:, :, 8:16],
               1.0, None, ALU.add, None, [r_colp], [r_upb1])

            def bcol(c):
                return colp[:, c:c + 1]

            n_k, Wk, r_Wk = RG.acquire(8)
            for hd in range(4):
                b = next_bank()
                for kc in range(8):
                    mm(bank(b)[:, 0:MEM], Wk[:, kc, hd * 128:(hd + 1) * 128], memT[:, kc, :],
                       kc == 0, kc == 7, [r_Wk, r_memT], [r_bank[b]])
                cp("act", kT[:, hd, :], bank(b)[:, 0:MEM], [r_bank[b]], [r_kT])
            RG.release(n_k)
            n_v, Wv, r_Wv = RG.acquire(8)
            for mc in range(2):
                b = next_bank()
                for kc in range(8):
                    mm(bank(b), memT[:, kc, mc * 128:(mc + 1) * 128], Wv[:, kc, :],
                       kc == 0, kc == 7, [r_Wv, r_memT], [r_bank[b]])
                cp("act", vv[:, mc, :], bank(b), [r_bank[b]], [r_vv])
            RG.release(n_v)

            def xT_regs(cbi):
                c0, n = CBS[cbi]
                return [r_xT[i] for i in range(c0 // 128, (c0 + n) // 128)]

            def mg_regs(cbi):
                c0, n = CBS[cbi]
                return [r_mg[i] for i in range(c0 // 128, (c0 + n) // 128)]

            def gate_pass(br, P, r_P, bias_cols=None):
                sg = [AR.alloc([128, 512], F32) for _ in range(2)]
                tmp = [AR.alloc([128, 512], F32) for _ in range(2)]
                r_sg = [R(), R()]
                r_tmp = [R(), R()]
                it = 0
                for half in range(2):
                    n_g, G, r_Gs = RG.acquire(8)
                    for jj in range(4):
                        j = half * 4 + jj
                        for cbi, (c0, n) in enumerate(CBS):
                            bg = next_bank()
                            for kc in range(8):
                                mm(bank(bg)[:, 0:n], G[:, kc, jj * 128:(jj + 1) * 128], xT[:, kc, c0:c0 + n],
                                   kc == 0, kc == 7, [r_Gs] + xT_regs(cbi), [r_bank[bg]])
                            by = next_bank()
                            for kc in range(4):
                                mm(bank(by)[:, 0:n], P[:, kc, j * 128:(j + 1) * 128], brT[:, kc, c0:c0 + n],
                                   kc == 0, kc == 3, [r_P] + [r_br[kc][cbi]], [r_bank[by]])
                            k = it % 2
                            it += 1
                            act(sg[k][:, 0:n], bank(bg)[:, 0:n], AF.Sigmoid, [r_bank[bg], r_colp], [r_sg[k]],
                                bias=bcol(C_BIN + 16 + br * 8 + j))
                            mdst = mergedT[:, j, c0:c0 + n]
                            if br == 0:
                                tt("dve", mdst, sg[k][:, 0:n], bank(by)[:, 0:n], ALU.mult,
                                   [r_sg[k], r_bank[by]], mg_regs(cbi))
                            else:
                                if bias_cols is not None:
                                    stt("dve", tmp[k][:, 0:n], bank(by)[:, 0:n], bcol(bias_cols + j), sg[k][:, 0:n],
                                        ALU.add, ALU.mult, [r_bank[by], r_sg[k], r_colp], [r_tmp[k]])
                                else:
                                    tt("dve", tmp[k][:, 0:n], sg[k][:, 0:n], bank(by)[:, 0:n], ALU.mult,
                                       [r_sg[k], r_bank[by]], [r_tmp[k]])
                                tt("dve", mdst, mdst, tmp[k][:, 0:n], ALU.add, [r_tmp[k]] + mg_regs(cbi),
                                   mg_regs(cbi))
                    RG.release(n_g)

            AR.reset()
            n_wp, Wp, r_Wp = RG.acquire(8)
            u = AR.alloc([128, 16 + T], F32)
            sA = AR.alloc([128, 16 + T], F32)
            sB = AR.alloc([128, 16 + T], F32)
            pooled = AR.alloc([128, T], BF16)
            r_u, r_sA, r_sB, r_pl = R(), R(), R(), R()
            memset("pool", u[:, 0:16], 0.0, [r_u])
            memset("pool", sA[:, 0:16], 0.0, [r_sA])
            memset("pool", sB[:, 0:16], 0.0, [r_sB])
            for g in range(4):
                for cbi, (c0, n) in enumerate(CBS):
                    b = next_bank()
                    for kc in range(8):
                        mm(bank(b)[:, 0:n], Wp[:, kc, g * 128:(g + 1) * 128], xT[:, kc, c0:c0 + n],
                           kc == 0, kc == 7, [r_Wp] + xT_regs(cbi), [r_bank[b]])
                    act(u[:, 16 + c0:16 + c0 + n], bank(b)[:, 0:n], AF.Identity, [r_bank[b], r_colp], [r_u],
                        bias=bcol(C_BIN + g))
                tt("dve", u[:, 16:144], u[:, 16:144], vmask, ALU.mult, [r_u, r_cst], [r_u])
                src, r_src = u, r_u
                bufs = [(sA, r_sA), (sB, r_sB)]
                sh = 1
                for step in range(g + 1):
                    dst, r_dst = bufs[step % 2]
                    tt("dve", dst[:, 16:16 + T], src[:, 16:16 + T], src[:, 16 - sh:16 + T - sh], ALU.add,
                       [r_src], [r_dst])
                    src, r_src = dst, r_dst
                    sh *= 2
                w_ = 2 ** (g + 1)
                tt("dve", src[:, 16 + 128:16 + 144], src[:, 16 + 128:16 + 144],
                   cst[:, K_PF + g * 16:K_PF + (g + 1) * 16], ALU.mult, [r_src, r_cst], [r_src])
                stt("dve", pooled[:, :], src[:, 16:16 + T], 1.0 / w_, u[:, 16:16 + T], ALU.mult, ALU.subtract,
                    [r_src, r_u], [r_pl])
                for cbi, (c0, n) in enumerate(CBS):
                    b = next_bank()
                    mm(bank(b)[:, 0:n], poolw[:, g, :], pooled[:, c0:c0 + n], True, True,
                       [r_poolw, r_pl], [r_bank[b]])
                    act(brT[:, g, c0:c0 + n], bank(b)[:, 0:n], AF.Identity, [r_bank[b], r_colp], [r_br[g][cbi]],
                        scale=bcol(C_PSC + g))
            RG.release(n_wp)
            n_pp, Pp, r_Pp = RG.acquire(4)
            gate_pass(0, Pp, r_Pp)
            RG.release(n_pp)

            AR.reset()
            n_wa, Wa, r_Wa = RG.acquire(8)
            n_wg, Wg, r_Wg = RG.acquire(8)
            diag = AR.alloc([128, 31, 128], BF16)
            hb = [AR.alloc([128, 32 + T], BF16) for _ in range(2)]
            sgc = [AR.alloc([128, 512], F32) for _ in range(2)]
            r_diag = R()
            r_hb = [R(), R()]
            r_sgc = [R(), R()]
            memset("pool", hb[0][:, 0:32], 0.0, [r_hb[0]])
            memset("pool", hb[1][:, 0:32], 0.0, [r_hb[1]])
            it = 0
            for c in range(4):
                h_, r_h = hb[c % 2], r_hb[c % 2]
                for k in range(31):
                    ts("dve", diag[:, k, :], ident_bf, bcol(C_CDW + c * 31 + k), None,
                       ALU.mult, None, [r_cbf, r_colp], [r_diag])
                for cbi, (c0, n) in enumerate(CBS):
                    ba = next_bank()
                    for kc in range(8):
                        mm(bank(ba)[:, 0:n], Wa[:, kc, c * 128:(c + 1) * 128], xT[:, kc, c0:c0 + n],
                           kc == 0, kc == 7, [r_Wa] + xT_regs(cbi), [r_bank[ba]])
                    bg = next_bank()
                    for kc in range(8):
                        mm(bank(bg)[:, 0:n], Wg[:, kc, c * 128:(c + 1) * 128], xT[:, kc, c0:c0 + n],
                           kc == 0, kc == 7, [r_Wg] + xT_regs(cbi), [r_bank[bg]])
                    k = it % 2
                    it += 1
                    act(sgc[k][:, 0:n], bank(bg)[:, 0:n], AF.Sigmoid, [r_bank[bg], r_colp], [r_sgc[k]],
                        bias=bcol(C_BIN + 8 + c))
                    stt("dve", h_[:, 32 + c0:32 + c0 + n], bank(ba)[:, 0:n], bcol(C_BIN + 4 + c), sgc[k][:, 0:n],
                        ALU.add, ALU.mult, [r_bank[ba], r_sgc[k], r_colp], [r_h])
                tt("dve", h_[:, 32:160], h_[:, 32:160], vmask, ALU.mult, [r_h, r_cst], [r_h])
                for cbi, (c0, n) in enumerate(CBS):
                    b = next_bank()
                    for k in range(31):
                        mm(bank(b)[:, 0:n], diag[:, k, :], h_[:, 32 + c0 - 30 + k:32 + c0 - 30 + k + n],
                           k == 0, k == 30, [r_diag, r_h], [r_bank[b]])
                    act(brT[:, c, c0:c0 + n], bank(b)[:, 0:n], AF.Identity, [r_bank[b], r_colp], [r_br[c][cbi]],
                        bias=bcol(C_CDWB + c))
            RG.release(n_wa)
            RG.release(n_wg)
            sq = AR.alloc([128, 4, 512], BF16)
            mean = AR.alloc([128, 512], F32)
            msq = AR.alloc([128, 512], F32)
            rstd = AR.alloc([128, 512], F32)
            zc = [AR.alloc([128, 512], F32) for _ in range(2)]
            r_sq, r_mean, r_msq, r_rstd = R(), R(), R(), R()
            r_zc = [R(), R()]
            for cbi, (c0, n) in enumerate(CBS):
                for c in range(4):
                    act(sq[:, c, 0:n], brT[:, c, c0:c0 + n], AF.Square, [r_br[c][cbi]], [r_sq])
                b1 = next_bank()
                for c in range(4):
                    mm(bank(b1)[:, 0:n], ones_bf, brT[:, c, c0:c0 + n], c == 0, c == 3,
                       [r_cbf, r_br[c][cbi]], [r_bank[b1]])
                b2 = next_bank()
                for c in range(4):
                    mm(bank(b2)[:, 0:n], ones_bf, sq[:, c, 0:n], c == 0, c == 3, [r_cbf, r_sq], [r_bank[b2]])
                ts("dve", mean[:, 0:n], bank(b1)[:, 0:n], 1.0 / 512, None, ALU.mult, None, [r_bank[b1]], [r_mean])
                tt("dve", msq[:, 0:n], mean[:, 0:n], mean[:, 0:n], ALU.mult, [r_mean], [r_msq])
                stt("dve", rstd[:, 0:n], bank(b2)[:, 0:n], 1.0 / 512, msq[:, 0:n], ALU.mult, ALU.subtract,
                    [r_bank[b2], r_msq], [r_rstd])
                ts("dve", rstd[:, 0:n], rstd[:, 0:n], 0.0, None, ALU.max, None, [r_rstd], [r_rstd])
                act(rstd[:, 0:n], rstd[:, 0:n], AF.Ln, [r_rstd, r_eps], [r_rstd], bias=epsc[:, 0:1])
                act(rstd[:, 0:n], rstd[:, 0:n], AF.Exp, [r_rstd], [r_rstd], scale=-0.5)
                for c in range(4):
                    k = c % 2
                    tt("dve", zc[k][:, 0:n], brT[:, c, c0:c0 + n], mean[:, 0:n], ALU.subtract,
                       [r_br[c][cbi], r_mean], [r_zc[k]])
                    tt("dve", zc[k][:, 0:n], zc[k][:, 0:n], rstd[:, 0:n], ALU.mult, [r_zc[k], r_rstd], [r_zc[k]])
                    act(brT[:, c, c0:c0 + n], zc[k][:, 0:n], AF.Silu, [r_zc[k], r_colp], [r_br[c][cbi]],
                        bias=bcol(C_CLB + c), scale=bcol(C_CLG + c))
            n_cp, Pc, r_Pc = RG.acquire(4)
            gate_pass(1, Pc, r_Pc, bias_cols=C_CPWB)
            RG.release(n_cp)

            AR.reset()
            n_wq, Wq, r_Wq = RG.acquire(8)
            qb = [AR.alloc([128, 512], BF16) for _ in range(2)]
            eb = [AR.alloc([128, 2, 512], BF16) for _ in range(2)]
            rden = [AR.alloc([128, 512], F32) for _ in range(2)]
            r_qb = [R(), R()]
            r_eb = [R(), R()]
            r_rden = [R(), R()]
            items = [(hd, cbi) for hd in range(4) for cbi in range(len(CBS))]

            def at_S1(t):
                hd, cbi = items[t]
                c0, n = CBS[cbi]
                k = t % 2
                b = next_bank()
                for kc in range(8):
                    mm(bank(b)[:, 0:n], Wq[:, kc, hd * 128:(hd + 1) * 128], xT[:, kc, c0:c0 + n],
                       kc == 0, kc == 7, [r_Wq] + xT_regs(cbi), [r_bank[b]])
                act(qb[k][:, 0:n], bank(b)[:, 0:n], AF.Identity, [r_bank[b], r_colp], [r_qb[k]],
                    bias=bcol(C_BIN + 12 + hd))

            def at_S2(t):
                hd, cbi = items[t]
                c0, n = CBS[cbi]
                k = t % 2
                for mc in range(2):
                    bs = next_bank()
                    mm(bank(bs)[:, 0:n], kT[:, hd, mc * 128:(mc + 1) * 128], qb[k][:, 0:n], True, True,
                       [r_kT, r_qb[k]], [r_bank[bs]])
                    act(eb[k][:, mc, 0:n], bank(bs)[:, 0:n], AF.Exp, [r_bank[bs]], [r_eb[k]], scale=ATT_SCALE)

            def at_S3(t):
                hd, cbi = items[t]
                c0, n = CBS[cbi]
                k = t % 2
                bo = next_bank()
                for mc in range(2):
                    mm(bank(bo)[:, 0:n], vv[:, mc, hd * 128:(hd + 1) * 128], eb[k][:, mc, 0:n],
                       mc == 0, mc == 1, [r_vv, r_eb[k]], [r_bank[bo]])
                bd = next_bank()
                for mc in range(2):
                    mm(bank(bd)[:, 0:n], ones_bf, eb[k][:, mc, 0:n], mc == 0, mc == 1,
                       [r_cbf, r_eb[k]], [r_bank[bd]])
                act(rden[k][:, 0:n], bank(bd)[:, 0:n], AF.Ln, [r_bank[bd]], [r_rden[k]])
                act(rden[k][:, 0:n], rden[k][:, 0:n], AF.Exp, [r_rden[k]], [r_rden[k]], scale=-1.0)
                tt("dve", brT[:, hd, c0:c0 + n], bank(bo)[:, 0:n], rden[k][:, 0:n], ALU.mult,
                   [r_bank[bo], r_rden[k]], [r_br[hd][cbi]])

            NI = len(items)
            for step in range(NI + 2):
                if step < NI:
                    at_S1(step)
                if 0 <= step - 1 < NI:
                    at_S2(step - 1)
                if 0 <= step - 2 < NI:
                    at_S3(step - 2)
            RG.release(n_wq)
            n_ao, Pa, r_Pa = RG.acquire(4)
            gate_pass(2, Pa, r_Pa)
            RG.release(n_ao)

            AR.reset()
            RG.set_alias(1)
            n_o0, Wo0, r_Wo0 = RG.acquire(8)
            n_o1, Wo1, r_Wo1 = RG.acquire(8)
            Wo = [(Wo0, r_Wo0), (Wo1, r_Wo1)]
            xr = [AR.alloc([128, D], F32) for _ in range(2)]
            zt = [AR.alloc([128, D], F32) for _ in range(2)]
            x1b = [AR.alloc([128, D], BF16) for _ in range(2)]
            x1T = AR.alloc([128, 8, 128], F32)
            lnt = [ln_tmp(), ln_tmp()]
            lg = AR.alloc([128, NE], F32)
            m8 = AR.alloc([128, 8], F32)
            nm = AR.alloc([128, 1], F32)
            Af = AR.alloc([128, NE], F32)
            ex = AR.alloc([128, NE], F32)
            ssum = AR.alloc([128, 1], F32)
            Abf = AR.alloc([128, NE], BF16)
            Asum = [AR.alloc([128, NE], BF16) for _ in range(2)]
            key = AR.alloc([128, NE], F32)
            k8 = AR.alloc([128, 8], F32)
            junk = AR.alloc([128, NE], F32)
            r_xr = [R(), R()]
            r_zt = [R(), R()]
            r_x1b = [R(), R()]
            r_x1T, r_lg, r_m8, r_nm, r_Af, r_ex, r_ss, r_Abf, r_key, r_k8, r_junk = (R() for _ in range(11))
            r_As = [R(), R()]
            scat_ops = []
            new_res = {}
            lg_all = AR.alloc([128, NT, NE], F32)
            r_lga = [R() for _ in range(NT)]

            x1b3 = [x1b[0], x1b[1], AR.alloc([128, D], BF16), AR.alloc([128, D], BF16)]
            r_x1b3 = [r_x1b[0], r_x1b[1], R(), R()]
            zt3 = [zt[0], zt[1], AR.alloc([128, D], F32)]
            r_zt3 = [r_zt[0], r_zt[1], R()]
            lnt3 = [lnt[0], lnt[1], ln_tmp()]

            def l1_A(i):
                k = i % 2
                z3 = i % 3
                src_t, src_op = res_src[i]
                dma("sp", xr[k], src_t[i * 128:(i + 1) * 128, :], (), [r_xr[k]], deps=[src_op])
                b = 2 * k
                for half in range(2):
                    W_, r_W = Wo[half]
                    for kc in range(8):
                        mm(bank(b + half), mergedT[:, kc, i * 128:(i + 1) * 128], W_[:, kc, :],
                           kc == 0, False, [r_mg[i], r_W], [r_bank[b + half]])
                    mm(bank(b + half), onesrow_f, rowsm[0:1, half * 512:(half + 1) * 512], False, True,
                       [r_cst, r_rowsm], [r_bank[b + half]])
                for half in range(2):
                    stt("dve", zt3[z3][:, half * 512:(half + 1) * 512], xr[k][:, half * 512:(half + 1) * 512], ALPHA,
                        bank(b + half), ALU.mult, ALU.add, [r_xr[k], r_bank[b + half]], [r_zt3[z3]])
                ln_stats(zt3[z3], r_zt3[z3], lnt3[z3])

            def l1_B1(i):
                z3 = i % 3
                ln_norm(zt3[z3], r_zt3[z3], lnt3[z3])

            def l1_Bg(i):
                z3 = i % 3
                k4 = i % 4
                ln_affine(zt3[z3], r_zt3[z3], lnbc[:, 0:D], lnbc[:, D:2 * D], r_lnbc)
                st_op = dma("sp", xres[i * 128:(i + 1) * 128, :], zt3[z3], [r_zt3[z3]], ())
                new_res[i] = (xres, st_op)
                cp("act", x1b3[k4], zt3[z3], [r_zt3[z3]], [r_x1b3[k4]])

            def l1_Bt(i):
                z3 = i % 3
                b = 4
                for kc in range(8):
                    bb = b + kc // 4
                    tr(bank(bb)[:, (kc % 4) * 128:(kc % 4 + 1) * 128], zt3[z3][:, kc * 128:(kc + 1) * 128], ident_f,
                       [r_zt3[z3], r_cst], [r_bank[bb]])
                cp("act", x1T[:, 0:4, :], bank(b).rearrange("p (a b) -> p a b", a=4), [r_bank[b]], [r_x1T])
                cp("act", x1T[:, 4:8, :], bank(b + 1).rearrange("p (a b) -> p a b", a=4), [r_bank[b + 1]], [r_x1T])
                c0_ = (i % 4) * NE
                for kc in range(8):
                    mm(bank(6)[:, c0_:c0_ + NE], x1T[:, kc, :], wr[:, kc, :], kc == 0, False,
                       [r_x1T, r_wr], [r_bank[6]])
                mm(bank(6)[:, c0_:c0_ + NE], onesrow_f, rowsm[0:1, D:D + NE], False, True,
                   [r_cst, r_rowsm], [r_bank[6]])

            def l1_C(i):
                k = i % 2
                c0_ = (i % 4) * NE
                lg = lg_all[:, i, :]
                r_lg = r_lga[i]
                cp("dve", lg, bank(6)[:, c0_:c0_ + NE], [r_bank[6]], [r_lg])
                S.op("dve", lambda e: e.max(out=m8, in_=lg), [r_lg], [r_m8])
                ts("dve", Af, lg, m8[:, 3:4], None, ALU.is_ge, None, [r_lg, r_m8], [r_Af])
                if i == 0:
                    ts("dve", Af, Af, vtok, None, ALU.mult, None, [r_Af, r_cst], [r_Af])
                ts("dve", nm, m8[:, 0:1], -1.0, None, ALU.mult, None, [r_m8], [r_nm])
                act(ex, lg, AF.Exp, [r_lg, r_nm], [r_ex], bias=nm[:, 0:1])
                stt("dve", ex, Af, 1.0, ex, ALU.mult, ALU.mult, [r_Af, r_ex], [r_ex, r_ss], accum_out=ssum)
                ts("dve", ssum, ssum, 1e-30, None, ALU.max, None, [r_ss], [r_ss])
                S.op("dve", lambda e: e.reciprocal(out=ssum, in_=ssum), [r_ss], [r_ss])
                ts("dve", G_all[:, i, :], ex, ssum[:, 0:1], None, ALU.mult, None, [r_ex, r_ss], [r_G[i]])
                cp("dve", Abf, Af, [r_Af], [r_Abf])
                if i > 0:
                    tt("dve", Asum[k], Asum[(i - 1) % 2], Abf, ALU.add, [r_As[(i - 1) % 2], r_Abf], [r_As[k]])
                else:
                    cp("dve", Asum[k], Abf, [r_Abf], [r_As[k]])

            def l1_D(i):
                k3 = i % 4
                c0_ = (i % 4) * NE
                pp = bank(7)[:, c0_:c0_ + NE]
                mm(pp, tri_bf, Abf, True, i == 0, [r_cbf, r_Abf], [r_bank[7]])
                if i > 0:
                    mm(pp, ones_bf, Asum[(i - 1) % 2], False, True,
                       [r_cbf, r_As[(i - 1) % 2]], [r_bank[7]])
                stt("dve", key, pp, float(CAP - 1), base1, ALU.min, ALU.add,
                    [r_bank[7], r_cst], [r_key])
                tt("dve", key, key, Af, ALU.mult, [r_key, r_Af], [r_key])
                S.op("dve", lambda e: e.max(out=k8, in_=key), [r_key], [r_k8])
                cp("dve", idx_all[:, i * 4:(i + 1) * 4], k8[:, 0:4], [r_k8], [r_idx[i]])
                for kk in range(4):
                    stt("dve", junk, key, k8[:, kk:kk + 1], G_all[:, i, :], ALU.is_equal, ALU.mult,
                        [r_key, r_k8, r_G[i]], [r_junk, r_gate[i]],
                        accum_out=gate_all[:, i * 4 + kk:i * 4 + kk + 1])
                for kk in range(4):
                    o = S.dma("pool", lambda e, s_=x1b3[k3], ix=idx_all[:, i * 4 + kk:i * 4 + kk + 1]:
                              e.indirect_dma_start(out=xd[:, :],
                                                   out_offset=bass.IndirectOffsetOnAxis(ap=ix, axis=0),
                                                   in_=s_, in_offset=None,
                                                   bounds_check=regs["bc"], oob_is_err=False),
                              [r_x1b3[k3], r_idx[i]], ())
                    scat_ops.append(o)

            for step in range(NT + 4):
                if 0 <= step - 2 < NT:
                    l1_Bt(step - 2)
                if 0 <= step - 1 < NT:
                    l1_B1(step - 1)
                if step < NT:
                    l1_A(step)
                if 0 <= step - 1 < NT:
                    l1_Bg(step - 1)
                if 0 <= step - 4 < NT:
                    l1_D(step - 4)
                if 0 <= step - 3 < NT:
                    l1_C(step - 3)
            RG.release(n_o0)
            RG.release(n_o1)
            res_src = new_res

            AR.reset()
            RG.set_alias(2)
            brflat = brT[:, :, :].rearrange("p a b -> p (a b)")
            xg = [brflat[:, q_ * 3 * D:(q_ + 1) * 3 * D].rearrange("p (a b) -> p a b", a=3) for q_ in range(2)]
            xgT = [AR.alloc([128, 8, CAP], BF16) for _ in range(2)]
            actT = [AR.alloc([128, 8, CAP], BF16) for _ in range(2)]
            gc = [AR.alloc([128, CAP], F32) for _ in range(2)]
            sgm = [AR.alloc([128, CAP], F32) for _ in range(2)]
            t1 = [AR.alloc([128, CAP], F32) for _ in range(2)]
            ybf = [AR.alloc([128, D], BF16) for _ in range(2)]
            r_xg = [R(), R()]
            r_xgT = [R(), R()]
            r_actT = [R(), R()]
            r_gc = [R(), R()]
            r_sgm = [R(), R()]
            r_t1 = [R(), R()]
            r_ybf = [R(), R()]
            ys_ops = []
            cnt_ = {"it": 0, "ity": 0}

            def load_xg(e_):
                p = e_ % 2
                r0 = 1 + e_ * CAP
                dma("sp", xg[p], xd[r0:r0 + CAP, :].rearrange("(t p) d -> p t d", p=128), (), [r_xg[p]],
                    deps=scat_ops if e_ < 2 else ())

            def transposes(e_):
                p = e_ % 2
                for kp in range(4):
                    b = next_bank()
                    pb = bank_bf(b)
                    for kc2 in range(2):
                        kc = kp * 2 + kc2
                        for ti in range(3):
                            tr(pb[:, kc2 * CAP + ti * 128:kc2 * CAP + (ti + 1) * 128],
                               xg[p][:, ti, kc * 128:(kc + 1) * 128], ident_bf, [r_xg[p], r_cbf], [r_bank[b]])
                    cp("act" if kp % 2 else "dve", xgT[p][:, kp * 2:kp * 2 + 2, :],
                       pb[:, 0:2 * CAP].rearrange("p (a b) -> p a b", a=2), [r_bank[b]], [r_xgT[p]])

            def up(e_, hf):
                p = e_ % 2
                n_ug, Ug, r_Ug = RG.acquire(8)
                n_ul, Ul, r_Ul = RG.acquire(8)
                for jj in range(4):
                    j = hf * 4 + jj
                    bg = next_bank()
                    for kc in range(8):
                        mm(bank(bg)[:, 0:CAP], Ug[:, kc, jj * 128:(jj + 1) * 128], xgT[p][:, kc, :],
                           kc == 0, kc == 7, [r_Ug, r_xgT[p]], [r_bank[bg]])
                    bl = next_bank()
                    for kc in range(8):
                        mm(bank(bl)[:, 0:CAP], Ul[:, kc, jj * 128:(jj + 1) * 128], xgT[p][:, kc, :],
                           kc == 0, kc == 7, [r_Ul, r_xgT[p]], [r_bank[bl]])
                    k = cnt_["it"] % 2
                    cnt_["it"] += 1
                    ts("dve", gc[k], bank(bg)[:, 0:CAP], bcol(C_UPB + e_ * 16 + j), 7.0, ALU.add, ALU.min,
                       [r_bank[bg], r_colp], [r_gc[k]])
                    act(sgm[k], gc[k], AF.Sigmoid, [r_gc[k]], [r_sgm[k]], scale=1.702)
                    act(t1[k], bank(bl)[:, 0:CAP], AF.Identity, [r_bank[bl], r_upb1], [r_t1[k]],
                        bias=upb1[:, e_ * 8 + j:e_ * 8 + j + 1])
                    ts("dve", t1[k], t1[k], -6.0, 8.0, ALU.max, ALU.min, [r_t1[k]], [r_t1[k]])
                    tt("dve", gc[k], gc[k], sgm[k], ALU.mult, [r_gc[k], r_sgm[k]], [r_gc[k]])
                    tt("dve", actT[p][:, j, :], t1[k], gc[k], ALU.mult, [r_t1[k], r_gc[k]], [r_actT[p]])
                RG.release(n_ug)
                RG.release(n_ul)

            def down(e_):
                p = e_ % 2
                r0 = 1 + e_ * CAP
                n_d0, D0, r_D0 = RG.acquire(8)
                n_d1, D1, r_D1 = RG.acquire(8)
                Dn = [(D0, r_D0), (D1, r_D1)]
                for ti in range(3):
                    b = next_bank2()
                    for half in range(2):
                        W_, r_W = Dn[half]
                        for kc in range(8):
                            mm(bank(b + half), actT[p][:, kc, ti * 128:(ti + 1) * 128], W_[:, kc, :],
                               kc == 0, kc == 7, [r_actT[p], r_W], [r_bank[b + half]])
                    k = cnt_["ity"] % 2
                    cnt_["ity"] += 1
                    cp("act", ybf[k][:, 0:512], bank(b), [r_bank[b]], [r_ybf[k]])
                    cp("dve", ybf[k][:, 512:1024], bank(b + 1), [r_bank[b + 1]], [r_ybf[k]])
                    o = dma("sp", ys[r0 + ti * 128:r0 + (ti + 1) * 128, :], ybf[k], [r_ybf[k]], ())
                    ys_ops.append(o)
                RG.release(n_d0)
                RG.release(n_d1)

            load_xg(0)
            load_xg(1)
            transposes(0)
            up(0, 0)
            up(0, 1)
            for e_ in range(NE):
                if e_ + 1 < NE:
                    if e_ + 2 < NE:
                        load_xg(e_ + 2)
                    transposes(e_ + 1)
                    up(e_ + 1, 0)
                down(e_)
                if e_ + 1 < NE:
                    up(e_ + 1, 1)

            RG.set_alias(0)
            AR.reset()
            yk = [AR.alloc([128, 4, D], BF16) for _ in range(2)]
            xr = [AR.alloc([128, D], F32) for _ in range(2)]
            zt = [AR.alloc([128, D], F32) for _ in range(2)]
            x2b = [AR.alloc([128, D], BF16) for _ in range(2)]
            GT = AR.alloc([NE, 128], F32)
            lnt = [ln_tmp(), ln_tmp()]
            r_yk = [[R() for _ in range(4)] for _ in range(2)]
            r_xr = [R(), R()]
            r_zt = [R(), R()]
            r_x2b = [R(), R()]
            r_GT = R()
            new_res = {}

            def l2_G(i):
                k = i % 2
                for kk in range(4):
                    S.dma("pool", lambda e, d_=yk[k][:, kk, :], ix=idx_all[:, i * 4 + kk:i * 4 + kk + 1]:
                          e.indirect_dma_start(out=d_, out_offset=None, in_=ys[:, :],
                                               in_offset=bass.IndirectOffsetOnAxis(ap=ix, axis=0),
                                               bounds_check=regs["bc"], oob_is_err=False),
                          [r_idx[i]], [r_yk[k][kk]], deps=ys_ops if (i == 0 and kk == 0) else ())
                src_t, src_op = res_src[i]
                dma("sp", xr[k], src_t[i * 128:(i + 1) * 128, :], (), [r_xr[k]], deps=[src_op])

            def l2_A0(i):
                k = i % 2
                tr(bank(4)[0:NE, 0:128], G_all[:, i, :], ident_f, [r_G[i], r_cst], [r_bank[4]])
                cp("act", GT, bank(4)[0:NE, 0:128], [r_bank[4]], [r_GT])
                b = 2 * k
                for half in range(2):
                    mm(bank(b + half), GT, bdown[:, half * 512:(half + 1) * 512], True, True,
                       [r_GT, r_bdown], [r_bank[b + half]])

            def l2_A(i):
                k = i % 2
                b = 2 * k
                for half in range(2):
                    stt("dve", zt[k][:, half * 512:(half + 1) * 512], xr[k][:, half * 512:(half + 1) * 512], ALPHA,
                        bank(b + half), ALU.mult, ALU.add, [r_xr[k], r_bank[b + half]], [r_zt[k]])
                for kk in range(4):
                    stt("dve", zt[k], yk[k][:, kk, :], gate_all[:, i * 4 + kk:i * 4 + kk + 1], zt[k],
                        ALU.mult, ALU.add, [r_yk[k][kk], r_gate[i], r_zt[k]], [r_zt[k]])
                ln_stats(zt[k], r_zt[k], lnt[k])

            def l2_B1(i):
                k = i % 2
                ln_norm(zt[k], r_zt[k], lnt[k])

            def l2_B2(i):
                k = i % 2
                ln_affine(zt[k], r_zt[k], lnbc[:, 2 * D:3 * D], lnbc[:, 3 * D:4 * D], r_lnbc)
                if last:
                    if i > 0:
                        o = dma("sp", out_d[(i - 1) * 128:i * 128, :], zt[k], [r_zt[k]], ())
                        final_ops.append(o)
                else:
                    st_op = dma("sp", xres[i * 128:(i + 1) * 128, :], zt[k], [r_zt[k]], ())
                    new_res[i] = (xres, st_op)
                    cp("act", x2b[k], zt[k], [r_zt[k]], [r_x2b[k]])
                    b = 5 + k
                    pb = bank_bf(b)
                    for kc in range(8):
                        tr(pb[:, kc * 128:(kc + 1) * 128], x2b[k][:, kc * 128:(kc + 1) * 128], ident_bf,
                           [r_x2b[k], r_cbf], [r_bank[b]])
                    cp("act", xT[:, :, i * 128:(i + 1) * 128], pb.rearrange("p (a b) -> p a b", a=8),
                       [r_bank[b]], [r_xT[i]])

            l2_G(0)
            for step in range(NT + 1):
                if step + 1 < NT:
                    l2_G(step + 1)
                if step < NT:
                    l2_A0(step)
                if 0 <= step - 1 < NT:
                    l2_B1(step - 1)
                if step < NT:
                    l2_A(step)
                if 0 <= step - 1 < NT:
                    l2_B2(step - 1)
            res_src = new_res

        assert RG.next_acq == len(plan), (RG.next_acq, len(plan))
        S.finalize(final_ops)
    return nc


def _consts(half):
    c = np.zeros((128, NCST), np.float32)
    c[:, K_ID:K_ID + 128] = np.eye(128, dtype=np.float32)
    tp = np.arange(128)
    c[:, K_TRI:K_TRI + 128] = (tp[:, None] < tp[None, :]).astype(np.float32)
    c[:, K_ONE:K_ONE + 128] = 1.0
    valid = 0.0 if half == 0 else 1.0
    c[:, K_VM:K_VM + 128] = valid
    c[:, K_VT] = valid
    for g, w in enumerate((2, 4, 8, 16)):
        for t in range(16):
            cnt = min(t + 1, w) if half == 0 else w
            c[:, K_PF + g * 16 + t] = w / cnt
    c[:, K_B1:K_B1 + 32] = (1 + np.arange(NE) * CAP)[None, :].astype(np.float32)
    return c


def _layer_params(inp, ls):
    f = lambda a: np.ascontiguousarray(np.asarray(a, dtype=np.float32))
    colp = np.zeros((len(ls), 128, NCOLP), np.float32)
    for n, l in enumerate(ls):
        colp[n, :, C_BIN:C_BIN + 40] = inp["b_in"][l].reshape(40, 128).T
        colp[n, :, C_PSC:C_PSC + 4] = inp["pool_scale"][l].reshape(4, 128).T
        colp[n, :, C_CDW:C_CDW + 124] = inp["conv_dw"][l].T.reshape(4, 128, 31).transpose(1, 0, 2).reshape(128, 124)
        colp[n, :, C_CDWB:C_CDWB + 4] = inp["conv_dw_b"][l].reshape(4, 128).T
        colp[n, :, C_CLG:C_CLG + 4] = inp["conv_ln_g"][l].reshape(4, 128).T
        colp[n, :, C_CLB:C_CLB + 4] = inp["conv_ln_b"][l].reshape(4, 128).T
        colp[n, :, C_CPWB:C_CPWB + 8] = inp["conv_pw_b"][l].reshape(8, 128).T
        colp[n, :, C_UPB:C_UPB + 512] = inp["exp_up_b"][l].reshape(NE, 16, 128).transpose(2, 0, 1).reshape(128, 512)
    rowbc = np.stack([np.concatenate([inp["ln1_g"][l], inp["ln1_b"][l], inp["ln2_g"][l], inp["ln2_b"][l]])[None, :]
                      for l in ls]).astype(np.float32)
    rowsm = np.stack([np.concatenate([inp["b_out"][l], inp["router_b"][l]])[None, :] for l in ls]).astype(np.float32)
    sl = slice(ls[0], ls[-1] + 1)
    d = {
        "w_in": f(inp["w_in"][sl]), "pool_w": f(inp["pool_w"][sl]), "pool_proj": f(inp["pool_proj"][sl]),
        "conv_pw": f(inp["conv_pw"][sl]), "attn_o": f(inp["attn_o"][sl]), "w_kv": f(inp["w_kv"][sl]),
        "w_out": f(inp["w_out"][sl]), "exp_up": f(inp["exp_up"][sl]), "exp_down": f(inp["exp_down"][sl]),
        "router_w": f(inp["router_w"][sl]), "exp_down_b": f(inp["exp_down_b"][sl]),
        "colp": colp, "rowbc": rowbc, "rowsm": rowsm,
    }
    return d


def _x_shards(xfull):
    outs = []
    for c in range(8):
        b, half = c // 2, c % 2
        xs = np.zeros((T, D), np.float32)
        s0 = half * 2048
        xs[128:] = xfull[b, s0:s0 + 2048]
        if half == 1:
            xs[:128] = xfull[b, s0 - 128:s0]
        outs.append(xs)
    return outs


_PROG_CACHE = {}


def _get_prog(NL):
    if NL not in _PROG_CACHE:
        _PROG_CACHE[NL] = build_program(NL)
    return _PROG_CACHE[NL]


LAYERS_PER_LAUNCH = 4


def kernel(**inp):
    inp = {k: np.asarray(v) for k, v in inp.items()}
    x = np.asarray(inp["x"], np.float32)
    memln = np.concatenate([inp["mem_ln_g"], inp["mem_ln_b"]])[None, :].astype(np.float32)
    NL = LAYERS_PER_LAUNCH
    nc = _get_prog(NL)
    csts = [_consts(c % 2) for c in range(8)]
    for l0 in range(0, DEPTH, NL):
        lp = _layer_params(inp, list(range(l0, l0 + NL)))
        xs = _x_shards(x)
        in_maps = []
        for c in range(8):
            m = dict(lp)
            m["x_in"] = xs[c]
            m["mem"] = np.ascontiguousarray(inp["mem"][c // 2], dtype=np.float32)
            m["memln"] = memln
            m["cst"] = csts[c]
            in_maps.append(m)
        res = run_bass_kernel_spmd(nc, in_maps, core_ids=list(range(8)))
        xn = np.empty_like(x)
        for c in range(8):
            b, half = c // 2, c % 2
            xn[b, half * 2048:(half + 1) * 2048] = np.asarray(res.results[c]["out"], np.float32)
        x = xn
    return x
```

```python
import contextlib
import numpy as np
import ml_dtypes
import concourse.bass as bass
import concourse.mybir as mybir
from concourse.bass_utils import run_bass_kernel_spmd

F32 = mybir.dt.float32
BF16 = mybir.dt.bfloat16
I32 = mybir.dt.int32
AF = mybir.ActivationFunctionType
ALU = mybir.AluOpType

D = 1024
NT = 17
T = NT * 128
SEQ = 4096
DEPTH = 4
NE = 32
CAP = 384
NSLOT = 1 + NE * CAP
IN_COLS = 5120
MEM = 256
ALPHA = (2.0 * DEPTH) ** 0.25
LN_EPS = 1e-5
CBS = [(0, 512), (512, 512), (1024, 512), (1536, 512), (2048, 128)]
NW = 4
ATT_SCALE = 128 ** -0.5

C_BIN = 0
C_PSC = 40
C_CDW = 44
C_CDWB = 168
C_CLG = 172
C_CLB = 176
C_CPWB = 180
C_UPB = 188
NCOLP = 700
K_ID = 0
K_TRI = 128
K_ONE = 256
K_VM = 384
K_VT = 512
K_PF = 513
K_B1 = 577
NCST = 609


ENGS = ("pe", "act", "dve", "pool", "sp")


class Reg:
    __slots__ = ("w", "r", "rd")

    def __init__(self):
        self.w = None
        self.r = {}
        self.rd = []


class Op:
    __slots__ = ("eng", "fn", "deps", "is_dma", "sem", "val", "needed", "epoch")


class Sched:
    def __init__(self, nc, n_dma_sems=20):
        self.nc = nc
        self.q = {e: [] for e in ENGS}
        self.n_dma_sems = n_dma_sems
        self.epoch = 0
        self.pending = {}
        self.last_compute = {}
        self.dma_since_barrier = []
        self.prologue = {}

    def _mk(self, eng, fn, reads, writes, deps, is_dma, bar=True):
        o = Op()
        o.eng = eng
        o.fn = fn
        o.is_dma = is_dma
        o.sem = None
        o.val = None
        o.needed = is_dma
        o.epoch = self.epoch
        d = [x for x in deps if x is not None]
        for r in reads:
            if r.w is not None:
                d.append(r.w)
        for w in writes:
            if w.w is not None:
                d.append(w.w)
            d.extend(w.r.values())
            d.extend(w.rd)
        pb = self.pending.pop(eng, None)
        if pb:
            d.extend(pb)
        o.deps = d
        for r in reads:
            if is_dma:
                r.rd.append(o)
            else:
                r.r[eng] = o
        for w in writes:
            w.w = o
            w.r = {}
            w.rd = []
        self.q[eng].append(o)
        if is_dma:
            if bar:
                self.dma_since_barrier.append(o)
        else:
            self.last_compute[eng] = o
        return o

    def op(self, eng, fn, reads=(), writes=(), deps=()):
        return self._mk(eng, fn, reads, writes, deps, False)

    def dma(self, eng, fn, reads=(), writes=(), deps=(), bar=True):
        return self._mk(eng, fn, reads, writes, deps, True, bar)

    def barrier(self):
        lst = list(self.last_compute.values()) + list(self.dma_since_barrier)
        self.dma_since_barrier = []
        for e in ENGS:
            self.pending[e] = list(lst) + self.pending.get(e, [])

    def finalize(self, final_ops):
        nc = self.nc
        for e in ENGS:
            for o in self.q[e]:
                for d in o.deps:
                    d.needed = True
        for o in final_ops:
            o.needed = True
        n_epochs = self.epoch + 1
        with contextlib.ExitStack() as stack:
            sems = {}
            for e in ENGS:
                for ep in range(n_epochs):
                    sems[(e, ep)] = stack.enter_context(nc.semaphore(f"s_{e}{ep}"))
            dsems = {}
            for e in ("sp", "act", "pool"):
                for i in range(self.n_dma_sems):
                    dsems[(e, i)] = stack.enter_context(nc.semaphore(f"d_{e}{i}"))
            cnt = {}
            slot_next = {e: 0 for e in ENGS}
            slot_cnt = {}
            slot_last = {}
            for e in ENGS:
                for o in self.q[e]:
                    if o.is_dma:
                        s = slot_next[e]
                        slot_next[e] = (s + 1) % self.n_dma_sems
                        key = (e, s)
                        prev = slot_last.get(key)
                        if prev is not None:
                            o.deps.append(prev)
                        slot_cnt[key] = slot_cnt.get(key, 0) + 16
                        slot_last[key] = o
                        o.sem = dsems[key]
                        o.val = slot_cnt[key]
                    elif o.needed:
                        k = (e, o.epoch)
                        cnt[k] = cnt.get(k, 0) + 1
                        o.sem = sems[k]
                        o.val = cnt[k]
            self.max_counts = cnt
            block = stack.enter_context(nc.Block())
            engmap = {"pe": block.tensor, "act": block.scalar, "dve": block.vector,
                      "pool": block.gpsimd, "sp": block.sync}

            def make(e):
                def body(eng):
                    seen = {}
                    for pf in self.prologue.get(e, ()):
                        pf(eng)
                    for o in self.q[e]:
                        for d in o.deps:
                            if (not d.is_dma) and d.eng == e and e == "pe":
                                continue
                            k = id(d.sem)
                            if seen.get(k, 0) >= d.val:
                                continue
                            seen[k] = d.val
                            eng.wait_ge(d.sem, d.val)
                        inst = o.fn(eng)
                        if o.sem is not None:
                            inst.then_inc(o.sem, 16 if o.is_dma else 1)
                    if e == "sp":
                        for o in final_ops:
                            if seen.get(id(o.sem), 0) >= o.val:
                                continue
                            seen[id(o.sem)] = o.val
                            eng.wait_ge(o.sem, o.val)
                return body

            for e in ENGS:
                engmap[e](make(e))


def build_program(NL, first_layer_is_input=True):
    nc = bass.Bass("TRN2", target_bir_lowering=False)
    dt_in = lambda n, s, d=F32: nc.dram_tensor(n, s, d, kind="ExternalInput")
    x_in = dt_in("x_in", [T, D])
    mem_in = dt_in("mem", [MEM, D])
    memln = dt_in("memln", [1, 2 * D])
    cst_in = dt_in("cst", [128, NCST])
    w_in = dt_in("w_in", [NL, D, IN_COLS])
    pool_w = dt_in("pool_w", [NL, 4, 128, 128])
    pool_proj = dt_in("pool_proj", [NL, 512, D])
    conv_pw = dt_in("conv_pw", [NL, 512, D])
    attn_o = dt_in("attn_o", [NL, 512, D])
    w_kv = dt_in("w_kv", [NL, D, D])
    w_out = dt_in("w_out", [NL, D, D])
    exp_up = dt_in("exp_up", [NL, NE, D, 2 * D])
    exp_down = dt_in("exp_down", [NL, NE, D, D])
    router_w = dt_in("router_w", [NL, D, NE])
    exp_down_b = dt_in("exp_down_b", [NL, NE, D])
    colp_in = dt_in("colp", [NL, 128, NCOLP])
    rowbc_in = dt_in("rowbc", [NL, 1, 4 * D])
    rowsm_in = dt_in("rowsm", [NL, 1, D + NE])
    out_d = nc.dram_tensor("out", [T - 128, D], F32, kind="ExternalOutput")
    xres = nc.dram_tensor("xres", [T, D], F32, kind="Internal")
    xd = nc.dram_tensor("xd", [NSLOT, D], BF16, kind="Internal")
    ys = nc.dram_tensor("ys", [NSLOT, D], BF16, kind="Internal")

    with contextlib.ExitStack() as st:
        sb = lambda n, s, d: st.enter_context(nc.sbuf_tensor("sb_" + n, s, d))
        S = Sched(nc)

        xT = sb("xT", [128, 8, T], BF16)
        mergedT = sb("mergedT", [128, 8, T], BF16)
        brT = sb("brT", [128, 4, T], BF16)
        ring = [sb(f"ring{i}", [128, 4096], BF16) for i in range(NW)]
        cst = sb("cst", [128, NCST], F32)
        cbf = sb("cbf", [128, 384], BF16)
        colp = sb("colp", [128, NCOLP], F32)
        upb1 = sb("upb1", [128, NE * 8], F32)
        lnbc = sb("lnbc", [128, 4 * D], F32)
        rowsm = sb("rowsm", [1, D + NE], F32)
        wr = sb("wr", [128, 8, NE], F32)
        bdown = sb("bdown", [NE, D], F32)
        memT = sb("memT", [128, 8, MEM], BF16)
        kT = sb("kT", [128, 4, MEM], BF16)
        vv = sb("vv", [128, 2, 512], BF16)
        poolw = sb("poolw", [128, 4, 128], BF16)
        idx_all = sb("idx_all", [128, NT * 4], I32)
        gate_all = sb("gate_all", [128, NT * 4], F32)
        G_all = sb("G_all", [128, NT, NE], F32)
        ARENA_BF = 24064
        arena = sb("arena", [128, ARENA_BF], BF16)
        ps = [st.enter_context(nc.psum_tensor(f"ps{i}", [128, 1024], F32)) for i in range(4)]

        epsc = sb("epsc", [128, 1], F32)
        r_eps = Reg()
        ident_bf = cbf[:, 0:128]
        tri_bf = cbf[:, 128:256]
        ones_bf = cbf[:, 256:384]
        ident_f = cst[:, K_ID:K_ID + 128]
        onesrow_f = cst[0:1, K_ONE:K_ONE + 128]
        vmask = cst[:, K_VM:K_VM + 128]
        vtok = cst[:, K_VT:K_VT + 1]
        base1 = cst[:, K_B1:K_B1 + 32]

        R = lambda: Reg()
        r_xT = [R() for _ in range(NT)]
        r_mg = [R() for _ in range(NT)]
        r_br = [[R() for _ in CBS] for _ in range(4)]
        r_ring = [R() for _ in range(NW)]
        r_cst, r_cbf, r_colp, r_upb1, r_lnbc, r_rowsm, r_wr, r_bdown = (R() for _ in range(8))
        r_memT, r_kT, r_vv, r_poolw = R(), R(), R(), R()
        r_idx = [R() for _ in range(NT)]
        r_gate = [R() for _ in range(NT)]
        r_G = [R() for _ in range(NT)]
        r_bank = [R() for _ in range(8)]

        def bank(b):
            return ps[b // 2][:, (b % 2) * 512:(b % 2) * 512 + 512]

        def bank_bf(b):
            return ps[b // 2][:, (b % 2) * 512:(b % 2) * 512 + 512].bitcast(BF16)

        bank_rr = [0]

        def next_bank():
            b = bank_rr[0]
            bank_rr[0] = (b + 1) % 8
            return b

        def next_bank2():
            b = bank_rr[0]
            if b % 2:
                b = (b + 1) % 8
            bank_rr[0] = (b + 2) % 8
            return b

        class Arena:
            def __init__(self):
                self.off = 0

            def reset(self):
                S.barrier()
                self.off = 0

            def alloc(self, shape, dtype):
                n = 1
                for s_ in shape[1:]:
                    n *= s_
                nb = n * (2 if dtype == BF16 else 4)
                self.off = (self.off + 63) // 64 * 64
                o_bf = self.off // 2
                self.off += nb
                assert self.off <= ARENA_BF * 2, f"arena overflow {self.off}"
                v = arena[0:shape[0], o_bf:o_bf + nb // 2]
                if dtype != BF16:
                    v = v.bitcast(dtype)
                if len(shape) == 3:
                    v = v.rearrange("p (a b) -> p a b", a=shape[1])
                return v

        AR = Arena()
        regs = {}
        S.prologue["pool"] = [lambda e: regs.__setitem__("bc", e.to_reg(NSLOT - 1))]

        def mm(out, lhsT, rhs, start, stop, reads, writes):
            return S.op("pe", lambda e: e.matmul(out, lhsT=lhsT, rhs=rhs, start=start, stop=stop),
                        reads, writes)

        def tr(out, in_, ident, reads, writes):
            return S.op("pe", lambda e: e.transpose(out=out, in_=in_, identity=ident), reads, writes)

        def act(out, in_, func, reads, writes, bias=None, scale=None, eng="act"):
            kw = {}
            if bias is not None:
                kw["bias"] = bias
            if scale is not None:
                kw["scale"] = scale
            return S.op(eng, lambda e: e.activation(out=out, in_=in_, func=func, **kw), reads, writes)

        def ts(eng, out, in0, s1, s2, op0, op1, reads, writes, accum_out=None):
            kw = {}
            if op1 is not None:
                kw["op1"] = op1
            if accum_out is not None:
                kw["accum_out"] = accum_out
            return S.op(eng, lambda e: e.tensor_scalar(out=out, in0=in0, scalar1=s1, scalar2=s2,
                                                       op0=op0, **kw), reads, writes)

        def tt(eng, out, in0, in1, op, reads, writes):
            return S.op(eng, lambda e: e.tensor_tensor(out=out, in0=in0, in1=in1, op=op), reads, writes)

        def stt(eng, out, in0, scalar, in1, op0, op1, reads, writes, accum_out=None):
            kw = {}
            if accum_out is not None:
                kw["accum_out"] = accum_out
            return S.op(eng, lambda e: e.scalar_tensor_tensor(out=out, in0=in0, scalar=scalar, in1=in1,
                                                              op0=op0, op1=op1, **kw), reads, writes)

        def cp(eng, out, in_, reads, writes):
            if eng == "act":
                return S.op(eng, lambda e: e.activation(out=out, in_=in_, func=AF.Copy), reads, writes)
            return S.op(eng, lambda e: e.tensor_copy(out=out, in_=in_), reads, writes)

        def memset(eng, ap, val, writes):
            return S.op(eng, lambda e: e.memset(ap, val), (), writes)

        def dma(eng, out, in_, reads, writes, deps=(), bar=True):
            return S.dma(eng, lambda e: e.dma_start(out=out, in_=in_), reads, writes, deps, bar)

        memset("dve", epsc[:, :], LN_EPS, [r_eps])

        plan = []
        kinds = []

        def kcview(ap2d):
            return ap2d.rearrange("(kc p) f -> p kc f", p=128)

        for l in range(NL):
            plan.append(kcview(w_kv[l][:, 0:512]))
            plan.append(kcview(w_kv[l][:, 512:1024]))
            plan.append(kcview(w_in[l][:, 0:512]))
            plan.append(kcview(pool_proj[l]))
            plan.append(kcview(w_in[l][:, 2048:2560]))
            plan.append(kcview(w_in[l][:, 2560:3072]))
            plan.append(kcview(w_in[l][:, 512:1024]))
            plan.append(kcview(w_in[l][:, 1024:1536]))
            plan.append(kcview(conv_pw[l]))
            plan.append(kcview(w_in[l][:, 3072:3584]))
            plan.append(kcview(w_in[l][:, 3584:4096]))
            plan.append(kcview(w_in[l][:, 1536:2048]))
            plan.append(kcview(attn_o[l]))
            plan.append(kcview(w_in[l][:, 4096:4608]))
            plan.append(kcview(w_in[l][:, 4608:5120]))
            plan.append(kcview(w_out[l][:, 0:512]))
            plan.append(kcview(w_out[l][:, 512:1024]))
            kinds.extend(["m"] * 17)
            def _up(e, hf):
                kinds.extend(["e"] * 2)
                plan.append(kcview(exp_up[l, e][:, hf * 512:(hf + 1) * 512]))
                plan.append(kcview(exp_up[l, e][:, 1024 + hf * 512:1024 + (hf + 1) * 512]))

            def _down(e):
                kinds.extend(["e"] * 2)
                plan.append(kcview(exp_down[l, e][:, 0:512]))
                plan.append(kcview(exp_down[l, e][:, 512:1024]))

            _up(0, 0)
            _up(0, 1)
            for e in range(NE):
                if e + 1 < NE:
                    _up(e + 1, 0)
                _down(e)
                if e + 1 < NE:
                    _up(e + 1, 1)

        NWE = NW + 8
        xTflat = xT[:, :, :].rearrange("p a b -> p (a b)")
        mgflat = mergedT[:, :, :].rearrange("p a b -> p (a b)")
        ringv = [ring[i][:, :] for i in range(NW)]
        ringv += [xTflat[:, q_ * 4096:(q_ + 1) * 4096] for q_ in range(4)]
        ringv += [mgflat[:, q_ * 4096:(q_ + 1) * 4096] for q_ in range(4)]
        r_ring.extend(Reg() for _ in range(8))

        class Ring:
            def __init__(self):
                self.next_load = 0
                self.next_acq = 0
                self.released = [False] * len(plan)
                self.allow_alias = 0
                self.slot = []
                self.prev = []
                last = {}
                bc = ec = 0
                for n, k_ in enumerate(kinds):
                    if k_ == "m":
                        sl = bc % NW
                        bc += 1
                    else:
                        sl = ec % NWE
                        ec += 1
                    self.slot.append(sl)
                    self.prev.append(last.get(sl))
                    last[sl] = n

            def _try_load(self):
                while self.next_load < len(plan):
                    n = self.next_load
                    pv = self.prev[n]
                    if pv is not None and not self.released[pv]:
                        break
                    sl = self.slot[n]
                    if sl >= NW + 4 and self.allow_alias < 2:
                        break
                    if sl >= NW and self.allow_alias < 1:
                        break
                    src = plan[n]
                    a = src.shape[1]
                    dst = ringv[sl].rearrange("p (a b) -> p a b", a=a)
                    dma("pool", dst, src, (), [r_ring[sl]], bar=False)
                    self.next_load += 1

            def acquire(self, a):
                self._try_load()
                n = self.next_acq
                assert n < self.next_load, "ring: item not loaded (release order problem)"
                assert plan[n].shape[1] == a
                self.next_acq += 1
                sl = self.slot[n]
                v = ringv[sl].rearrange("p (a b) -> p a b", a=a)
                return n, v, r_ring[sl]

            def release(self, n):
                self.released[n] = True
                self._try_load()

            def set_alias(self, level):
                self.allow_alias = level
                if level:
                    self._try_load()

        RG = Ring()

        dma("sp", cst[:, :], cst_in[:, :], (), [r_cst])
        cp("dve", cbf[:, :], cst[:, 0:384], [r_cst], [r_cbf])

        def ln_tmp():
            return (AR.alloc([128, 12], F32), AR.alloc([128, 2], F32), AR.alloc([128, 1], F32),
                    AR.alloc([128, 1], F32), R(), R(), R(), R())

        def ln_stats(zt, r_z, tmp_):
            stt_, mv, rs, nmr, r_s, r_mv, r_rs, r_nmr = tmp_
            S.op("dve", lambda e: e.bn_stats(out=stt_[:, 0:6], in_=zt[:, 0:512]), [r_z], [r_s])
            S.op("dve", lambda e: e.bn_stats(out=stt_[:, 6:12], in_=zt[:, 512:1024]), [r_z], [r_s])
            S.op("dve", lambda e: e.bn_aggr(out=mv, in_=stt_), [r_s], [r_mv])
            act(rs, mv[:, 1:2], AF.Sqrt, [r_mv, r_eps], [r_rs], bias=epsc[:, 0:1])

        def ln_norm(zt, r_z, tmp_):
            stt_, mv, rs, nmr, r_s, r_mv, r_rs, r_nmr = tmp_
            S.op("dve", lambda e: e.reciprocal(out=rs, in_=rs), [r_rs], [r_rs])
            ts("dve", nmr, mv[:, 0:1], rs[:, 0:1], -1.0, ALU.mult, ALU.mult, [r_mv, r_rs], [r_nmr])
            act(zt, zt, AF.Identity, [r_z, r_rs, r_nmr], [r_z], bias=nmr[:, 0:1], scale=rs[:, 0:1])

        def ln_affine(zt, r_z, gv, bv, r_gb):
            tt("dve", zt, zt, gv, ALU.mult, [r_z, r_gb], [r_z])
            tt("dve", zt, zt, bv, ALU.add, [r_z, r_gb], [r_z])

        def ln_apply(zt, r_z, gv, bv, r_gb, tmp_):
            ln_norm(zt, r_z, tmp_)
            ln_affine(zt, r_z, gv, bv, r_gb)

        def ln_rows(zt, r_z, out32, r_out, gv, bv, r_gb, tmp_):
            ln_stats(zt, r_z, tmp_)
            ln_apply(zt, r_z, gv, bv, r_gb, tmp_)

        def to_xT(src_bf, r_src, i):
            b = next_bank()
            pb = bank_bf(b)
            for kc in range(8):
                tr(pb[:, kc * 128:(kc + 1) * 128], src_bf[:, kc * 128:(kc + 1) * 128], ident_bf,
                   [r_src, r_cbf], [r_bank[b]])
            cp("act" if i % 2 else "dve", xT[:, :, i * 128:(i + 1) * 128],
               pb.rearrange("p (a b) -> p a b", a=8), [r_bank[b]], [r_xT[i]])

        AR.reset()
        mg_bc = AR.alloc([128, 2 * D], F32)
        r_mgbc = R()
        lnt = [ln_tmp(), ln_tmp()]
        zer = AR.alloc([128, 8 * D], BF16)
        r_zer = R()
        memset("pool", zer, 0.0, [r_zer])
        for j_ in range(NE * CAP // 1024):
            dma("sp", xd[1 + j_ * 1024:1 + (j_ + 1) * 1024, :].rearrange("(p r) d -> p r d", p=128),
                zer.rearrange("p (r d) -> p r d", r=8), [r_zer], ())
        dma("sp", xd[0:1, :], zer[0:1, 0:D], [r_zer], ())
        dma("sp", ys[0:1, :], zer[0:1, 0:D], [r_zer], ())
        dma("sp", mg_bc, memln[0:1, :].to_broadcast([128, 2 * D]), (), [r_mgbc])
        for mt in range(2):
            mtile = AR.alloc([128, D], F32)
            mbf = AR.alloc([128, D], BF16)
            r_mt, r_mb = R(), R()
            dma("sp", mtile, mem_in[mt * 128:(mt + 1) * 128, :], (), [r_mt])
            ln_rows(mtile, r_mt, mtile, r_mt, mg_bc[:, 0:D], mg_bc[:, D:2 * D], r_mgbc, lnt[mt])
            cp("act", mbf, mtile, [r_mt], [r_mb])
            b = next_bank()
            pb = bank_bf(b)
            for kc in range(8):
                tr(pb[:, kc * 128:(kc + 1) * 128], mbf[:, kc * 128:(kc + 1) * 128], ident_bf,
                   [r_mb, r_cbf], [r_bank[b]])
            cp("dve", memT[:, :, mt * 128:(mt + 1) * 128], pb.rearrange("p (a b) -> p a b", a=8),
               [r_bank[b]], [r_memT])

        AR.reset()
        xl = [AR.alloc([128, D], F32) for _ in range(2)]
        xb = [AR.alloc([128, D], BF16) for _ in range(2)]
        r_xl = [R(), R()]
        r_xb = [R(), R()]
        for i in range(NT):
            k = i % 2
            dma("sp", xl[k], x_in[i * 128:(i + 1) * 128, :], (), [r_xl[k]])
            cp("act" if i % 2 == 0 else "dve", xb[k], xl[k], [r_xl[k]], [r_xb[k]])
            to_xT(xb[k], r_xb[k], i)

        res_src = {i: (x_in, None) for i in range(NT)}
        final_ops = []

        for l in range(NL):
            last = (l == NL - 1)
            S.epoch += 1
            AR.reset()
            dma("sp", colp[:, :], colp_in[l], (), [r_colp])
            dma("sp", lnbc[:, :], rowbc_in[l].to_broadcast([128, 4 * D]), (), [r_lnbc])
            dma("sp", rowsm[:, :], rowsm_in[l], (), [r_rowsm])
            dma("sp", wr[:, :, :], router_w[l].rearrange("(kc p) e -> p kc e", p=128), (), [r_wr])
            dma("sp", bdown[:, :], exp_down_b[l], (), [r_bdown])
            dma("pool", poolw[:, :, :], pool_w[l].rearrange("g c d -> c g d"), (), [r_poolw])
            ts("dve", upb1[:, :].rearrange("p (e j) -> p e j", e=NE),
               colp[:, C_UPB:C_UPB + 512].rearrange("p (e j) -> p e j", e=NE)[:, :, 8:16],
               1.0, None, ALU.add, None, [r_colp], [r_upb1])

            def bcol(c):
                return colp[:, c:c + 1]

            n_k, Wk, r_Wk = RG.acquire(8)
            for hd in range(4):
                b = next_bank()
                for kc in range(8):
                    mm(bank(b)[:, 0:MEM], Wk[:, kc, hd * 128:(hd + 1) * 128], memT[:, kc, :],
                       kc == 0, kc == 7, [r_Wk, r_memT], [r_bank[b]])
                cp("act", kT[:, hd, :], bank(b)[:, 0:MEM], [r_bank[b]], [r_kT])
            RG.release(n_k)
            n_v, Wv, r_Wv = RG.acquire(8)
            for mc in range(2):
                b = next_bank()
                for kc in range(8):
                    mm(bank(b), memT[:, kc, mc * 128:(mc + 1) * 128], Wv[:, kc, :],
                       kc == 0, kc == 7, [r_Wv, r_memT], [r_bank[b]])
                cp("act", vv[:, mc, :], bank(b), [r_bank[b]], [r_vv])
            RG.release(n_v)

            def xT_regs(cbi):
                c0, n = CBS[cbi]
                return [r_xT[i] for i in range(c0 // 128, (c0 + n) // 128)]

            def mg_regs(cbi):
                c0, n = CBS[cbi]
                return [r_mg[i] for i in range(c0 // 128, (c0 + n) // 128)]

            def gate_pass(br, P, r_P, bias_cols=None):
                sg = [AR.alloc([128, 512], F32) for _ in range(2)]
                tmp = [AR.alloc([128, 512], F32) for _ in range(2)]
                r_sg = [R(), R()]
                r_tmp = [R(), R()]
                it = 0
                for half in range(2):
                    n_g, G, r_Gs = RG.acquire(8)
                    for jj in range(4):
                        j = half * 4 + jj
                        for cbi, (c0, n) in enumerate(CBS):
                            bg = next_bank()
                            for kc in range(8):
                                mm(bank(bg)[:, 0:n], G[:, kc, jj * 128:(jj + 1) * 128], xT[:, kc, c0:c0 + n],
                                   kc == 0, kc == 7, [r_Gs] + xT_regs(cbi), [r_bank[bg]])
                            by = next_bank()
                            for kc in range(4):
                                mm(bank(by)[:, 0:n], P[:, kc, j * 128:(j + 1) * 128], brT[:, kc, c0:c0 + n],
                                   kc == 0, kc == 3, [r_P] + [r_br[kc][cbi]], [r_bank[by]])
                            k = it % 2
                            it += 1
                            act(sg[k][:, 0:n], bank(bg)[:, 0:n], AF.Sigmoid, [r_bank[bg], r_colp], [r_sg[k]],
                                bias=bcol(C_BIN + 16 + br * 8 + j))
                            mdst = mergedT[:, j, c0:c0 + n]
                            if br == 0:
                                tt("dve", mdst, sg[k][:, 0:n], bank(by)[:, 0:n], ALU.mult,
                                   [r_sg[k], r_bank[by]], mg_regs(cbi))
                            else:
                                if bias_cols is not None:
                                    stt("dve", tmp[k][:, 0:n], bank(by)[:, 0:n], bcol(bias_cols + j), sg[k][:, 0:n],
                                        ALU.add, ALU.mult, [r_bank[by], r_sg[k], r_colp], [r_tmp[k]])
                                else:
                                    tt("dve", tmp[k][:, 0:n], sg[k][:, 0:n], bank(by)[:, 0:n], ALU.mult,
                                       [r_sg[k], r_bank[by]], [r_tmp[k]])
                                tt("dve", mdst, mdst, tmp[k][:, 0:n], ALU.add, [r_tmp[k]] + mg_regs(cbi),
                                   mg_regs(cbi))
                    RG.release(n_g)

            AR.reset()
            n_wp, Wp, r_Wp = RG.acquire(8)
            u = AR.alloc([128, 16 + T], F32)
            sA = AR.alloc([128, 16 + T], F32)
            sB = AR.alloc([128, 16 + T], F32)
            pooled = AR.alloc([128, T], BF16)
            r_u, r_sA, r_sB, r_pl = R(), R(), R(), R()
            memset("pool", u[:, 0:16], 0.0, [r_u])
            memset("pool", sA[:, 0:16], 0.0, [r_sA])
            memset("pool", sB[:, 0:16], 0.0, [r_sB])
            for g in range(4):
                for cbi, (c0, n) in enumerate(CBS):
                    b = next_bank()
                    for kc in range(8):
                        mm(bank(b)[:, 0:n], Wp[:, kc, g * 128:(g + 1) * 128], xT[:, kc, c0:c0 + n],
                           kc == 0, kc == 7, [r_Wp] + xT_regs(cbi), [r_bank[b]])
                    act(u[:, 16 + c0:16 + c0 + n], bank(b)[:, 0:n], AF.Identity, [r_bank[b], r_colp], [r_u],
                        bias=bcol(C_BIN + g))
                tt("dve", u[:, 16:144], u[:, 16:144], vmask, ALU.mult, [r_u, r_cst], [r_u])
                src, r_src = u, r_u
                bufs = [(sA, r_sA), (sB, r_sB)]
                sh = 1
                for step in range(g + 1):
                    dst, r_dst = bufs[step % 2]
                    tt("dve", dst[:, 16:16 + T], src[:, 16:16 + T], src[:, 16 - sh:16 + T - sh], ALU.add,
                       [r_src], [r_dst])
                    src, r_src = dst, r_dst
                    sh *= 2
                w_ = 2 ** (g + 1)
                tt("dve", src[:, 16 + 128:16 + 144], src[:, 16 + 128:16 + 144],
                   cst[:, K_PF + g * 16:K_PF + (g + 1) * 16], ALU.mult, [r_src, r_cst], [r_src])
                stt("dve", pooled[:, :], src[:, 16:16 + T], 1.0 / w_, u[:, 16:16 + T], ALU.mult, ALU.subtract,
                    [r_src, r_u], [r_pl])
                for cbi, (c0, n) in enumerate(CBS):
                    b = next_bank()
                    mm(bank(b)[:, 0:n], poolw[:, g, :], pooled[:, c0:c0 + n], True, True,
                       [r_poolw, r_pl], [r_bank[b]])
                    act(brT[:, g, c0:c0 + n], bank(b)[:, 0:n], AF.Identity, [r_bank[b], r_colp], [r_br[g][cbi]],
                        scale=bcol(C_PSC + g))
            RG.release(n_wp)
            n_pp, Pp, r_Pp = RG.acquire(4)
            gate_pass(0, Pp, r_Pp)
            RG.release(n_pp)

            AR.reset()
            n_wa, Wa, r_Wa = RG.acquire(8)
            n_wg, Wg, r_Wg = RG.acquire(8)
            diag = AR.alloc([128, 31, 128], BF16)
            hb = [AR.alloc([128, 32 + T], BF16) for _ in range(2)]
            sgc = [AR.alloc([128, 512], F32) for _ in range(2)]
            r_diag = R()
            r_hb = [R(), R()]
            r_sgc = [R(), R()]
            memset("pool", hb[0][:, 0:32], 0.0, [r_hb[0]])
            memset("pool", hb[1][:, 0:32], 0.0, [r_hb[1]])
            it = 0
            for c in range(4):
                h_, r_h = hb[c % 2], r_hb[c % 2]
                for k in range(31):
                    ts("dve", diag[:, k, :], ident_bf, bcol(C_CDW + c * 31 + k), None,
                       ALU.mult, None, [r_cbf, r_colp], [r_diag])
                for cbi, (c0, n) in enumerate(CBS):
                    ba = next_bank()
                    for kc in range(8):
                        mm(bank(ba)[:, 0:n], Wa[:, kc, c * 128:(c + 1) * 128], xT[:, kc, c0:c0 + n],
                           kc == 0, kc == 7, [r_Wa] + xT_regs(cbi), [r_bank[ba]])
                    bg = next_bank()
                    for kc in range(8):
                        mm(bank(bg)[:, 0:n], Wg[:, kc, c * 128:(c + 1) * 128], xT[:, kc, c0:c0 + n],
                           kc == 0, kc == 7, [r_Wg] + xT_regs(cbi), [r_bank[bg]])
                    k = it % 2
                    it += 1
                    act(sgc[k][:, 0:n], bank(bg)[:, 0:n], AF.Sigmoid, [r_bank[bg], r_colp], [r_sgc[k]],
                        bias=bcol(C_BIN + 8 + c))
                    stt("dve", h_[:, 32 + c0:32 + c0 + n], bank(ba)[:, 0:n], bcol(C_BIN + 4 + c), sgc[k][:, 0:n],
                        ALU.add, ALU.mult, [r_bank[ba], r_sgc[k], r_colp], [r_h])
                tt("dve", h_[:, 32:160], h_[:, 32:160], vmask, ALU.mult, [r_h, r_cst], [r_h])
                for cbi, (c0, n) in enumerate(CBS):
                    b = next_bank()
                    for k in range(31):
                        mm(bank(b)[:, 0:n], diag[:, k, :], h_[:, 32 + c0 - 30 + k:32 + c0 - 30 + k + n],
                           k == 0, k == 30, [r_diag, r_h], [r_bank[b]])
                    act(brT[:, c, c0:c0 + n], bank(b)[:, 0:n], AF.Identity, [r_bank[b], r_colp], [r_br[c][cbi]],
                        bias=bcol(C_CDWB + c))
            RG.release(n_wa)
            RG.release(n_wg)
            sq = AR.alloc([128, 4, 512], BF16)
            mean = AR.alloc([128, 512], F32)
            msq = AR.alloc([128, 512], F32)
            rstd = AR.alloc([128, 512], F32)
            zc = [AR.alloc([128, 512], F32) for _ in range(2)]
            r_sq, r_mean, r_msq, r_rstd = R(), R(), R(), R()
            r_zc = [R(), R()]
            for cbi, (c0, n) in enumerate(CBS):
                for c in range(4):
                    act(sq[:, c, 0:n], brT[:, c, c0:c0 + n], AF.Square, [r_br[c][cbi]], [r_sq])
                b1 = next_bank()
                for c in range(4):
                    mm(bank(b1)[:, 0:n], ones_bf, brT[:, c, c0:c0 + n], c == 0, c == 3,
                       [r_cbf, r_br[c][cbi]], [r_bank[b1]])
                b2 = next_bank()
                for c in range(4):
                    mm(bank(b2)[:, 0:n], ones_bf, sq[:, c, 0:n], c == 0, c == 3, [r_cbf, r_sq], [r_bank[b2]])
                ts("dve", mean[:, 0:n], bank(b1)[:, 0:n], 1.0 / 512, None, ALU.mult, None, [r_bank[b1]], [r_mean])
                tt("dve", msq[:, 0:n], mean[:, 0:n], mean[:, 0:n], ALU.mult, [r_mean], [r_msq])
                stt("dve", rstd[:, 0:n], bank(b2)[:, 0:n], 1.0 / 512, msq[:, 0:n], ALU.mult, ALU.subtract,
                    [r_bank[b2], r_msq], [r_rstd])
                ts("dve", rstd[:, 0:n], rstd[:, 0:n], 0.0, None, ALU.max, None, [r_rstd], [r_rstd])
                act(rstd[:, 0:n], rstd[:, 0:n], AF.Ln, [r_rstd, r_eps], [r_rstd], bias=epsc[:, 0:1])
                act(rstd[:, 0:n], rstd[:, 0:n], AF.Exp, [r_rstd], [r_rstd], scale=-0.5)
                for c in range(4):
                    k = c % 2
                    tt("dve", zc[k][:, 0:n], brT[:, c, c0:c0 + n], mean[:, 0:n], ALU.subtract,
                       [r_br[c][cbi], r_mean], [r_zc[k]])
                    tt("dve", zc[k][:, 0:n], zc[k][:, 0:n], rstd[:, 0:n], ALU.mult, [r_zc[k], r_rstd], [r_zc[k]])
                    act(brT[:, c, c0:c0 + n], zc[k][:, 0:n], AF.Silu, [r_zc[k], r_colp], [r_br[c][cbi]],
                        bias=bcol(C_CLB + c), scale=bcol(C_CLG + c))
            n_cp, Pc, r_Pc = RG.acquire(4)
            gate_pass(1, Pc, r_Pc, bias_cols=C_CPWB)
            RG.release(n_cp)

            AR.reset()
            n_wq, Wq, r_Wq = RG.acquire(8)
            qb = [AR.alloc([128, 512], BF16) for _ in range(2)]
            eb = [AR.alloc([128, 2, 512], BF16) for _ in range(2)]
            rden = [AR.alloc([128, 512], F32) for _ in range(2)]
            r_qb = [R(), R()]
            r_eb = [R(), R()]
            r_rden = [R(), R()]
            items = [(hd, cbi) for hd in range(4) for cbi in range(len(CBS))]

            def at_S1(t):
                hd, cbi = items[t]
                c0, n = CBS[cbi]
                k = t % 2
                b = next_bank()
                for kc in range(8):
                    mm(bank(b)[:, 0:n], Wq[:, kc, hd * 128:(hd + 1) * 128], xT[:, kc, c0:c0 + n],
                       kc == 0, kc == 7, [r_Wq] + xT_regs(cbi), [r_bank[b]])
                act(qb[k][:, 0:n], bank(b)[:, 0:n], AF.Identity, [r_bank[b], r_colp], [r_qb[k]],
                    bias=bcol(C_BIN + 12 + hd))

            def at_S2(t):
                hd, cbi = items[t]
                c0, n = CBS[cbi]
                k = t % 2
                for mc in range(2):
                    bs = next_bank()
                    mm(bank(bs)[:, 0:n], kT[:, hd, mc * 128:(mc + 1) * 128], qb[k][:, 0:n], True, True,
                       [r_kT, r_qb[k]], [r_bank[bs]])
                    act(eb[k][:, mc, 0:n], bank(bs)[:, 0:n], AF.Exp, [r_bank[bs]], [r_eb[k]], scale=ATT_SCALE)

            def at_S3(t):
                hd, cbi = items[t]
                c0, n = CBS[cbi]
                k = t % 2
                bo = next_bank()
                for mc in range(2):
                    mm(bank(bo)[:, 0:n], vv[:, mc, hd * 128:(hd + 1) * 128], eb[k][:, mc, 0:n],
                       mc == 0, mc == 1, [r_vv, r_eb[k]], [r_bank[bo]])
                bd = next_bank()
                for mc in range(2):
                    mm(bank(bd)[:, 0:n], ones_bf, eb[k][:, mc, 0:n], mc == 0, mc == 1,
                       [r_cbf, r_eb[k]], [r_bank[bd]])
                act(rden[k][:, 0:n], bank(bd)[:, 0:n], AF.Ln, [r_bank[bd]], [r_rden[k]])
                act(rden[k][:, 0:n], rden[k][:, 0:n], AF.Exp, [r_rden[k]], [r_rden[k]], scale=-1.0)
                tt("dve", brT[:, hd, c0:c0 + n], bank(bo)[:, 0:n], rden[k][:, 0:n], ALU.mult,
                   [r_bank[bo], r_rden[k]], [r_br[hd][cbi]])

            NI = len(items)
            for step in range(NI + 2):
                if step < NI:
                    at_S1(step)
                if 0 <= step - 1 < NI:
                    at_S2(step - 1)
                if 0 <= step - 2 < NI:
                    at_S3(step - 2)
            RG.release(n_wq)
            n_ao, Pa, r_Pa = RG.acquire(4)
            gate_pass(2, Pa, r_Pa)
            RG.release(n_ao)

            AR.reset()
            RG.set_alias(1)
            n_o0, Wo0, r_Wo0 = RG.acquire(8)
            n_o1, Wo1, r_Wo1 = RG.acquire(8)
            Wo = [(Wo0, r_Wo0), (Wo1, r_Wo1)]
            xr = [AR.alloc([128, D], F32) for _ in range(2)]
            zt = [AR.alloc([128, D], F32) for _ in range(2)]
            x1b = [AR.alloc([128, D], BF16) for _ in range(2)]
            x1T = AR.alloc([128, 8, 128], F32)
            lnt = [ln_tmp(), ln_tmp()]
            lg = AR.alloc([128, NE], F32)
            m8 = AR.alloc([128, 8], F32)
            nm = AR.alloc([128, 1], F32)
            Af = AR.alloc([128, NE], F32)
            ex = AR.alloc([128, NE], F32)
            ssum = AR.alloc([128, 1], F32)
            Abf = AR.alloc([128, NE], BF16)
            Asum = [AR.alloc([128, NE], BF16) for _ in range(2)]
            key = AR.alloc([128, NE], F32)
            k8 = AR.alloc([128, 8], F32)
            junk = AR.alloc([128, NE], F32)
            r_xr = [R(), R()]
            r_zt = [R(), R()]
            r_x1b = [R(), R()]
            r_x1T, r_lg, r_m8, r_nm, r_Af, r_ex, r_ss, r_Abf, r_key, r_k8, r_junk = (R() for _ in range(11))
            r_As = [R(), R()]
            scat_ops = []
            new_res = {}
            lg_all = AR.alloc([128, NT, NE], F32)
            r_lga = [R() for _ in range(NT)]

            x1b3 = [x1b[0], x1b[1], AR.alloc([128, D], BF16), AR.alloc([128, D], BF16)]
            r_x1b3 = [r_x1b[0], r_x1b[1], R(), R()]
            zt3 = [zt[0], zt[1], AR.alloc([128, D], F32)]
            r_zt3 = [r_zt[0], r_zt[1], R()]
            lnt3 = [lnt[0], lnt[1], ln_tmp()]

            def l1_A(i):
                k = i % 2
                z3 = i % 3
                src_t, src_op = res_src[i]
                dma("sp", xr[k], src_t[i * 128:(i + 1) * 128, :], (), [r_xr[k]], deps=[src_op])
                b = 2 * k
                for half in range(2):
                    W_, r_W = Wo[half]
                    for kc in range(8):
                        mm(bank(b + half), mergedT[:, kc, i * 128:(i + 1) * 128], W_[:, kc, :],
                           kc == 0, False, [r_mg[i], r_W], [r_bank[b + half]])
                    mm(bank(b + half), onesrow_f, rowsm[0:1, half * 512:(half + 1) * 512], False, True,
                       [r_cst, r_rowsm], [r_bank[b + half]])
                for half in range(2):
                    stt("dve", zt3[z3][:, half * 512:(half + 1) * 512], xr[k][:, half * 512:(half + 1) * 512], ALPHA,
                        bank(b + half), ALU.mult, ALU.add, [r_xr[k], r_bank[b + half]], [r_zt3[z3]])
                ln_stats(zt3[z3], r_zt3[z3], lnt3[z3])

            def l1_B1(i):
                z3 = i % 3
                ln_norm(zt3[z3], r_zt3[z3], lnt3[z3])

            def l1_Bg(i):
                z3 = i % 3
                k4 = i % 4
                ln_affine(zt3[z3], r_zt3[z3], lnbc[:, 0:D], lnbc[:, D:2 * D], r_lnbc)
                st_op = dma("sp", xres[i * 128:(i + 1) * 128, :], zt3[z3], [r_zt3[z3]], ())
                new_res[i] = (xres, st_op)
                cp("act", x1b3[k4], zt3[z3], [r_zt3[z3]], [r_x1b3[k4]])

            def l1_Bt1(i):
                z3 = i % 3
                b = 4
                for kc in range(8):
                    bb = b + kc // 4
                    tr(bank(bb)[:, (kc % 4) * 128:(kc % 4 + 1) * 128], zt3[z3][:, kc * 128:(kc + 1) * 128], ident_f,
                       [r_zt3[z3], r_cst], [r_bank[bb]])
                cp("act", x1T[:, 0:4, :], bank(b).rearrange("p (a b) -> p a b", a=4), [r_bank[b]], [r_x1T])
                cp("act", x1T[:, 4:8, :], bank(b + 1).rearrange("p (a b) -> p a b", a=4), [r_bank[b + 1]], [r_x1T])

            def l1_Bt2(i):
                c0_ = (i % 4) * NE
                for kc in range(8):
                    mm(bank(6)[:, c0_:c0_ + NE], x1T[:, kc, :], wr[:, kc, :], kc == 0, False,
                       [r_x1T, r_wr], [r_bank[6]])
                mm(bank(6)[:, c0_:c0_ + NE], onesrow_f, rowsm[0:1, D:D + NE], False, True,
                   [r_cst, r_rowsm], [r_bank[6]])

            def l1_C(i):
                k = i % 2
                c0_ = (i % 4) * NE
                lg = lg_all[:, i, :]
                r_lg = r_lga[i]
                cp("dve", lg, bank(6)[:, c0_:c0_ + NE], [r_bank[6]], [r_lg])
                S.op("dve", lambda e: e.max(out=m8, in_=lg), [r_lg], [r_m8])
                ts("dve", Af, lg, m8[:, 3:4], None, ALU.is_ge, None, [r_lg, r_m8], [r_Af])
                if i == 0:
                    ts("dve", Af, Af, vtok, None, ALU.mult, None, [r_Af, r_cst], [r_Af])
                ts("dve", nm, m8[:, 0:1], -1.0, None, ALU.mult, None, [r_m8], [r_nm])
                act(ex, lg, AF.Exp, [r_lg, r_nm], [r_ex], bias=nm[:, 0:1])
                stt("dve", ex, Af, 1.0, ex, ALU.mult, ALU.mult, [r_Af, r_ex], [r_ex, r_ss], accum_out=ssum)
                ts("dve", ssum, ssum, 1e-30, None, ALU.max, None, [r_ss], [r_ss])
                S.op("dve", lambda e: e.reciprocal(out=ssum, in_=ssum), [r_ss], [r_ss])
                ts("dve", G_all[:, i, :], ex, ssum[:, 0:1], None, ALU.mult, None, [r_ex, r_ss], [r_G[i]])
                cp("dve", Abf, Af, [r_Af], [r_Abf])
                if i > 0:
                    tt("dve", Asum[k], Asum[(i - 1) % 2], Abf, ALU.add, [r_As[(i - 1) % 2], r_Abf], [r_As[k]])
                else:
                    cp("dve", Asum[k], Abf, [r_Abf], [r_As[k]])

            def l1_Dm(i):
                c0_ = (i % 4) * NE
                pp = bank(7)[:, c0_:c0_ + NE]
                mm(pp, tri_bf, Abf, True, i == 0, [r_cbf, r_Abf], [r_bank[7]])
                if i > 0:
                    mm(pp, ones_bf, Asum[(i - 1) % 2], False, True,
                       [r_cbf, r_As[(i - 1) % 2]], [r_bank[7]])

            def l1_D(i):
                k3 = i % 4
                c0_ = (i % 4) * NE
                pp = bank(7)[:, c0_:c0_ + NE]
                stt("dve", key, pp, float(CAP - 1), base1, ALU.min, ALU.add,
                    [r_bank[7], r_cst], [r_key])
                tt("dve", key, key, Af, ALU.mult, [r_key, r_Af], [r_key])
                S.op("dve", lambda e: e.max(out=k8, in_=key), [r_key], [r_k8])
                cp("dve", idx_all[:, i * 4:(i + 1) * 4], k8[:, 0:4], [r_k8], [r_idx[i]])
                for kk in range(4):
                    stt("dve", junk, key, k8[:, kk:kk + 1], G_all[:, i, :], ALU.is_equal, ALU.mult,
                        [r_key, r_k8, r_G[i]], [r_junk, r_gate[i]],
                        accum_out=gate_all[:, i * 4 + kk:i * 4 + kk + 1])
                for kk in range(4):
                    o = S.dma("pool", lambda e, s_=x1b3[k3], ix=idx_all[:, i * 4 + kk:i * 4 + kk + 1]:
                              e.indirect_dma_start(out=xd[:, :],
                                                   out_offset=bass.IndirectOffsetOnAxis(ap=ix, axis=0),
                                                   in_=s_, in_offset=None,
                                                   bounds_check=regs["bc"], oob_is_err=False),
                              [r_x1b3[k3], r_idx[i]], ())
                    scat_ops.append(o)

            for step in range(NT + 4):
                if 0 <= step - 2 < NT:
                    l1_Bt1(step - 2)
                if 0 <= step - 1 < NT:
                    l1_B1(step - 1)
                if 0 <= step - 4 < NT:
                    l1_Dm(step - 4)
                    l1_D(step - 4)
                if 0 <= step - 3 < NT:
                    l1_C(step - 3)
                if step < NT:
                    l1_A(step)
                if 0 <= step - 1 < NT:
                    l1_Bg(step - 1)
                if 0 <= step - 2 < NT:
                    l1_Bt2(step - 2)
            RG.release(n_o0)
            RG.release(n_o1)
            res_src = new_res

            AR.reset()
            RG.set_alias(2)
            brflat = brT[:, :, :].rearrange("p a b -> p (a b)")
            xg = [brflat[:, q_ * 3 * D:(q_ + 1) * 3 * D].rearrange("p (a b) -> p a b", a=3) for q_ in range(2)]
            xgT = [AR.alloc([128, 8, CAP], BF16) for _ in range(2)]
            actT = [AR.alloc([128, 8, CAP], BF16) for _ in range(2)]
            gc = [AR.alloc([128, CAP], F32) for _ in range(2)]
            sgm = [AR.alloc([128, CAP], F32) for _ in range(2)]
            t1 = [AR.alloc([128, CAP], F32) for _ in range(2)]
            ybf = [AR.alloc([128, D], BF16) for _ in range(2)]
            r_xg = [R(), R()]
            r_xgT = [R(), R()]
            r_actT = [R(), R()]
            r_gc = [R(), R()]
            r_sgm = [R(), R()]
            r_t1 = [R(), R()]
            r_ybf = [R(), R()]
            ys_ops = []
            cnt_ = {"it": 0, "ity": 0}

            def load_xg(e_):
                p = e_ % 2
                r0 = 1 + e_ * CAP
                dma("sp", xg[p], xd[r0:r0 + CAP, :].rearrange("(t p) d -> p t d", p=128), (), [r_xg[p]],
                    deps=scat_ops if e_ < 2 else ())

            def transposes(e_):
                p = e_ % 2
                for kp in range(4):
                    b = next_bank()
                    pb = bank_bf(b)
                    for kc2 in range(2):
                        kc = kp * 2 + kc2
                        for ti in range(3):
                            tr(pb[:, kc2 * CAP + ti * 128:kc2 * CAP + (ti + 1) * 128],
                               xg[p][:, ti, kc * 128:(kc + 1) * 128], ident_bf, [r_xg[p], r_cbf], [r_bank[b]])
                    cp("act" if kp % 2 else "dve", xgT[p][:, kp * 2:kp * 2 + 2, :],
                       pb[:, 0:2 * CAP].rearrange("p (a b) -> p a b", a=2), [r_bank[b]], [r_xgT[p]])

            def up(e_, hf):
                p = e_ % 2
                n_ug, Ug, r_Ug = RG.acquire(8)
                n_ul, Ul, r_Ul = RG.acquire(8)
                for jj in range(4):
                    j = hf * 4 + jj
                    bg = next_bank()
                    for kc in range(8):
                        mm(bank(bg)[:, 0:CAP], Ug[:, kc, jj * 128:(jj + 1) * 128], xgT[p][:, kc, :],
                           kc == 0, kc == 7, [r_Ug, r_xgT[p]], [r_bank[bg]])
                    bl = next_bank()
                    for kc in range(8):
                        mm(bank(bl)[:, 0:CAP], Ul[:, kc, jj * 128:(jj + 1) * 128], xgT[p][:, kc, :],
                           kc == 0, kc == 7, [r_Ul, r_xgT[p]], [r_bank[bl]])
                    k = cnt_["it"] % 2
                    cnt_["it"] += 1
                    ts("dve", gc[k], bank(bg)[:, 0:CAP], bcol(C_UPB + e_ * 16 + j), 7.0, ALU.add, ALU.min,
                       [r_bank[bg], r_colp], [r_gc[k]])
                    act(sgm[k], gc[k], AF.Sigmoid, [r_gc[k]], [r_sgm[k]], scale=1.702)
                    act(t1[k], bank(bl)[:, 0:CAP], AF.Identity, [r_bank[bl], r_upb1], [r_t1[k]],
                        bias=upb1[:, e_ * 8 + j:e_ * 8 + j + 1])
                    ts("dve", t1[k], t1[k], -6.0, 8.0, ALU.max, ALU.min, [r_t1[k]], [r_t1[k]])
                    tt("dve", gc[k], gc[k], sgm[k], ALU.mult, [r_gc[k], r_sgm[k]], [r_gc[k]])
                    tt("dve", actT[p][:, j, :], t1[k], gc[k], ALU.mult, [r_t1[k], r_gc[k]], [r_actT[p]])
                RG.release(n_ug)
                RG.release(n_ul)

            def down(e_):
                p = e_ % 2
                r0 = 1 + e_ * CAP
                n_d0, D0, r_D0 = RG.acquire(8)
                n_d1, D1, r_D1 = RG.acquire(8)
                Dn = [(D0, r_D0), (D1, r_D1)]
                for ti in range(3):
                    b = next_bank2()
                    for half in range(2):
                        W_, r_W = Dn[half]
                        for kc in range(8):
                            mm(bank(b + half), actT[p][:, kc, ti * 128:(ti + 1) * 128], W_[:, kc, :],
                               kc == 0, kc == 7, [r_actT[p], r_W], [r_bank[b + half]])
                    k = cnt_["ity"] % 2
                    cnt_["ity"] += 1
                    cp("act", ybf[k][:, 0:512], bank(b), [r_bank[b]], [r_ybf[k]])
                    cp("dve", ybf[k][:, 512:1024], bank(b + 1), [r_bank[b + 1]], [r_ybf[k]])
                    o = dma("sp", ys[r0 + ti * 128:r0 + (ti + 1) * 128, :], ybf[k], [r_ybf[k]], ())
                    ys_ops.append(o)
                RG.release(n_d0)
                RG.release(n_d1)

            load_xg(0)
            load_xg(1)
            transposes(0)
            up(0, 0)
            up(0, 1)
            for e_ in range(NE):
                if e_ + 1 < NE:
                    if e_ + 2 < NE:
                        load_xg(e_ + 2)
                    transposes(e_ + 1)
                    up(e_ + 1, 0)
                down(e_)
                if e_ + 1 < NE:
                    up(e_ + 1, 1)

            RG.set_alias(0)
            AR.reset()
            yk = [AR.alloc([128, 4, D], BF16) for _ in range(2)]
            xr = [AR.alloc([128, D], F32) for _ in range(2)]
            zt = [AR.alloc([128, D], F32) for _ in range(2)]
            x2b = [AR.alloc([128, D], BF16) for _ in range(2)]
            GT2 = [AR.alloc([NE, 128], F32), AR.alloc([NE, 128], F32)]
            r_GT2 = [R(), R()]
            lnt = [ln_tmp(), ln_tmp()]
            r_yk = [[R() for _ in range(4)] for _ in range(2)]
            r_xr = [R(), R()]
            r_zt = [R(), R()]
            r_x2b = [R(), R()]
            new_res = {}

            def l2_G(i):
                k = i % 2
                for kk in range(4):
                    S.dma("pool", lambda e, d_=yk[k][:, kk, :], ix=idx_all[:, i * 4 + kk:i * 4 + kk + 1]:
                          e.indirect_dma_start(out=d_, out_offset=None, in_=ys[:, :],
                                               in_offset=bass.IndirectOffsetOnAxis(ap=ix, axis=0),
                                               bounds_check=regs["bc"], oob_is_err=False),
                          [r_idx[i]], [r_yk[k][kk]], deps=ys_ops if (i == 0 and kk == 0) else ())
                src_t, src_op = res_src[i]
                dma("sp", xr[k], src_t[i * 128:(i + 1) * 128, :], (), [r_xr[k]], deps=[src_op])

            def l2_A0(i):
                k = i % 2
                tr(bank(4)[0:NE, 0:128], G_all[:, i, :], ident_f, [r_G[i], r_cst], [r_bank[4]])
                cp("act", GT2[k], bank(4)[0:NE, 0:128], [r_bank[4]], [r_GT2[k]])
                b = 2 * k
                for half in range(2):
                    mm(bank(b + half), GT2[k], bdown[:, half * 512:(half + 1) * 512], True, True,
                       [r_GT2[k], r_bdown], [r_bank[b + half]])

            def l2_A(i):
                k = i % 2
                b = 2 * k
                for half in range(2):
                    stt("dve", zt[k][:, half * 512:(half + 1) * 512], xr[k][:, half * 512:(half + 1) * 512], ALPHA,
                        bank(b + half), ALU.mult, ALU.add, [r_xr[k], r_bank[b + half]], [r_zt[k]])
                for kk in range(4):
                    stt("dve", zt[k], yk[k][:, kk, :], gate_all[:, i * 4 + kk:i * 4 + kk + 1], zt[k],
                        ALU.mult, ALU.add, [r_yk[k][kk], r_gate[i], r_zt[k]], [r_zt[k]])
                ln_stats(zt[k], r_zt[k], lnt[k])

            def l2_B1(i):
                k = i % 2
                ln_norm(zt[k], r_zt[k], lnt[k])

            def l2_B2(i):
                k = i % 2
                ln_affine(zt[k], r_zt[k], lnbc[:, 2 * D:3 * D], lnbc[:, 3 * D:4 * D], r_lnbc)
                if last:
                    if i > 0:
                        o = dma("sp", out_d[(i - 1) * 128:i * 128, :], zt[k], [r_zt[k]], ())
                        final_ops.append(o)
                else:
                    st_op = dma("sp", xres[i * 128:(i + 1) * 128, :], zt[k], [r_zt[k]], ())
                    new_res[i] = (xres, st_op)
                    cp("act", x2b[k], zt[k], [r_zt[k]], [r_x2b[k]])
                    b = 5 + k
                    pb = bank_bf(b)
                    for kc in range(8):
                        tr(pb[:, kc * 128:(kc + 1) * 128], x2b[k][:, kc * 128:(kc + 1) * 128], ident_bf,
                           [r_x2b[k], r_cbf], [r_bank[b]])
                    cp("act", xT[:, :, i * 128:(i + 1) * 128], pb.rearrange("p (a b) -> p a b", a=8),
                       [r_bank[b]], [r_xT[i]])

            l2_G(0)
            l2_A0(0)
            for step in range(NT + 1):
                if step + 1 < NT:
                    l2_G(step + 1)
                if 0 <= step - 1 < NT:
                    l2_B1(step - 1)
                if step < NT:
                    l2_A(step)
                if step + 1 < NT:
                    l2_A0(step + 1)
                if 0 <= step - 1 < NT:
                    l2_B2(step - 1)
            res_src = new_res

        assert RG.next_acq == len(plan), (RG.next_acq, len(plan))
        S.finalize(final_ops)
    return nc


def _consts(half):
    c = np.zeros((128, NCST), np.float32)
    c[:, K_ID:K_ID + 128] = np.eye(128, dtype=np.float32)
    tp = np.arange(128)
    c[:, K_TRI:K_TRI + 128] = (tp[:, None] < tp[None, :]).astype(np.float32)
    c[:, K_ONE:K_ONE + 128] = 1.0
    valid = 0.0 if half == 0 else 1.0
    c[:, K_VM:K_VM + 128] = valid
    c[:, K_VT] = valid
    for g, w in enumerate((2, 4, 8, 16)):
        for t in range(16):
            cnt = min(t + 1, w) if half == 0 else w
            c[:, K_PF + g * 16 + t] = w / cnt
    c[:, K_B1:K_B1 + 32] = (1 + np.arange(NE) * CAP)[None, :].astype(np.float32)
    return c


def _layer_params(inp, ls):
    f = lambda a: np.ascontiguousarray(np.asarray(a, dtype=np.float32))
    colp = np.zeros((len(ls), 128, NCOLP), np.float32)
    for n, l in enumerate(ls):
        colp[n, :, C_BIN:C_BIN + 40] = inp["b_in"][l].reshape(40, 128).T
        colp[n, :, C_PSC:C_PSC + 4] = inp["pool_scale"][l].reshape(4, 128).T
        colp[n, :, C_CDW:C_CDW + 124] = inp["conv_dw"][l].T.reshape(4, 128, 31).transpose(1, 0, 2).reshape(128, 124)
        colp[n, :, C_CDWB:C_CDWB + 4] = inp["conv_dw_b"][l].reshape(4, 128).T
        colp[n, :, C_CLG:C_CLG + 4] = inp["conv_ln_g"][l].reshape(4, 128).T
        colp[n, :, C_CLB:C_CLB + 4] = inp["conv_ln_b"][l].reshape(4, 128).T
        colp[n, :, C_CPWB:C_CPWB + 8] = inp["conv_pw_b"][l].reshape(8, 128).T
        colp[n, :, C_UPB:C_UPB + 512] = inp["exp_up_b"][l].reshape(NE, 16, 128).transpose(2, 0, 1).reshape(128, 512)
    rowbc = np.stack([np.concatenate([inp["ln1_g"][l], inp["ln1_b"][l], inp["ln2_g"][l], inp["ln2_b"][l]])[None, :]
                      for l in ls]).astype(np.float32)
    rowsm = np.stack([np.concatenate([inp["b_out"][l], inp["router_b"][l]])[None, :] for l in ls]).astype(np.float32)
    sl = slice(ls[0], ls[-1] + 1)
    d = {
        "w_in": f(inp["w_in"][sl]), "pool_w": f(inp["pool_w"][sl]), "pool_proj": f(inp["pool_proj"][sl]),
        "conv_pw": f(inp["conv_pw"][sl]), "attn_o": f(inp["attn_o"][sl]), "w_kv": f(inp["w_kv"][sl]),
        "w_out": f(inp["w_out"][sl]), "exp_up": f(inp["exp_up"][sl]), "exp_down": f(inp["exp_down"][sl]),
        "router_w": f(inp["router_w"][sl]), "exp_down_b": f(inp["exp_down_b"][sl]),
        "colp": colp, "rowbc": rowbc, "rowsm": rowsm,
    }
    return d


def _x_shards(xfull):
    outs = []
    for c in range(8):
        b, half = c // 2, c % 2
        xs = np.zeros((T, D), np.float32)
        s0 = half * 2048
        xs[128:] = xfull[b, s0:s0 + 2048]
        if half == 1:
            xs[:128] = xfull[b, s0 - 128:s0]
        outs.append(xs)
    return outs


_PROG_CACHE = {}


def _get_prog(NL):
    if NL not in _PROG_CACHE:
        _PROG_CACHE[NL] = build_program(NL)
    return _PROG_CACHE[NL]


LAYERS_PER_LAUNCH = 4


def kernel(**inp):
    inp = {k: np.asarray(v) for k, v in inp.items()}
    x = np.asarray(inp["x"], np.float32)
    memln = np.concatenate([inp["mem_ln_g"], inp["mem_ln_b"]])[None, :].astype(np.float32)
    NL = LAYERS_PER_LAUNCH
    nc = _get_prog(NL)
    csts = [_consts(c % 2) for c in range(8)]
    for l0 in range(0, DEPTH, NL):
        lp = _layer_params(inp, list(range(l0, l0 + NL)))
        xs = _x_shards(x)
        in_maps = []
        for c in range(8):
            m = dict(lp)
            m["x_in"] = xs[c]
            m["mem"] = np.ascontiguousarray(inp["mem"][c // 2], dtype=np.float32)
            m["memln"] = memln
            m["cst"] = csts[c]
            in_maps.append(m)
        res = run_bass_kernel_spmd(nc, in_maps, core_ids=list(range(8)))
        xn = np.empty_like(x)
        for c in range(8):
            b, half = c // 2, c % 2
            xn[b, half * 2048:(half + 1) * 2048] = np.asarray(res.results[c]["out"], np.float32)
        x = xn
    return x
```

```python
import contextlib
import numpy as np
import ml_dtypes
import concourse.bass as bass
import concourse.mybir as mybir
from concourse.bass_utils import run_bass_kernel_spmd

F32 = mybir.dt.float32
BF16 = mybir.dt.bfloat16
I32 = mybir.dt.int32
AF = mybir.ActivationFunctionType
ALU = mybir.AluOpType

D = 1024
NT = 17
T = NT * 128
SEQ = 4096
DEPTH = 4
NE = 32
CAP = 384
NSLOT = 1 + NE * CAP
IN_COLS = 5120
MEM = 256
ALPHA = (2.0 * DEPTH) ** 0.25
LN_EPS = 1e-5
CBS = [(0, 512), (512, 512), (1024, 512), (1536, 512), (2048, 128)]
NW = 4
ATT_SCALE = 128 ** -0.5

C_BIN = 0
C_PSC = 40
C_CDW = 44
C_CDWB = 168
C_CLG = 172
C_CLB = 176
C_CPWB = 180
C_UPB = 188
NCOLP = 700
K_ID = 0
K_TRI = 128
K_ONE = 256
K_VM = 384
K_VT = 512
K_PF = 513
K_B1 = 577
NCST = 609


ENGS = ("pe", "act", "dve", "pool", "sp")


class Reg:
    __slots__ = ("w", "r", "rd")

    def __init__(self):
        self.w = None
        self.r = {}
        self.rd = []


class Op:
    __slots__ = ("eng", "fn", "deps", "is_dma", "sem", "val", "needed", "epoch")


class Sched:
    def __init__(self, nc, n_dma_sems=20):
        self.nc = nc
        self.q = {e: [] for e in ENGS}
        self.n_dma_sems = n_dma_sems
        self.epoch = 0
        self.pending = {}
        self.last_compute = {}
        self.dma_since_barrier = []
        self.prologue = {}

    def _mk(self, eng, fn, reads, writes, deps, is_dma, bar=True):
        o = Op()
        o.eng = eng
        o.fn = fn
        o.is_dma = is_dma
        o.sem = None
        o.val = None
        o.needed = is_dma
        o.epoch = self.epoch
        d = [x for x in deps if x is not None]
        for r in reads:
            if r.w is not None:
                d.append(r.w)
        for w in writes:
            if w.w is not None:
                d.append(w.w)
            d.extend(w.r.values())
            d.extend(w.rd)
        pb = self.pending.pop(eng, None)
        if pb:
            d.extend(pb)
        o.deps = d
        for r in reads:
            if is_dma:
                r.rd.append(o)
            else:
                r.r[eng] = o
        for w in writes:
            w.w = o
            w.r = {}
            w.rd = []
        self.q[eng].append(o)
        if is_dma:
            if bar:
                self.dma_since_barrier.append(o)
        else:
            self.last_compute[eng] = o
        return o

    def op(self, eng, fn, reads=(), writes=(), deps=()):
        return self._mk(eng, fn, reads, writes, deps, False)

    def dma(self, eng, fn, reads=(), writes=(), deps=(), bar=True):
        return self._mk(eng, fn, reads, writes, deps, True, bar)

    def barrier(self):
        lst = list(self.last_compute.values()) + list(self.dma_since_barrier)
        self.dma_since_barrier = []
        for e in ENGS:
            self.pending[e] = list(lst) + self.pending.get(e, [])

    def finalize(self, final_ops):
        nc = self.nc
        for e in ENGS:
            for o in self.q[e]:
                for d in o.deps:
                    d.needed = True
        for o in final_ops:
            o.needed = True
        n_epochs = self.epoch + 1
        with contextlib.ExitStack() as stack:
            sems = {}
            for e in ENGS:
                for ep in range(n_epochs):
                    sems[(e, ep)] = stack.enter_context(nc.semaphore(f"s_{e}{ep}"))
            dsems = {}
            for e in ("sp", "act", "pool"):
                for i in range(self.n_dma_sems):
                    dsems[(e, i)] = stack.enter_context(nc.semaphore(f"d_{e}{i}"))
            cnt = {}
            slot_next = {e: 0 for e in ENGS}
            slot_cnt = {}
            slot_last = {}
            for e in ENGS:
                for o in self.q[e]:
                    if o.is_dma:
                        s = slot_next[e]
                        slot_next[e] = (s + 1) % self.n_dma_sems
                        key = (e, s)
                        prev = slot_last.get(key)
                        if prev is not None:
                            o.deps.append(prev)
                        slot_cnt[key] = slot_cnt.get(key, 0) + 16
                        slot_last[key] = o
                        o.sem = dsems[key]
                        o.val = slot_cnt[key]
                    elif o.needed:
                        k = (e, o.epoch)
                        cnt[k] = cnt.get(k, 0) + 1
                        o.sem = sems[k]
                        o.val = cnt[k]
            self.max_counts = cnt
            block = stack.enter_context(nc.Block())
            engmap = {"pe": block.tensor, "act": block.scalar, "dve": block.vector,
                      "pool": block.gpsimd, "sp": block.sync}

            def make(e):
                def body(eng):
                    seen = {}
                    for pf in self.prologue.get(e, ()):
                        pf(eng)
                    for o in self.q[e]:
                        for d in o.deps:
                            if (not d.is_dma) and d.eng == e and e == "pe":
                                continue
                            k = id(d.sem)
                            if seen.get(k, 0) >= d.val:
                                continue
                            seen[k] = d.val
                            eng.wait_ge(d.sem, d.val)
                        inst = o.fn(eng)
                        if o.sem is not None:
                            inst.then_inc(o.sem, 16 if o.is_dma else 1)
                    if e == "sp":
                        for o in final_ops:
                            if seen.get(id(o.sem), 0) >= o.val:
                                continue
                            seen[id(o.sem)] = o.val
                            eng.wait_ge(o.sem, o.val)
                return body

            for e in ENGS:
                engmap[e](make(e))


def build_program(NL, first_layer_is_input=True):
    nc = bass.Bass("TRN2", target_bir_lowering=False)
    dt_in = lambda n, s, d=F32: nc.dram_tensor(n, s, d, kind="ExternalInput")
    x_in = dt_in("x_in", [T, D])
    mem_in = dt_in("mem", [MEM, D])
    memln = dt_in("memln", [1, 2 * D])
    cst_in = dt_in("cst", [128, NCST])
    w_in = dt_in("w_in", [NL, D, IN_COLS])
    pool_w = dt_in("pool_w", [NL, 4, 128, 128])
    pool_proj = dt_in("pool_proj", [NL, 512, D])
    conv_pw = dt_in("conv_pw", [NL, 512, D])
    attn_o = dt_in("attn_o", [NL, 512, D])
    w_kv = dt_in("w_kv", [NL, D, D])
    w_out = dt_in("w_out", [NL, D, D])
    exp_up = dt_in("exp_up", [NL, NE, D, 2 * D])
    exp_down = dt_in("exp_down", [NL, NE, D, D])
    router_w = dt_in("router_w", [NL, D, NE])
    exp_down_b = dt_in("exp_down_b", [NL, NE, D])
    colp_in = dt_in("colp", [NL, 128, NCOLP])
    rowbc_in = dt_in("rowbc", [NL, 1, 4 * D])
    rowsm_in = dt_in("rowsm", [NL, 1, D + NE])
    out_d = nc.dram_tensor("out", [T - 128, D], F32, kind="ExternalOutput")
    xres = nc.dram_tensor("xres", [T, D], F32, kind="Internal")
    xd = nc.dram_tensor("xd", [NSLOT, D], BF16, kind="Internal")
    ys = nc.dram_tensor("ys", [NSLOT, D], BF16, kind="Internal")

    with contextlib.ExitStack() as st:
        sb = lambda n, s, d: st.enter_context(nc.sbuf_tensor("sb_" + n, s, d))
        S = Sched(nc)

        xT = sb("xT", [128, 8, T], BF16)
        mergedT = sb("mergedT", [128, 8, T], BF16)
        brT = sb("brT", [128, 4, T], BF16)
        ring = [sb(f"ring{i}", [128, 4096], BF16) for i in range(NW)]
        cst = sb("cst", [128, NCST], F32)
        cbf = sb("cbf", [128, 384], BF16)
        colp = sb("colp", [128, NCOLP], F32)
        upb1 = sb("upb1", [128, NE * 8], F32)
        lnbc = sb("lnbc", [128, 4 * D], F32)
        rowsm = sb("rowsm", [1, D + NE], F32)
        wr = sb("wr", [128, 8, NE], F32)
        bdown = sb("bdown", [NE, D], F32)
        memT = sb("memT", [128, 8, MEM], BF16)
        kT = sb("kT", [128, 4, MEM], BF16)
        vv = sb("vv", [128, 2, 512], BF16)
        poolw = sb("poolw", [128, 4, 128], BF16)
        idx_all = sb("idx_all", [128, NT * 4], I32)
        gate_all = sb("gate_all", [128, NT * 4], F32)
        G_all = sb("G_all", [128, NT, NE], F32)
        ARENA_BF = 24064
        arena = sb("arena", [128, ARENA_BF], BF16)
        ps = [st.enter_context(nc.psum_tensor(f"ps{i}", [128, 1024], F32)) for i in range(4)]

        epsc = sb("epsc", [128, 1], F32)
        r_eps = Reg()
        ident_bf = cbf[:, 0:128]
        tri_bf = cbf[:, 128:256]
        ones_bf = cbf[:, 256:384]
        ident_f = cst[:, K_ID:K_ID + 128]
        onesrow_f = cst[0:1, K_ONE:K_ONE + 128]
        vmask = cst[:, K_VM:K_VM + 128]
        vtok = cst[:, K_VT:K_VT + 1]
        base1 = cst[:, K_B1:K_B1 + 32]

        R = lambda: Reg()
        r_xT = [R() for _ in range(NT)]
        r_mg = [R() for _ in range(NT)]
        r_br = [[R() for _ in CBS] for _ in range(4)]
        r_ring = [R() for _ in range(NW)]
        r_cst, r_cbf, r_colp, r_upb1, r_lnbc, r_rowsm, r_wr, r_bdown = (R() for _ in range(8))
        r_memT, r_kT, r_vv, r_poolw = R(), R(), R(), R()
        r_idx = [R() for _ in range(NT)]
        r_gate = [R() for _ in range(NT)]
        r_G = [R() for _ in range(NT)]
        r_bank = [R() for _ in range(8)]

        def bank(b):
            return ps[b // 2][:, (b % 2) * 512:(b % 2) * 512 + 512]

        def bank_bf(b):
            return ps[b // 2][:, (b % 2) * 512:(b % 2) * 512 + 512].bitcast(BF16)

        bank_rr = [0]

        def next_bank():
            b = bank_rr[0]
            bank_rr[0] = (b + 1) % 8
            return b

        def next_bank2():
            b = bank_rr[0]
            if b % 2:
                b = (b + 1) % 8
            bank_rr[0] = (b + 2) % 8
            return b

        class Arena:
            def __init__(self):
                self.off = 0

            def reset(self):
                S.barrier()
                self.off = 0

            def alloc(self, shape, dtype):
                n = 1
                for s_ in shape[1:]:
                    n *= s_
                nb = n * (2 if dtype == BF16 else 4)
                self.off = (self.off + 63) // 64 * 64
                o_bf = self.off // 2
                self.off += nb
                assert self.off <= ARENA_BF * 2, f"arena overflow {self.off}"
                v = arena[0:shape[0], o_bf:o_bf + nb // 2]
                if dtype != BF16:
                    v = v.bitcast(dtype)
                if len(shape) == 3:
                    v = v.rearrange("p (a b) -> p a b", a=shape[1])
                return v

        AR = Arena()
        regs = {}
        S.prologue["pool"] = [lambda e: regs.__setitem__("bc", e.to_reg(NSLOT - 1))]

        def mm(out, lhsT, rhs, start, stop, reads, writes):
            return S.op("pe", lambda e: e.matmul(out, lhsT=lhsT, rhs=rhs, start=start, stop=stop),
                        reads, writes)

        def tr(out, in_, ident, reads, writes):
            return S.op("pe", lambda e: e.transpose(out=out, in_=in_, identity=ident), reads, writes)

        def act(out, in_, func, reads, writes, bias=None, scale=None, eng="act"):
            kw = {}
            if bias is not None:
                kw["bias"] = bias
            if scale is not None:
                kw["scale"] = scale
            return S.op(eng, lambda e: e.activation(out=out, in_=in_, func=func, **kw), reads, writes)

        def ts(eng, out, in0, s1, s2, op0, op1, reads, writes, accum_out=None):
            kw = {}
            if op1 is not None:
                kw["op1"] = op1
            if accum_out is not None:
                kw["accum_out"] = accum_out
            return S.op(eng, lambda e: e.tensor_scalar(out=out, in0=in0, scalar1=s1, scalar2=s2,
                                                       op0=op0, **kw), reads, writes)

        def tt(eng, out, in0, in1, op, reads, writes):
            return S.op(eng, lambda e: e.tensor_tensor(out=out, in0=in0, in1=in1, op=op), reads, writes)

        def stt(eng, out, in0, scalar, in1, op0, op1, reads, writes, accum_out=None):
            kw = {}
            if accum_out is not None:
                kw["accum_out"] = accum_out
            return S.op(eng, lambda e: e.scalar_tensor_tensor(out=out, in0=in0, scalar=scalar, in1=in1,
                                                              op0=op0, op1=op1, **kw), reads, writes)

        def cp(eng, out, in_, reads, writes):
            if eng == "act":
                return S.op(eng, lambda e: e.activation(out=out, in_=in_, func=AF.Copy), reads, writes)
            return S.op(eng, lambda e: e.tensor_copy(out=out, in_=in_), reads, writes)

        def memset(eng, ap, val, writes):
            return S.op(eng, lambda e: e.memset(ap, val), (), writes)

        def dma(eng, out, in_, reads, writes, deps=(), bar=True):
            return S.dma(eng, lambda e: e.dma_start(out=out, in_=in_), reads, writes, deps, bar)

        memset("dve", epsc[:, :], LN_EPS, [r_eps])

        plan = []
        kinds = []

        def kcview(ap2d):
            return ap2d.rearrange("(kc p) f -> p kc f", p=128)

        for l in range(NL):
            plan.append(kcview(w_kv[l][:, 0:512]))
            plan.append(kcview(w_kv[l][:, 512:1024]))
            plan.append(kcview(w_in[l][:, 0:512]))
            plan.append(kcview(pool_proj[l]))
            plan.append(kcview(w_in[l][:, 2048:2560]))
            plan.append(kcview(w_in[l][:, 2560:3072]))
            plan.append(kcview(w_in[l][:, 512:1024]))
            plan.append(kcview(w_in[l][:, 1024:1536]))
            plan.append(kcview(conv_pw[l]))
            plan.append(kcview(w_in[l][:, 3072:3584]))
            plan.append(kcview(w_in[l][:, 3584:4096]))
            plan.append(kcview(w_in[l][:, 1536:2048]))
            plan.append(kcview(attn_o[l]))
            plan.append(kcview(w_in[l][:, 4096:4608]))
            plan.append(kcview(w_in[l][:, 4608:5120]))
            plan.append(kcview(w_out[l][:, 0:512]))
            plan.append(kcview(w_out[l][:, 512:1024]))
            kinds.extend(["m"] * 17)
            def _up(e, hf):
                kinds.extend(["e"] * 2)
                plan.append(kcview(exp_up[l, e][:, hf * 512:(hf + 1) * 512]))
                plan.append(kcview(exp_up[l, e][:, 1024 + hf * 512:1024 + (hf + 1) * 512]))

            def _down(e):
                kinds.extend(["e"] * 2)
                plan.append(kcview(exp_down[l, e][:, 0:512]))
                plan.append(kcview(exp_down[l, e][:, 512:1024]))

            _up(0, 0)
            _up(0, 1)
            for e in range(NE):
                if e + 1 < NE:
                    _up(e + 1, 0)
                _down(e)
                if e + 1 < NE:
                    _up(e + 1, 1)

        NWE = NW + 8
        xTflat = xT[:, :, :].rearrange("p a b -> p (a b)")
        mgflat = mergedT[:, :, :].rearrange("p a b -> p (a b)")
        ringv = [ring[i][:, :] for i in range(NW)]
        ringv += [xTflat[:, q_ * 4096:(q_ + 1) * 4096] for q_ in range(4)]
        ringv += [mgflat[:, q_ * 4096:(q_ + 1) * 4096] for q_ in range(4)]
        r_ring.extend(Reg() for _ in range(8))

        class Ring:
            def __init__(self):
                self.next_load = 0
                self.next_acq = 0
                self.released = [False] * len(plan)
                self.allow_alias = 0
                self.slot = []
                self.prev = []
                last = {}
                bc = ec = 0
                for n, k_ in enumerate(kinds):
                    if k_ == "m":
                        sl = bc % NW
                        bc += 1
                    else:
                        sl = ec % NWE
                        ec += 1
                    self.slot.append(sl)
                    self.prev.append(last.get(sl))
                    last[sl] = n

            def _try_load(self):
                while self.next_load < len(plan):
                    n = self.next_load
                    pv = self.prev[n]
                    if pv is not None and not self.released[pv]:
                        break
                    sl = self.slot[n]
                    if sl >= NW + 4 and self.allow_alias < 2:
                        break
                    if sl >= NW and self.allow_alias < 1:
                        break
                    src = plan[n]
                    a = src.shape[1]
                    dst = ringv[sl].rearrange("p (a b) -> p a b", a=a)
                    dma("pool", dst, src, (), [r_ring[sl]], bar=False)
                    self.next_load += 1

            def acquire(self, a):
                self._try_load()
                n = self.next_acq
                assert n < self.next_load, "ring: item not loaded (release order problem)"
                assert plan[n].shape[1] == a
                self.next_acq += 1
                sl = self.slot[n]
                v = ringv[sl].rearrange("p (a b) -> p a b", a=a)
                return n, v, r_ring[sl]

            def release(self, n):
                self.released[n] = True
                self._try_load()

            def set_alias(self, level):
                self.allow_alias = level
                if level:
                    self._try_load()

        RG = Ring()

        dma("sp", cst[:, :], cst_in[:, :], (), [r_cst])
        cp("dve", cbf[:, :], cst[:, 0:384], [r_cst], [r_cbf])

        def ln_tmp():
            return (AR.alloc([128, 12], F32), AR.alloc([128, 2], F32), AR.alloc([128, 1], F32),
                    AR.alloc([128, 1], F32), R(), R(), R(), R())

        def ln_stats(zt, r_z, tmp_):
            stt_, mv, rs, nmr, r_s, r_mv, r_rs, r_nmr = tmp_
            S.op("dve", lambda e: e.bn_stats(out=stt_[:, 0:6], in_=zt[:, 0:512]), [r_z], [r_s])
            S.op("dve", lambda e: e.bn_stats(out=stt_[:, 6:12], in_=zt[:, 512:1024]), [r_z], [r_s])
            S.op("dve", lambda e: e.bn_aggr(out=mv, in_=stt_), [r_s], [r_mv])
            act(rs, mv[:, 1:2], AF.Sqrt, [r_mv, r_eps], [r_rs], bias=epsc[:, 0:1])

        def ln_norm(zt, r_z, tmp_):
            stt_, mv, rs, nmr, r_s, r_mv, r_rs, r_nmr = tmp_
            S.op("dve", lambda e: e.reciprocal(out=rs, in_=rs), [r_rs], [r_rs])
            ts("dve", nmr, mv[:, 0:1], rs[:, 0:1], -1.0, ALU.mult, ALU.mult, [r_mv, r_rs], [r_nmr])
            act(zt, zt, AF.Identity, [r_z, r_rs, r_nmr], [r_z], bias=nmr[:, 0:1], scale=rs[:, 0:1])

        def ln_affine(zt, r_z, gv, bv, r_gb):
            tt("dve", zt, zt, gv, ALU.mult, [r_z, r_gb], [r_z])
            tt("dve", zt, zt, bv, ALU.add, [r_z, r_gb], [r_z])

        def ln_apply(zt, r_z, gv, bv, r_gb, tmp_):
            ln_norm(zt, r_z, tmp_)
            ln_affine(zt, r_z, gv, bv, r_gb)

        def ln_rows(zt, r_z, out32, r_out, gv, bv, r_gb, tmp_):
            ln_stats(zt, r_z, tmp_)
            ln_apply(zt, r_z, gv, bv, r_gb, tmp_)

        def to_xT(src_bf, r_src, i):
            b = next_bank()
            pb = bank_bf(b)
            for kc in range(8):
                tr(pb[:, kc * 128:(kc + 1) * 128], src_bf[:, kc * 128:(kc + 1) * 128], ident_bf,
                   [r_src, r_cbf], [r_bank[b]])
            cp("act" if i % 2 else "dve", xT[:, :, i * 128:(i + 1) * 128],
               pb.rearrange("p (a b) -> p a b", a=8), [r_bank[b]], [r_xT[i]])

        AR.reset()
        mg_bc = AR.alloc([128, 2 * D], F32)
        r_mgbc = R()
        lnt = [ln_tmp(), ln_tmp()]
        zer = AR.alloc([128, 8 * D], BF16)
        r_zer = R()
        memset("pool", zer, 0.0, [r_zer])
        for j_ in range(NE * CAP // 1024):
            dma("sp", xd[1 + j_ * 1024:1 + (j_ + 1) * 1024, :].rearrange("(p r) d -> p r d", p=128),
                zer.rearrange("p (r d) -> p r d", r=8), [r_zer], ())
        dma("sp", xd[0:1, :], zer[0:1, 0:D], [r_zer], ())
        dma("sp", ys[0:1, :], zer[0:1, 0:D], [r_zer], ())
        dma("sp", mg_bc, memln[0:1, :].to_broadcast([128, 2 * D]), (), [r_mgbc])
        for mt in range(2):
            mtile = AR.alloc([128, D], F32)
            mbf = AR.alloc([128, D], BF16)
            r_mt, r_mb = R(), R()
            dma("sp", mtile, mem_in[mt * 128:(mt + 1) * 128, :], (), [r_mt])
            ln_rows(mtile, r_mt, mtile, r_mt, mg_bc[:, 0:D], mg_bc[:, D:2 * D], r_mgbc, lnt[mt])
            cp("act", mbf, mtile, [r_mt], [r_mb])
            b = next_bank()
            pb = bank_bf(b)
            for kc in range(8):
                tr(pb[:, kc * 128:(kc + 1) * 128], mbf[:, kc * 128:(kc + 1) * 128], ident_bf,
                   [r_mb, r_cbf], [r_bank[b]])
            cp("dve", memT[:, :, mt * 128:(mt + 1) * 128], pb.rearrange("p (a b) -> p a b", a=8),
               [r_bank[b]], [r_memT])

        AR.reset()
        xl = [AR.alloc([128, D], F32) for _ in range(2)]
        xb = [AR.alloc([128, D], BF16) for _ in range(2)]
        r_xl = [R(), R()]
        r_xb = [R(), R()]
        for i in range(NT):
            k = i % 2
            dma("sp", xl[k], x_in[i * 128:(i + 1) * 128, :], (), [r_xl[k]])
            cp("act" if i % 2 == 0 else "dve", xb[k], xl[k], [r_xl[k]], [r_xb[k]])
            to_xT(xb[k], r_xb[k], i)

        res_src = {i: (x_in, None) for i in range(NT)}
        final_ops = []

        for l in range(NL):
            last = (l == NL - 1)
            S.epoch += 1
            AR.reset()
            dma("sp", colp[:, :], colp_in[l], (), [r_colp])
            dma("sp", lnbc[:, :], rowbc_in[l].to_broadcast([128, 4 * D]), (), [r_lnbc])
            dma("sp", rowsm[:, :], rowsm_in[l], (), [r_rowsm])
            dma("sp", wr[:, :, :], router_w[l].rearrange("(kc p) e -> p kc e", p=128), (), [r_wr])
            dma("sp", bdown[:, :], exp_down_b[l], (), [r_bdown])
            dma("pool", poolw[:, :, :], pool_w[l].rearrange("g c d -> c g d"), (), [r_poolw])
            ts("dve", upb1[:, :].rearrange("p (e j) -> p e j", e=NE),
               colp[:, C_UPB:C_UPB + 512].rearrange("p (e j) -> p e j", e=NE)[:, :, 8:16],
               1.0, None, ALU.add, None, [r_colp], [r_upb1])

            def bcol(c):
                return colp[:, c:c + 1]

            n_k, Wk, r_Wk = RG.acquire(8)
            for hd in range(4):
                b = next_bank()
                for kc in range(8):
                    mm(bank(b)[:, 0:MEM], Wk[:, kc, hd * 128:(hd + 1) * 128], memT[:, kc, :],
                       kc == 0, kc == 7, [r_Wk, r_memT], [r_bank[b]])
                cp("act", kT[:, hd, :], bank(b)[:, 0:MEM], [r_bank[b]], [r_kT])
            RG.release(n_k)
            n_v, Wv, r_Wv = RG.acquire(8)
            for mc in range(2):
                b = next_bank()
                for kc in range(8):
                    mm(bank(b), memT[:, kc, mc * 128:(mc + 1) * 128], Wv[:, kc, :],
                       kc == 0, kc == 7, [r_Wv, r_memT], [r_bank[b]])
                cp("act", vv[:, mc, :], bank(b), [r_bank[b]], [r_vv])
            RG.release(n_v)

            def xT_regs(cbi):
                c0, n = CBS[cbi]
                return [r_xT[i] for i in range(c0 // 128, (c0 + n) // 128)]

            def mg_regs(cbi):
                c0, n = CBS[cbi]
                return [r_mg[i] for i in range(c0 // 128, (c0 + n) // 128)]

            def gate_pass(br, P, r_P, bias_cols=None):
                sg = [AR.alloc([128, 512], F32) for _ in range(2)]
                tmp = [AR.alloc([128, 512], F32) for _ in range(2)]
                r_sg = [R(), R()]
                r_tmp = [R(), R()]
                it = 0
                for half in range(2):
                    n_g, G, r_Gs = RG.acquire(8)
                    for jj in range(4):
                        j = half * 4 + jj
                        for cbi, (c0, n) in enumerate(CBS):
                            bg = next_bank()
                            for kc in range(8):
                                mm(bank(bg)[:, 0:n], G[:, kc, jj * 128:(jj + 1) * 128], xT[:, kc, c0:c0 + n],
                                   kc == 0, kc == 7, [r_Gs] + xT_regs(cbi), [r_bank[bg]])
                            by = next_bank()
                            for kc in range(4):
                                mm(bank(by)[:, 0:n], P[:, kc, j * 128:(j + 1) * 128], brT[:, kc, c0:c0 + n],
                                   kc == 0, kc == 3, [r_P] + [r_br[kc][cbi]], [r_bank[by]])
                            k = it % 2
                            it += 1
                            act(sg[k][:, 0:n], bank(bg)[:, 0:n], AF.Sigmoid, [r_bank[bg], r_colp], [r_sg[k]],
                                bias=bcol(C_BIN + 16 + br * 8 + j))
                            mdst = mergedT[:, j, c0:c0 + n]
                            if br == 0:
                                tt("dve", mdst, sg[k][:, 0:n], bank(by)[:, 0:n], ALU.mult,
                                   [r_sg[k], r_bank[by]], mg_regs(cbi))
                            else:
                                if bias_cols is not None:
                                    stt("dve", tmp[k][:, 0:n], bank(by)[:, 0:n], bcol(bias_cols + j), sg[k][:, 0:n],
                                        ALU.add, ALU.mult, [r_bank[by], r_sg[k], r_colp], [r_tmp[k]])
                                else:
                                    tt("dve", tmp[k][:, 0:n], sg[k][:, 0:n], bank(by)[:, 0:n], ALU.mult,
                                       [r_sg[k], r_bank[by]], [r_tmp[k]])
                                tt("dve", mdst, mdst, tmp[k][:, 0:n], ALU.add, [r_tmp[k]] + mg_regs(cbi),
                                   mg_regs(cbi))
                    RG.release(n_g)

            AR.reset()
            n_wp, Wp, r_Wp = RG.acquire(8)
            u2 = [AR.alloc([128, 16 + T], F32) for _ in range(2)]
            sA = AR.alloc([128, 16 + T], F32)
            sB = AR.alloc([128, 16 + T], F32)
            pooled2 = [AR.alloc([128, T], BF16) for _ in range(2)]
            r_u2 = [R(), R()]
            r_pl2 = [R(), R()]
            r_sA, r_sB = R(), R()
            memset("pool", u2[0][:, 0:16], 0.0, [r_u2[0]])
            memset("pool", u2[1][:, 0:16], 0.0, [r_u2[1]])
            memset("pool", sA[:, 0:16], 0.0, [r_sA])
            memset("pool", sB[:, 0:16], 0.0, [r_sB])

            def pb_U(g):
                u, r_u = u2[g % 2], r_u2[g % 2]
                for cbi, (c0, n) in enumerate(CBS):
                    b = next_bank()
                    for kc in range(8):
                        mm(bank(b)[:, 0:n], Wp[:, kc, g * 128:(g + 1) * 128], xT[:, kc, c0:c0 + n],
                           kc == 0, kc == 7, [r_Wp] + xT_regs(cbi), [r_bank[b]])
                    act(u[:, 16 + c0:16 + c0 + n], bank(b)[:, 0:n], AF.Identity, [r_bank[b], r_colp], [r_u],
                        bias=bcol(C_BIN + g))

            def pb_V(g):
                u, r_u = u2[g % 2], r_u2[g % 2]
                pooled, r_pl = pooled2[g % 2], r_pl2[g % 2]
                tt("dve", u[:, 16:144], u[:, 16:144], vmask, ALU.mult, [r_u, r_cst], [r_u])
                src, r_src = u, r_u
                bufs = [(sA, r_sA), (sB, r_sB)]
                sh = 1
                for step in range(g + 1):
                    dst, r_dst = bufs[step % 2]
                    tt("dve", dst[:, 16:16 + T], src[:, 16:16 + T], src[:, 16 - sh:16 + T - sh], ALU.add,
                       [r_src], [r_dst])
                    src, r_src = dst, r_dst
                    sh *= 2
                w_ = 2 ** (g + 1)
                tt("dve", src[:, 16 + 128:16 + 144], src[:, 16 + 128:16 + 144],
                   cst[:, K_PF + g * 16:K_PF + (g + 1) * 16], ALU.mult, [r_src, r_cst], [r_src])
                stt("dve", pooled[:, :], src[:, 16:16 + T], 1.0 / w_, u[:, 16:16 + T], ALU.mult, ALU.subtract,
                    [r_src, r_u], [r_pl])

            def pb_W(g):
                pooled, r_pl = pooled2[g % 2], r_pl2[g % 2]
                for cbi, (c0, n) in enumerate(CBS):
                    b = next_bank()
                    mm(bank(b)[:, 0:n], poolw[:, g, :], pooled[:, c0:c0 + n], True, True,
                       [r_poolw, r_pl], [r_bank[b]])
                    act(brT[:, g, c0:c0 + n], bank(b)[:, 0:n], AF.Identity, [r_bank[b], r_colp], [r_br[g][cbi]],
                        scale=bcol(C_PSC + g))

            pb_U(0)
            pb_U(1)
            pb_V(0)
            pb_W(0)
            pb_U(2)
            pb_V(1)
            pb_W(1)
            pb_U(3)
            pb_V(2)
            pb_W(2)
            pb_V(3)
            pb_W(3)
            AR.reset()
            RG.release(n_wp)
            n_pp, Pp, r_Pp = RG.acquire(4)
            gate_pass(0, Pp, r_Pp)
            RG.release(n_pp)

            AR.reset()
            n_wa, Wa, r_Wa = RG.acquire(8)
            n_wg, Wg, r_Wg = RG.acquire(8)
            diag = AR.alloc([128, 31, 128], BF16)
            hb = [AR.alloc([128, 32 + T], BF16) for _ in range(2)]
            sgc = [AR.alloc([128, 512], F32) for _ in range(2)]
            r_diag = R()
            r_hb = [R(), R()]
            r_sgc = [R(), R()]
            memset("pool", hb[0][:, 0:32], 0.0, [r_hb[0]])
            memset("pool", hb[1][:, 0:32], 0.0, [r_hb[1]])
            it = 0
            for c in range(4):
                h_, r_h = hb[c % 2], r_hb[c % 2]
                for k in range(31):
                    ts("dve", diag[:, k, :], ident_bf, bcol(C_CDW + c * 31 + k), None,
                       ALU.mult, None, [r_cbf, r_colp], [r_diag])
                for cbi, (c0, n) in enumerate(CBS):
                    ba = next_bank()
                    for kc in range(8):
                        mm(bank(ba)[:, 0:n], Wa[:, kc, c * 128:(c + 1) * 128], xT[:, kc, c0:c0 + n],
                           kc == 0, kc == 7, [r_Wa] + xT_regs(cbi), [r_bank[ba]])
                    bg = next_bank()
                    for kc in range(8):
                        mm(bank(bg)[:, 0:n], Wg[:, kc, c * 128:(c + 1) * 128], xT[:, kc, c0:c0 + n],
                           kc == 0, kc == 7, [r_Wg] + xT_regs(cbi), [r_bank[bg]])
                    k = it % 2
                    it += 1
                    act(sgc[k][:, 0:n], bank(bg)[:, 0:n], AF.Sigmoid, [r_bank[bg], r_colp], [r_sgc[k]],
                        bias=bcol(C_BIN + 8 + c))
                    stt("dve", h_[:, 32 + c0:32 + c0 + n], bank(ba)[:, 0:n], bcol(C_BIN + 4 + c), sgc[k][:, 0:n],
                        ALU.add, ALU.mult, [r_bank[ba], r_sgc[k], r_colp], [r_h])
                tt("dve", h_[:, 32:160], h_[:, 32:160], vmask, ALU.mult, [r_h, r_cst], [r_h])
                for cbi, (c0, n) in enumerate(CBS):
                    b = next_bank()
                    for k in range(31):
                        mm(bank(b)[:, 0:n], diag[:, k, :], h_[:, 32 + c0 - 30 + k:32 + c0 - 30 + k + n],
                           k == 0, k == 30, [r_diag, r_h], [r_bank[b]])
                    act(brT[:, c, c0:c0 + n], bank(b)[:, 0:n], AF.Identity, [r_bank[b], r_colp], [r_br[c][cbi]],
                        bias=bcol(C_CDWB + c))
            RG.release(n_wa)
            RG.release(n_wg)
            sq = AR.alloc([128, 4, 512], BF16)
            mean = AR.alloc([128, 512], F32)
            msq = AR.alloc([128, 512], F32)
            rstd = AR.alloc([128, 512], F32)
            zc = [AR.alloc([128, 512], F32) for _ in range(2)]
            r_sq, r_mean, r_msq, r_rstd = R(), R(), R(), R()
            r_zc = [R(), R()]
            for cbi, (c0, n) in enumerate(CBS):
                for c in range(4):
                    act(sq[:, c, 0:n], brT[:, c, c0:c0 + n], AF.Square, [r_br[c][cbi]], [r_sq])
                b1 = next_bank()
                for c in range(4):
                    mm(bank(b1)[:, 0:n], ones_bf, brT[:, c, c0:c0 + n], c == 0, c == 3,
                       [r_cbf, r_br[c][cbi]], [r_bank[b1]])
                b2 = next_bank()
                for c in range(4):
                    mm(bank(b2)[:, 0:n], ones_bf, sq[:, c, 0:n], c == 0, c == 3, [r_cbf, r_sq], [r_bank[b2]])
                ts("dve", mean[:, 0:n], bank(b1)[:, 0:n], 1.0 / 512, None, ALU.mult, None, [r_bank[b1]], [r_mean])
                tt("dve", msq[:, 0:n], mean[:, 0:n], mean[:, 0:n], ALU.mult, [r_mean], [r_msq])
                stt("dve", rstd[:, 0:n], bank(b2)[:, 0:n], 1.0 / 512, msq[:, 0:n], ALU.mult, ALU.subtract,
                    [r_bank[b2], r_msq], [r_rstd])
                ts("dve", rstd[:, 0:n], rstd[:, 0:n], 0.0, None, ALU.max, None, [r_rstd], [r_rstd])
                act(rstd[:, 0:n], rstd[:, 0:n], AF.Ln, [r_rstd, r_eps], [r_rstd], bias=epsc[:, 0:1])
                act(rstd[:, 0:n], rstd[:, 0:n], AF.Exp, [r_rstd], [r_rstd], scale=-0.5)
                for c in range(4):
                    k = c % 2
                    tt("dve", zc[k][:, 0:n], brT[:, c, c0:c0 + n], mean[:, 0:n], ALU.subtract,
                       [r_br[c][cbi], r_mean], [r_zc[k]])
                    tt("dve", zc[k][:, 0:n], zc[k][:, 0:n], rstd[:, 0:n], ALU.mult, [r_zc[k], r_rstd], [r_zc[k]])
                    act(brT[:, c, c0:c0 + n], zc[k][:, 0:n], AF.Silu, [r_zc[k], r_colp], [r_br[c][cbi]],
                        bias=bcol(C_CLB + c), scale=bcol(C_CLG + c))
            n_cp, Pc, r_Pc = RG.acquire(4)
            gate_pass(1, Pc, r_Pc, bias_cols=C_CPWB)
            RG.release(n_cp)

            AR.reset()
            n_wq, Wq, r_Wq = RG.acquire(8)
            qb = [AR.alloc([128, 512], BF16) for _ in range(2)]
            eb = [AR.alloc([128, 2, 512], BF16) for _ in range(2)]
            rden = [AR.alloc([128, 512], F32) for _ in range(2)]
            r_qb = [R(), R()]
            r_eb = [R(), R()]
            r_rden = [R(), R()]
            items = [(hd, cbi) for hd in range(4) for cbi in range(len(CBS))]

            def at_S1(t):
                hd, cbi = items[t]
                c0, n = CBS[cbi]
                k = t % 2
                b = next_bank()
                for kc in range(8):
                    mm(bank(b)[:, 0:n], Wq[:, kc, hd * 128:(hd + 1) * 128], xT[:, kc, c0:c0 + n],
                       kc == 0, kc == 7, [r_Wq] + xT_regs(cbi), [r_bank[b]])
                act(qb[k][:, 0:n], bank(b)[:, 0:n], AF.Identity, [r_bank[b], r_colp], [r_qb[k]],
                    bias=bcol(C_BIN + 12 + hd))

            def at_S2(t):
                hd, cbi = items[t]
                c0, n = CBS[cbi]
                k = t % 2
                for mc in range(2):
                    bs = next_bank()
                    mm(bank(bs)[:, 0:n], kT[:, hd, mc * 128:(mc + 1) * 128], qb[k][:, 0:n], True, True,
                       [r_kT, r_qb[k]], [r_bank[bs]])
                    act(eb[k][:, mc, 0:n], bank(bs)[:, 0:n], AF.Exp, [r_bank[bs]], [r_eb[k]], scale=ATT_SCALE)

            def at_S3(t):
                hd, cbi = items[t]
                c0, n = CBS[cbi]
                k = t % 2
                bo = next_bank()
                for mc in range(2):
                    mm(bank(bo)[:, 0:n], vv[:, mc, hd * 128:(hd + 1) * 128], eb[k][:, mc, 0:n],
                       mc == 0, mc == 1, [r_vv, r_eb[k]], [r_bank[bo]])
                bd = next_bank()
                for mc in range(2):
                    mm(bank(bd)[:, 0:n], ones_bf, eb[k][:, mc, 0:n], mc == 0, mc == 1,
                       [r_cbf, r_eb[k]], [r_bank[bd]])
                act(rden[k][:, 0:n], bank(bd)[:, 0:n], AF.Ln, [r_bank[bd]], [r_rden[k]])
                act(rden[k][:, 0:n], rden[k][:, 0:n], AF.Exp, [r_rden[k]], [r_rden[k]], scale=-1.0)
                tt("dve", brT[:, hd, c0:c0 + n], bank(bo)[:, 0:n], rden[k][:, 0:n], ALU.mult,
                   [r_bank[bo], r_rden[k]], [r_br[hd][cbi]])

            NI = len(items)
            for step in range(NI + 2):
                if step < NI:
                    at_S1(step)
                if 0 <= step - 1 < NI:
                    at_S2(step - 1)
                if 0 <= step - 2 < NI:
                    at_S3(step - 2)
            RG.release(n_wq)
            n_ao, Pa, r_Pa = RG.acquire(4)
            gate_pass(2, Pa, r_Pa)
            RG.release(n_ao)

            AR.reset()
            RG.set_alias(1)
            n_o0, Wo0, r_Wo0 = RG.acquire(8)
            n_o1, Wo1, r_Wo1 = RG.acquire(8)
            Wo = [(Wo0, r_Wo0), (Wo1, r_Wo1)]
            xr = [AR.alloc([128, D], F32) for _ in range(2)]
            zt = [AR.alloc([128, D], F32) for _ in range(2)]
            x1b = [AR.alloc([128, D], BF16) for _ in range(2)]
            x1T = AR.alloc([128, 8, 128], F32)
            lnt = [ln_tmp(), ln_tmp()]
            lg = AR.alloc([128, NE], F32)
            m8 = AR.alloc([128, 8], F32)
            nm = AR.alloc([128, 1], F32)
            Af = AR.alloc([128, NE], F32)
            ex = AR.alloc([128, NE], F32)
            ssum = AR.alloc([128, 1], F32)
            Abf = AR.alloc([128, NE], BF16)
            Asum = [AR.alloc([128, NE], BF16) for _ in range(2)]
            key = AR.alloc([128, NE], F32)
            k8 = AR.alloc([128, 8], F32)
            junk = AR.alloc([128, NE], F32)
            r_xr = [R(), R()]
            r_zt = [R(), R()]
            r_x1b = [R(), R()]
            r_x1T, r_lg, r_m8, r_nm, r_Af, r_ex, r_ss, r_Abf, r_key, r_k8, r_junk = (R() for _ in range(11))
            r_As = [R(), R()]
            scat_ops = []
            new_res = {}
            lg_all = AR.alloc([128, NT, NE], F32)
            r_lga = [R() for _ in range(NT)]

            x1b3 = [x1b[0], x1b[1], AR.alloc([128, D], BF16), AR.alloc([128, D], BF16)]
            r_x1b3 = [r_x1b[0], r_x1b[1], R(), R()]
            zt3 = [zt[0], zt[1], AR.alloc([128, D], F32)]
            r_zt3 = [r_zt[0], r_zt[1], R()]
            lnt3 = [lnt[0], lnt[1], ln_tmp()]

            def l1_A(i):
                k = i % 2
                z3 = i % 3
                src_t, src_op = res_src[i]
                dma("sp", xr[k], src_t[i * 128:(i + 1) * 128, :], (), [r_xr[k]], deps=[src_op])
                b = 2 * k
                for half in range(2):
                    W_, r_W = Wo[half]
                    for kc in range(8):
                        mm(bank(b + half), mergedT[:, kc, i * 128:(i + 1) * 128], W_[:, kc, :],
                           kc == 0, False, [r_mg[i], r_W], [r_bank[b + half]])
                    mm(bank(b + half), onesrow_f, rowsm[0:1, half * 512:(half + 1) * 512], False, True,
                       [r_cst, r_rowsm], [r_bank[b + half]])
                for half in range(2):
                    stt("dve", zt3[z3][:, half * 512:(half + 1) * 512], xr[k][:, half * 512:(half + 1) * 512], ALPHA,
                        bank(b + half), ALU.mult, ALU.add, [r_xr[k], r_bank[b + half]], [r_zt3[z3]])
                ln_stats(zt3[z3], r_zt3[z3], lnt3[z3])

            def l1_B1(i):
                z3 = i % 3
                ln_norm(zt3[z3], r_zt3[z3], lnt3[z3])

            def l1_Bg(i):
                z3 = i % 3
                k4 = i % 4
                ln_affine(zt3[z3], r_zt3[z3], lnbc[:, 0:D], lnbc[:, D:2 * D], r_lnbc)
                st_op = dma("sp", xres[i * 128:(i + 1) * 128, :], zt3[z3], [r_zt3[z3]], ())
                new_res[i] = (xres, st_op)
                cp("act", x1b3[k4], zt3[z3], [r_zt3[z3]], [r_x1b3[k4]])

            def l1_Bt1(i):
                z3 = i % 3
                b = 4
                for kc in range(8):
                    bb = b + kc // 4
                    tr(bank(bb)[:, (kc % 4) * 128:(kc % 4 + 1) * 128], zt3[z3][:, kc * 128:(kc + 1) * 128], ident_f,
                       [r_zt3[z3], r_cst], [r_bank[bb]])
                cp("act", x1T[:, 0:4, :], bank(b).rearrange("p (a b) -> p a b", a=4), [r_bank[b]], [r_x1T])
                cp("act", x1T[:, 4:8, :], bank(b + 1).rearrange("p (a b) -> p a b", a=4), [r_bank[b + 1]], [r_x1T])

            def l1_Bt2(i):
                c0_ = (i % 4) * NE
                for kc in range(8):
                    mm(bank(6)[:, c0_:c0_ + NE], x1T[:, kc, :], wr[:, kc, :], kc == 0, False,
                       [r_x1T, r_wr], [r_bank[6]])
                mm(bank(6)[:, c0_:c0_ + NE], onesrow_f, rowsm[0:1, D:D + NE], False, True,
                   [r_cst, r_rowsm], [r_bank[6]])

            def l1_C(i):
                k = i % 2
                c0_ = (i % 4) * NE
                lg = lg_all[:, i, :]
                r_lg = r_lga[i]
                cp("dve", lg, bank(6)[:, c0_:c0_ + NE], [r_bank[6]], [r_lg])
                S.op("dve", lambda e: e.max(out=m8, in_=lg), [r_lg], [r_m8])
                ts("dve", Af, lg, m8[:, 3:4], None, ALU.is_ge, None, [r_lg, r_m8], [r_Af])
                if i == 0:
                    ts("dve", Af, Af, vtok, None, ALU.mult, None, [r_Af, r_cst], [r_Af])
                ts("dve", nm, m8[:, 0:1], -1.0, None, ALU.mult, None, [r_m8], [r_nm])
                act(ex, lg, AF.Exp, [r_lg, r_nm], [r_ex], bias=nm[:, 0:1])
                stt("dve", ex, Af, 1.0, ex, ALU.mult, ALU.mult, [r_Af, r_ex], [r_ex, r_ss], accum_out=ssum)
                ts("dve", ssum, ssum, 1e-30, None, ALU.max, None, [r_ss], [r_ss])
                S.op("dve", lambda e: e.reciprocal(out=ssum, in_=ssum), [r_ss], [r_ss])
                ts("dve", G_all[:, i, :], ex, ssum[:, 0:1], None, ALU.mult, None, [r_ex, r_ss], [r_G[i]])
                cp("dve", Abf, Af, [r_Af], [r_Abf])
                if i > 0:
                    tt("dve", Asum[k], Asum[(i - 1) % 2], Abf, ALU.add, [r_As[(i - 1) % 2], r_Abf], [r_As[k]])
                else:
                    cp("dve", Asum[k], Abf, [r_Abf], [r_As[k]])

            def l1_Dm(i):
                c0_ = (i % 4) * NE
                pp = bank(7)[:, c0_:c0_ + NE]
                mm(pp, tri_bf, Abf, True, i == 0, [r_cbf, r_Abf], [r_bank[7]])
                if i > 0:
                    mm(pp, ones_bf, Asum[(i - 1) % 2], False, True,
                       [r_cbf, r_As[(i - 1) % 2]], [r_bank[7]])

            def l1_D(i):
                k3 = i % 4
                c0_ = (i % 4) * NE
                pp = bank(7)[:, c0_:c0_ + NE]
                stt("dve", key, pp, float(CAP - 1), base1, ALU.min, ALU.add,
                    [r_bank[7], r_cst], [r_key])
                tt("dve", key, key, Af, ALU.mult, [r_key, r_Af], [r_key])
                S.op("dve", lambda e: e.max(out=k8, in_=key), [r_key], [r_k8])
                cp("dve", idx_all[:, i * 4:(i + 1) * 4], k8[:, 0:4], [r_k8], [r_idx[i]])
                for kk in range(4):
                    stt("dve", junk, key, k8[:, kk:kk + 1], G_all[:, i, :], ALU.is_equal, ALU.mult,
                        [r_key, r_k8, r_G[i]], [r_junk, r_gate[i]],
                        accum_out=gate_all[:, i * 4 + kk:i * 4 + kk + 1])
                for kk in range(4):
                    o = S.dma("pool", lambda e, s_=x1b3[k3], ix=idx_all[:, i * 4 + kk:i * 4 + kk + 1]:
                              e.indirect_dma_start(out=xd[:, :],
                                                   out_offset=bass.IndirectOffsetOnAxis(ap=ix, axis=0),
                                                   in_=s_, in_offset=None,
                                                   bounds_check=regs["bc"], oob_is_err=False),
                              [r_x1b3[k3], r_idx[i]], ())
                    scat_ops.append(o)

            for step in range(NT + 4):
                if 0 <= step - 2 < NT:
                    l1_Bt1(step - 2)
                if 0 <= step - 1 < NT:
                    l1_B1(step - 1)
                if 0 <= step - 4 < NT:
                    l1_Dm(step - 4)
                    l1_D(step - 4)
                if 0 <= step - 3 < NT:
                    l1_C(step - 3)
                if step < NT:
                    l1_A(step)
                if 0 <= step - 1 < NT:
                    l1_Bg(step - 1)
                if 0 <= step - 2 < NT:
                    l1_Bt2(step - 2)
            RG.release(n_o0)
            RG.release(n_o1)
            res_src = new_res

            AR.reset()
            RG.set_alias(2)
            brflat = brT[:, :, :].rearrange("p a b -> p (a b)")
            xg = [brflat[:, q_ * 3 * D:(q_ + 1) * 3 * D].rearrange("p (a b) -> p a b", a=3) for q_ in range(2)]
            xgT = [AR.alloc([128, 8, CAP], BF16) for _ in range(2)]
            actT = [AR.alloc([128, 8, CAP], BF16) for _ in range(2)]
            gc = [AR.alloc([128, CAP], F32) for _ in range(2)]
            sgm = [AR.alloc([128, CAP], F32) for _ in range(2)]
            t1 = [AR.alloc([128, CAP], F32) for _ in range(2)]
            ybf = [AR.alloc([128, D], BF16) for _ in range(2)]
            r_xg = [R(), R()]
            r_xgT = [R(), R()]
            r_actT = [R(), R()]
            r_gc = [R(), R()]
            r_sgm = [R(), R()]
            r_t1 = [R(), R()]
            r_ybf = [R(), R()]
            ys_ops = []
            cnt_ = {"it": 0, "ity": 0}

            def load_xg(e_):
                p = e_ % 2
                r0 = 1 + e_ * CAP
                dma("sp", xg[p], xd[r0:r0 + CAP, :].rearrange("(t p) d -> p t d", p=128), (), [r_xg[p]],
                    deps=scat_ops if e_ < 2 else ())

            def transposes(e_):
                p = e_ % 2
                for kp in range(4):
                    b = next_bank()
                    pb = bank_bf(b)
                    for kc2 in range(2):
                        kc = kp * 2 + kc2
                        for ti in range(3):
                            tr(pb[:, kc2 * CAP + ti * 128:kc2 * CAP + (ti + 1) * 128],
                               xg[p][:, ti, kc * 128:(kc + 1) * 128], ident_bf, [r_xg[p], r_cbf], [r_bank[b]])
                    cp("act" if kp % 2 else "dve", xgT[p][:, kp * 2:kp * 2 + 2, :],
                       pb[:, 0:2 * CAP].rearrange("p (a b) -> p a b", a=2), [r_bank[b]], [r_xgT[p]])

            def up(e_, hf):
                p = e_ % 2
                n_ug, Ug, r_Ug = RG.acquire(8)
                n_ul, Ul, r_Ul = RG.acquire(8)
                for jj in range(4):
                    j = hf * 4 + jj
                    bg = next_bank()
                    for kc in range(8):
                        mm(bank(bg)[:, 0:CAP], Ug[:, kc, jj * 128:(jj + 1) * 128], xgT[p][:, kc, :],
                           kc == 0, kc == 7, [r_Ug, r_xgT[p]], [r_bank[bg]])
                    bl = next_bank()
                    for kc in range(8):
                        mm(bank(bl)[:, 0:CAP], Ul[:, kc, jj * 128:(jj + 1) * 128], xgT[p][:, kc, :],
                           kc == 0, kc == 7, [r_Ul, r_xgT[p]], [r_bank[bl]])
                    k = cnt_["it"] % 2
                    cnt_["it"] += 1
                    ts("dve", gc[k], bank(bg)[:, 0:CAP], bcol(C_UPB + e_ * 16 + j), 7.0, ALU.add, ALU.min,
                       [r_bank[bg], r_colp], [r_gc[k]])
                    act(sgm[k], gc[k], AF.Sigmoid, [r_gc[k]], [r_sgm[k]], scale=1.702)
                    act(t1[k], bank(bl)[:, 0:CAP], AF.Identity, [r_bank[bl], r_upb1], [r_t1[k]],
                        bias=upb1[:, e_ * 8 + j:e_ * 8 + j + 1])
                    ts("dve", t1[k], t1[k], -6.0, 8.0, ALU.max, ALU.min, [r_t1[k]], [r_t1[k]])
                    tt("dve", gc[k], gc[k], sgm[k], ALU.mult, [r_gc[k], r_sgm[k]], [r_gc[k]])
                    tt("dve", actT[p][:, j, :], t1[k], gc[k], ALU.mult, [r_t1[k], r_gc[k]], [r_actT[p]])
                RG.release(n_ug)
                RG.release(n_ul)

            def down(e_):
                p = e_ % 2
                r0 = 1 + e_ * CAP
                n_d0, D0, r_D0 = RG.acquire(8)
                n_d1, D1, r_D1 = RG.acquire(8)
                Dn = [(D0, r_D0), (D1, r_D1)]
                for ti in range(3):
                    b = next_bank2()
                    for half in range(2):
                        W_, r_W = Dn[half]
                        for kc in range(8):
                            mm(bank(b + half), actT[p][:, kc, ti * 128:(ti + 1) * 128], W_[:, kc, :],
                               kc == 0, kc == 7, [r_actT[p], r_W], [r_bank[b + half]])
                    k = cnt_["ity"] % 2
                    cnt_["ity"] += 1
                    cp("act", ybf[k][:, 0:512], bank(b), [r_bank[b]], [r_ybf[k]])
                    cp("dve", ybf[k][:, 512:1024], bank(b + 1), [r_bank[b + 1]], [r_ybf[k]])
                    o = dma("sp", ys[r0 + ti * 128:r0 + (ti + 1) * 128, :], ybf[k], [r_ybf[k]], ())
                    ys_ops.append(o)
                RG.release(n_d0)
                RG.release(n_d1)

            load_xg(0)
            load_xg(1)
            transposes(0)
            up(0, 0)
            up(0, 1)
            for e_ in range(NE):
                if e_ + 1 < NE:
                    if e_ + 2 < NE:
                        load_xg(e_ + 2)
                    transposes(e_ + 1)
                    up(e_ + 1, 0)
                down(e_)
                if e_ + 1 < NE:
                    up(e_ + 1, 1)

            RG.set_alias(0)
            AR.reset()
            yk = [AR.alloc([128, 4, D], BF16) for _ in range(2)]
            xr = [AR.alloc([128, D], F32) for _ in range(2)]
            zt = [AR.alloc([128, D], F32) for _ in range(2)]
            x2b = [AR.alloc([128, D], BF16) for _ in range(2)]
            GT2 = [AR.alloc([NE, 128], F32), AR.alloc([NE, 128], F32)]
            r_GT2 = [R(), R()]
            lnt = [ln_tmp(), ln_tmp()]
            r_yk = [[R() for _ in range(4)] for _ in range(2)]
            r_xr = [R(), R()]
            r_zt = [R(), R()]
            r_x2b = [R(), R()]
            new_res = {}

            def l2_G(i):
                k = i % 2
                for kk in range(4):
                    S.dma("pool", lambda e, d_=yk[k][:, kk, :], ix=idx_all[:, i * 4 + kk:i * 4 + kk + 1]:
                          e.indirect_dma_start(out=d_, out_offset=None, in_=ys[:, :],
                                               in_offset=bass.IndirectOffsetOnAxis(ap=ix, axis=0),
                                               bounds_check=regs["bc"], oob_is_err=False),
                          [r_idx[i]], [r_yk[k][kk]], deps=ys_ops if (i == 0 and kk == 0) else ())
                src_t, src_op = res_src[i]
                dma("sp", xr[k], src_t[i * 128:(i + 1) * 128, :], (), [r_xr[k]], deps=[src_op])

            def l2_A0(i):
                k = i % 2
                tr(bank(4)[0:NE, 0:128], G_all[:, i, :], ident_f, [r_G[i], r_cst], [r_bank[4]])
                cp("act", GT2[k], bank(4)[0:NE, 0:128], [r_bank[4]], [r_GT2[k]])
                b = 2 * k
                for half in range(2):
                    mm(bank(b + half), GT2[k], bdown[:, half * 512:(half + 1) * 512], True, True,
                       [r_GT2[k], r_bdown], [r_bank[b + half]])

            def l2_A(i):
                k = i % 2
                b = 2 * k
                for half in range(2):
                    stt("dve", zt[k][:, half * 512:(half + 1) * 512], xr[k][:, half * 512:(half + 1) * 512], ALPHA,
                        bank(b + half), ALU.mult, ALU.add, [r_xr[k], r_bank[b + half]], [r_zt[k]])
                for kk in range(4):
                    stt("dve", zt[k], yk[k][:, kk, :], gate_all[:, i * 4 + kk:i * 4 + kk + 1], zt[k],
                        ALU.mult, ALU.add, [r_yk[k][kk], r_gate[i], r_zt[k]], [r_zt[k]])
                ln_stats(zt[k], r_zt[k], lnt[k])

            def l2_B1(i):
                k = i % 2
                ln_norm(zt[k], r_zt[k], lnt[k])

            def l2_B2(i):
                k = i % 2
                ln_affine(zt[k], r_zt[k], lnbc[:, 2 * D:3 * D], lnbc[:, 3 * D:4 * D], r_lnbc)
                if last:
                    if i > 0:
                        o = dma("sp", out_d[(i - 1) * 128:i * 128, :], zt[k], [r_zt[k]], ())
                        final_ops.append(o)
                else:
                    st_op = dma("sp", xres[i * 128:(i + 1) * 128, :], zt[k], [r_zt[k]], ())
                    new_res[i] = (xres, st_op)
                    cp("act", x2b[k], zt[k], [r_zt[k]], [r_x2b[k]])
                    b = 5 + k
                    pb = bank_bf(b)
                    for kc in range(8):
                        tr(pb[:, kc * 128:(kc + 1) * 128], x2b[k][:, kc * 128:(kc + 1) * 128], ident_bf,
                           [r_x2b[k], r_cbf], [r_bank[b]])
                    cp("act", xT[:, :, i * 128:(i + 1) * 128], pb.rearrange("p (a b) -> p a b", a=8),
                       [r_bank[b]], [r_xT[i]])

            l2_G(0)
            l2_A0(0)
            for step in range(NT + 1):
                if step + 1 < NT:
                    l2_G(step + 1)
                if 0 <= step - 1 < NT:
                    l2_B1(step - 1)
                if step < NT:
                    l2_A(step)
                if step + 1 < NT:
                    l2_A0(step + 1)
                if 0 <= step - 1 < NT:
                    l2_B2(step - 1)
            res_src = new_res

        assert RG.next_acq == len(plan), (RG.next_acq, len(plan))
        S.finalize(final_ops)
    return nc


def _consts(half):
    c = np.zeros((128, NCST), np.float32)
    c[:, K_ID:K_ID + 128] = np.eye(128, dtype=np.float32)
    tp = np.arange(128)
    c[:, K_TRI:K_TRI + 128] = (tp[:, None] < tp[None, :]).astype(np.float32)
    c[:, K_ONE:K_ONE + 128] = 1.0
    valid = 0.0 if half == 0 else 1.0
    c[:, K_VM:K_VM + 128] = valid
    c[:, K_VT] = valid
    for g, w in enumerate((2, 4, 8, 16)):
        for t in range(16):
            cnt = min(t + 1, w) if half == 0 else w
            c[:, K_PF + g * 16 + t] = w / cnt
    c[:, K_B1:K_B1 + 32] = (1 + np.arange(NE) * CAP)[None, :].astype(np.float32)
    return c


def _layer_params(inp, ls):
    f = lambda a: np.ascontiguousarray(np.asarray(a, dtype=np.float32))
    colp = np.zeros((len(ls), 128, NCOLP), np.float32)
    for n, l in enumerate(ls):
        colp[n, :, C_BIN:C_BIN + 40] = inp["b_in"][l].reshape(40, 128).T
        colp[n, :, C_PSC:C_PSC + 4] = inp["pool_scale"][l].reshape(4, 128).T
        colp[n, :, C_CDW:C_CDW + 124] = inp["conv_dw"][l].T.reshape(4, 128, 31).transpose(1, 0, 2).reshape(128, 124)
        colp[n, :, C_CDWB:C_CDWB + 4] = inp["conv_dw_b"][l].reshape(4, 128).T
        colp[n, :, C_CLG:C_CLG + 4] = inp["conv_ln_g"][l].reshape(4, 128).T
        colp[n, :, C_CLB:C_CLB + 4] = inp["conv_ln_b"][l].reshape(4, 128).T
        colp[n, :, C_CPWB:C_CPWB + 8] = inp["conv_pw_b"][l].reshape(8, 128).T
        colp[n, :, C_UPB:C_UPB + 512] = inp["exp_up_b"][l].reshape(NE, 16, 128).transpose(2, 0, 1).reshape(128, 512)
    rowbc = np.stack([np.concatenate([inp["ln1_g"][l], inp["ln1_b"][l], inp["ln2_g"][l], inp["ln2_b"][l]])[None, :]
                      for l in ls]).astype(np.float32)
    rowsm = np.stack([np.concatenate([inp["b_out"][l], inp["router_b"][l]])[None, :] for l in ls]).astype(np.float32)
    sl = slice(ls[0], ls[-1] + 1)
    d = {
        "w_in": f(inp["w_in"][sl]), "pool_w": f(inp["pool_w"][sl]), "pool_proj": f(inp["pool_proj"][sl]),
        "conv_pw": f(inp["conv_pw"][sl]), "attn_o": f(inp["attn_o"][sl]), "w_kv": f(inp["w_kv"][sl]),
        "w_out": f(inp["w_out"][sl]), "exp_up": f(inp["exp_up"][sl]), "exp_down": f(inp["exp_down"][sl]),
        "router_w": f(inp["router_w"][sl]), "exp_down_b": f(inp["exp_down_b"][sl]),
        "colp": colp, "rowbc": rowbc, "rowsm": rowsm,
    }
    return d


def _x_shards(xfull):
    outs = []
    for c in range(8):
        b, half = c // 2, c % 2
        xs = np.zeros((T, D), np.float32)
        s0 = half * 2048
        xs[128:] = xfull[b, s0:s0 + 2048]
        if half == 1:
            xs[:128] = xfull[b, s0 - 128:s0]
        outs.append(xs)
    return outs


_PROG_CACHE = {}


def _get_prog(NL):
    if NL not in _PROG_CACHE:
        _PROG_CACHE[NL] = build_program(NL)
    return _PROG_CACHE[NL]


LAYERS_PER_LAUNCH = 4


def kernel(**inp):
    inp = {k: np.asarray(v) for k, v in inp.items()}
    x = np.asarray(inp["x"], np.float32)
    memln = np.concatenate([inp["mem_ln_g"], inp["mem_ln_b"]])[None, :].astype(np.float32)
    NL = LAYERS_PER_LAUNCH
    nc = _get_prog(NL)
    csts = [_consts(c % 2) for c in range(8)]
    for l0 in range(0, DEPTH, NL):
        lp = _layer_params(inp, list(range(l0, l0 + NL)))
        xs = _x_shards(x)
        in_maps = []
        for c in range(8):
            m = dict(lp)
            m["x_in"] = xs[c]
            m["mem"] = np.ascontiguousarray(inp["mem"][c // 2], dtype=np.float32)
            m["memln"] = memln
            m["cst"] = csts[c]
            in_maps.append(m)
        res = run_bass_kernel_spmd(nc, in_maps, core_ids=list(range(8)))
        xn = np.empty_like(x)
        for c in range(8):
            b, half = c // 2, c % 2
            xn[b, half * 2048:(half + 1) * 2048] = np.asarray(res.results[c]["out"], np.float32)
        x = xn
    return x
```
